# Optimizing a Trainium2 kernel written in Bass

```python
import math
import jax, jax.numpy as jnp
from jax import lax
import numpy as np

D_MODEL = 2048
BATCH = 1
SEQ = 16384
DEPTH = 1

GRID_W = 64
CTX_LEN = 256
A_HEADS = 8
A_QK_DIM = 64
A_V_DIM = 2 * A_QK_DIM
ROPE_THETA = 10000.0
B_HEADS = 8
B_HEAD_DIM = 128
NA_KH = 8
NA_KW = 16
A_WIDTH = A_HEADS * A_V_DIM
B_WIDTH = B_HEADS * B_HEAD_DIM
IN_SIZES = (A_HEADS * 2 * A_QK_DIM, A_HEADS * 2 * A_QK_DIM, A_WIDTH,
            B_WIDTH, B_WIDTH, B_WIDTH, D_MODEL, D_MODEL)
N_IN = sum(IN_SIZES)
PEER_HEADS = 8
PEER_NKEYS = 128
PEER_N = PEER_NKEYS * PEER_NKEYS
PEER_TOPK = 16
PEER_QDIM = 256
PEER_CHUNK = 128
Q_BLOCK = 128
EPS = 1e-6

kernel_name = "hybrid_diffattn_natten_peer_dit_block"


def rmsnorm(x, g):
    xf = x.astype(jnp.float32)
    y = xf * lax.rsqrt(jnp.mean(xf * xf, axis=-1, keepdims=True) + EPS)
    return (y * g.astype(jnp.float32)).astype(x.dtype)


def ada_params(cond, w, b):
    m = jax.nn.silu(cond) @ w + b
    return [t[:, None, :] for t in jnp.split(m, 6, axis=-1)]


def modulate(x, g, shift, scale):
    return rmsnorm(x, g) * (1 + scale) + shift


def split_proj(p):
    B, T, _ = p.shape
    offs = [int(o) for o in np.cumsum(IN_SIZES)[:-1]]
    qa, ka, va, qb, kb, vb, ga, gb = jnp.split(p, offs, axis=-1)
    qa = qa.reshape(B, T, A_HEADS, 2, A_QK_DIM)
    ka = ka.reshape(B, T, A_HEADS, 2, A_QK_DIM)
    va = va.reshape(B, T, A_HEADS, A_V_DIM)
    qb = qb.reshape(B, T, B_HEADS, B_HEAD_DIM)
    kb = kb.reshape(B, T, B_HEADS, B_HEAD_DIM)
    vb = vb.reshape(B, T, B_HEADS, B_HEAD_DIM)
    return qa, ka, va, qb, kb, vb, ga, gb


def axial_rope_tables(seq_len, dtype):
    t = jnp.arange(seq_len)
    row = (t // GRID_W).astype(jnp.float32)
    col = (t % GRID_W).astype(jnp.float32)
    n_freq = A_QK_DIM // 4
    freqs = ROPE_THETA ** (-jnp.arange(n_freq, dtype=jnp.float32) / n_freq)
    ar = row[:, None] * freqs[None, :]
    ac = col[:, None] * freqs[None, :]
    ang = jnp.concatenate([ar, ar, ac, ac], axis=-1)
    return jnp.cos(ang).astype(dtype), jnp.sin(ang).astype(dtype)


def rotate_half_axial(x):
    xr = x.reshape(x.shape[:-1] + (2, 2, A_QK_DIM // 4))
    xr = jnp.stack([-xr[..., 1, :], xr[..., 0, :]], axis=-2)
    return xr.reshape(x.shape)


def apply_rope(x, cos, sin):
    c = cos[None, :, None, None, :]
    s = sin[None, :, None, None, :]
    return x * c + rotate_half_axial(x) * s


def diff_attention(q, k, v, lam, lam_init, g):
    B, T, H, _, d = q.shape
    nb = T // Q_BLOCK
    scale = d ** -0.5
    qb = q.reshape(B, nb, Q_BLOCK, H, 2, d).swapaxes(0, 1)

    def block(qblk):
        s = jnp.einsum('bqhid,bkhid->ibhqk', qblk, k).astype(jnp.float32) * scale
        p = jax.nn.softmax(s, axis=-1)
        a = p[0] - lam * p[1]
        return jnp.einsum('bhqk,bkhe->bqhe', a.astype(v.dtype), v)

    o = lax.map(block, qb)
    o = o.swapaxes(0, 1).reshape(B, T, H, v.shape[-1])
    o = rmsnorm(o, g) * (1.0 - lam_init)
    return o.reshape(B, T, H * v.shape[-1])


def neighbourhood_attention(q, k, v, kc, vc, rpb):
    B, T, H, d = q.shape
    rows = T // GRID_W
    kh = min(NA_KH, rows)
    kw = NA_KW
    scale = d ** -0.5
    qg = q.reshape(B, rows, GRID_W, H, d)
    kg = k.reshape(B, rows, GRID_W, H, d)
    vg = v.reshape(B, rows, GRID_W, H, d)
    cols = jnp.arange(GRID_W)
    col_start = jnp.clip(cols - kw // 2, 0, GRID_W - kw)
    col_idx = col_start[:, None] + jnp.arange(kw)[None, :]
    bias_c = rpb[:, :, col_idx - cols[:, None] + (NA_KW - 1)]

    def row(r):
        rs = jnp.clip(r - kh // 2, 0, rows - kh)
        dr = rs + jnp.arange(kh) - r
        bias = bias_c[:, dr + (NA_KH - 1)].transpose(0, 2, 1, 3)
        q_r = lax.dynamic_index_in_dim(qg, r, axis=1, keepdims=False)
        k_win = lax.dynamic_slice_in_dim(kg, rs, kh, axis=1)[:, :, col_idx]
        v_win = lax.dynamic_slice_in_dim(vg, rs, kh, axis=1)[:, :, col_idx]
        s_win = jnp.einsum('bqhd,biqjhd->bhqij', q_r, k_win).astype(jnp.float32) * scale
        s_win = s_win + bias[None].astype(jnp.float32)
        s_ctx = jnp.einsum('bqhd,bchd->bhqc', q_r, kc).astype(jnp.float32) * scale
        s = jnp.concatenate([s_win.reshape(B, H, GRID_W, kh * kw), s_ctx], axis=-1)
        p = jax.nn.softmax(s, axis=-1).astype(v.dtype)
        p_win = p[..., :kh * kw].reshape(B, H, GRID_W, kh, kw)
        p_ctx = p[..., kh * kw:]
        return (jnp.einsum('bhqij,biqjhd->bqhd', p_win, v_win)
                + jnp.einsum('bhqc,bchd->bqhd', p_ctx, vc))

    o = lax.map(row, jnp.arange(rows))
    return o.transpose(1, 0, 2, 3, 4).reshape(B, T, H * d)


def full_attention(q, k, v):
    B, T, H, d = q.shape
    s = jnp.einsum('bqhd,bkhd->bhqk', q, k).astype(jnp.float32) * (d ** -0.5)
    p = jax.nn.softmax(s, axis=-1).astype(v.dtype)
    return jnp.einsum('bhqk,bkhd->bqhd', p, v).reshape(B, T, H * d)


def merge(out_a, out_b, ga, gb, w_a, w_b, w_o):
    return (jax.nn.sigmoid(ga) * (out_a @ w_a) + jax.nn.sigmoid(gb) * (out_b @ w_b)) @ w_o


def peer(h, w_q, subkeys, u, v):
    B, T, D = h.shape
    n = B * T
    tok = h.reshape(n, D)
    q = (tok @ w_q).reshape(n, PEER_HEADS, 2, PEER_QDIM // 2)
    s = jnp.einsum('nhpd,hpkd->nhpk', q, subkeys).astype(jnp.float32)
    s1, i1 = lax.top_k(s[:, :, 0], PEER_TOPK)
    s2, i2 = lax.top_k(s[:, :, 1], PEER_TOPK)
    cand = (s1[..., :, None] + s2[..., None, :]).reshape(n, PEER_HEADS, PEER_TOPK * PEER_TOPK)
    cidx = (i1[..., :, None] * PEER_NKEYS + i2[..., None, :]).reshape(n, PEER_HEADS, PEER_TOPK * PEER_TOPK)
    best, pos = lax.top_k(cand, PEER_TOPK)
    idx = jnp.take_along_axis(cidx, pos, axis=-1)
    gates = jax.nn.softmax(best, axis=-1).astype(h.dtype)
    nc = n // PEER_CHUNK

    def chunk(args):
        t, ix, gg = args
        act = jax.nn.gelu(jnp.einsum('nd,nhkd->nhk', t, u[ix]))
        return jnp.einsum('nhk,nhkd->nd', gg * act, v[ix])

    out = lax.map(chunk, (tok.reshape(nc, PEER_CHUNK, D),
                          idx.reshape(nc, PEER_CHUNK, PEER_HEADS, PEER_TOPK),
                          gates.reshape(nc, PEER_CHUNK, PEER_HEADS, PEER_TOPK)))
    return out.reshape(B, T, D)


def setup_inputs(seed: int = 0) -> dict:
    key = jax.random.key(seed)
    ks = jax.random.split(key, 24)
    D = D_MODEL

    def nrm(k, shape, s):
        return jax.random.normal(k, shape, jnp.float32) * s

    return {
        'x': nrm(ks[0], (BATCH, SEQ, D), 1.0),
        'c': nrm(ks[1], (BATCH, D), 1.0),
        'ctx': nrm(ks[2], (BATCH, CTX_LEN, D), 1.0),
        'c_ctx': nrm(ks[3], (D,), 1.0),
        'w_mod': nrm(ks[4], (DEPTH, D, 6 * D), 0.5 * D ** -0.5),
        'b_mod': nrm(ks[5], (DEPTH, 6 * D), 0.01),
        'norm1_g': 1.0 + nrm(ks[6], (DEPTH, D), 0.02),
        'norm2_g': 1.0 + nrm(ks[7], (DEPTH, D), 0.02),
        'w_in': nrm(ks[8], (DEPTH, D, N_IN), D ** -0.5),
        'lambda_q1': nrm(ks[9], (DEPTH, A_QK_DIM), 0.1),
        'lambda_k1': nrm(ks[10], (DEPTH, A_QK_DIM), 0.1),
        'lambda_q2': nrm(ks[11], (DEPTH, A_QK_DIM), 0.1),
        'lambda_k2': nrm(ks[12], (DEPTH, A_QK_DIM), 0.1),
        'subln_g': 1.0 + nrm(ks[13], (DEPTH, A_V_DIM), 0.02),
        'na_rpb': nrm(ks[14], (DEPTH, B_HEADS, 2 * NA_KH - 1, 2 * NA_KW - 1), 0.1),
        'w_branch_a': nrm(ks[15], (DEPTH, A_WIDTH, D), A_WIDTH ** -0.5),
        'w_branch_b': nrm(ks[16], (DEPTH, B_WIDTH, D), B_WIDTH ** -0.5),
        'w_out': nrm(ks[17], (DEPTH, D, D), D ** -0.5),
        'peer_w_q': nrm(ks[18], (DEPTH, D, PEER_HEADS * PEER_QDIM), D ** -0.5),
        'peer_subkeys': nrm(ks[19], (DEPTH, PEER_HEADS, 2, PEER_NKEYS, PEER_QDIM // 2), (PEER_QDIM // 2) ** -0.5),
        'peer_u': nrm(ks[20], (DEPTH, PEER_N, D), D ** -0.5),
        'peer_v': nrm(ks[21], (DEPTH, PEER_N, D), PEER_HEADS ** -0.5),
        'final_g': 1.0 + nrm(ks[22], (D,), 0.02),
    }


def reference(x, c, ctx, c_ctx, w_mod, b_mod, norm1_g, norm2_g, w_in,
              lambda_q1, lambda_k1, lambda_q2, lambda_k2, subln_g, na_rpb,
              w_branch_a, w_branch_b, w_out, peer_w_q, peer_subkeys, peer_u, peer_v,
              final_g):
    S = x.shape[1]
    cos, sin = axial_rope_tables(S, x.dtype)
    for l in range(DEPTH):
        lam_init = 0.8 - 0.6 * math.exp(-0.3 * l)
        lam = (jnp.exp(jnp.sum(lambda_q1[l].astype(jnp.float32) * lambda_k1[l].astype(jnp.float32)))
               - jnp.exp(jnp.sum(lambda_q2[l].astype(jnp.float32) * lambda_k2[l].astype(jnp.float32)))
               + lam_init)
        sh1, sc1, gt1, sh2, sc2, gt2 = ada_params(c, w_mod[l], b_mod[l])
        sh1c, sc1c, gt1c, sh2c, sc2c, gt2c = ada_params(c_ctx[None, :], w_mod[l], b_mod[l])

        hx = modulate(x, norm1_g[l], sh1, sc1)
        hc = modulate(ctx, norm1_g[l], sh1c, sc1c)
        qa, ka, va, qb, kb, vb, ga, gb = split_proj(hx @ w_in[l])
        qa_c, ka_c, va_c, qb_c, kb_c, vb_c, ga_c, gb_c = split_proj(hc @ w_in[l])
        qa = apply_rope(qa, cos, sin)
        ka = apply_rope(ka, cos, sin)
        out_a = diff_attention(qa, jnp.concatenate([ka, ka_c], axis=1),
                               jnp.concatenate([va, va_c], axis=1), lam, lam_init, subln_g[l])
        out_b = neighbourhood_attention(qb, kb, vb, kb_c, vb_c, na_rpb[l])
        x_new = x + gt1 * merge(out_a, out_b, ga, gb, w_branch_a[l], w_branch_b[l], w_out[l])

        x_new = x_new + gt2 * peer(modulate(x_new, norm2_g[l], sh2, sc2),
                                   peer_w_q[l], peer_subkeys[l], peer_u[l], peer_v[l])

        if l < DEPTH - 1:
            ca = diff_attention(qa_c, ka_c, va_c, lam, lam_init, subln_g[l])
            cb = full_attention(qb_c, kb_c, vb_c)
            ctx = ctx + gt1c * merge(ca, cb, ga_c, gb_c, w_branch_a[l], w_branch_b[l], w_out[l])
            ctx = ctx + gt2c * peer(modulate(ctx, norm2_g[l], sh2c, sc2c),
                                    peer_w_q[l], peer_subkeys[l], peer_u[l], peer_v[l])
        x = x_new
    return rmsnorm(x, final_g)
```

```python
import numpy as np
from contextlib import ExitStack
import concourse.bass as bass
import concourse.mybir as mybir
from concourse.bass_utils import run_bass_kernel_spmd

F32 = mybir.dt.float32
BF16 = mybir.dt.bfloat16
U32 = mybir.dt.uint32
AF = mybir.ActivationFunctionType
ALU = mybir.AluOpType
AX = mybir.AxisListType

EPOCH = 12000
SAME_ENG_SYNC = {'pe': False, 'act': True, 'dve': True, 'pool': True, 'sp': False}

D = 2048
NTOK = 16384
NCORE = 8
OWN = 2048
NLOC = 2816
NKEY = NTOK + 256
EPS = 1e-6
NEG = -30000.0


class Buf:
    def __init__(self, name, t):
        self.name = name
        self.t = t
        self.w = {}
        self.r = {}
        self.dsem = None
        self.dcnt = 0

    def __getitem__(self, idx):
        return self.t[idx]


def _merge(d, s):
    for k, v in s.items():
        if d.get(k, 0) < v:
            d[k] = v


class Sched:
    CE = ['pe', 'act', 'dve', 'pool']
    ENG = ['pe', 'act', 'dve', 'pool', 'sp']

    def __init__(self, nc, es):
        self.nc = nc
        self.es = es
        self.es_phase = None
        self.prog = {e: [] for e in self.ENG}
        self.cnt = {e: 0 for e in self.CE}
        self.waited = {e: {} for e in self.ENG}
        self.semobj = {}
        self.cur = {}
        self.nd = 0
        self.nops = 0
        self.free_dsems = []
        self.phase_bufs = []

    def _sem(self, key):
        if key not in self.semobj:
            name = 's_' + '_'.join(str(x) for x in key)
            self.semobj[key] = self.es.enter_context(self.nc.semaphore(name))
        return self.semobj[key]

    def sb(self, name, shape, dt, persistent=False):
        st = self.es if persistent else self.es_phase
        self.nd_names = getattr(self, 'nd_names', 0) + 1
        name = "%s_%d" % (name, self.nd_names)
        b = Buf(name, st.enter_context(self.nc.sbuf_tensor(name, list(shape), dt)))
        if not persistent:
            self.phase_bufs.append(b)
        return b

    def ps(self, name, shape, dt):
        self.nd_names = getattr(self, 'nd_names', 0) + 1
        name = "%s_%d" % (name, self.nd_names)
        return Buf(name, self.es_phase.enter_context(self.nc.psum_tensor(name, list(shape), dt)))

    def _deps(self, eng, reads, writes, skip=None):
        deps = {}
        for b in reads:
            _merge(deps, b.w)
        for b in writes:
            _merge(deps, b.w)
            _merge(deps, b.r)
        waits = []
        for k, v in deps.items():
            if skip is not None and k == skip:
                continue
            if k[0] == eng and not SAME_ENG_SYNC[eng]:
                continue
            if self.waited[eng].get(k, 0) >= v:
                continue
            self.waited[eng][k] = v
            waits.append((k, v))
        return waits

    def op(self, eng, fn, reads=(), writes=()):
        waits = self._deps(eng, reads, writes)
        self.cnt[eng] += 1
        c = self.cnt[eng]
        key = (eng, (c - 1) // EPOCH)
        val = (c - 1) % EPOCH + 1
        self._sem(key)
        self.cur[key] = val
        self.prog[eng].append((waits, fn, key, 1))
        for b in reads:
            b.r[key] = max(b.r.get(key, 0), val)
        for b in writes:
            b.w[key] = max(b.w.get(key, 0), val)
            b.r = {}
        self.nops += 1

    def dma(self, out_ap, in_ap, sembuf, reads=(), writes=(), **kw):
        if sembuf.dsem is None:
            if self.free_dsems:
                sembuf.dsem, sembuf.dcnt = self.free_dsems.pop()
            else:
                sembuf.dsem = ('d', self.nd)
                self.nd += 1
                self._sem(sembuf.dsem)
        key = sembuf.dsem
        waits = self._deps('sp', reads, writes, skip=key)
        sembuf.dcnt += 16
        val = sembuf.dcnt
        self.cur[key] = val

        def fn(e, out_ap=out_ap, in_ap=in_ap, kw=kw):
            return e.dma_start(out=out_ap, in_=in_ap, **kw)
        self.prog['sp'].append((waits, fn, key, 16))
        for b in reads:
            b.r[key] = max(b.r.get(key, 0), val)
        for b in writes:
            b.w[key] = max(b.w.get(key, 0), val)
            b.r = {}
        self.nops += 1

    def barrier(self):
        for e in self.ENG:
            waits = []
            for k, v in self.cur.items():
                if k[0] == e:
                    continue
                if self.waited[e].get(k, 0) >= v:
                    continue
                self.waited[e][k] = v
                waits.append((k, v))
            if waits:
                self.prog[e].append((waits, None, None, 0))

    def flush(self):
        nc = self.nc
        prog = self.prog
        self.prog = {e: [] for e in self.ENG}

        def run(eobj, lst):
            for waits, fn, key, inc in lst:
                for k, v in waits:
                    eobj.wait_ge(self.semobj[k], v)
                if fn is not None:
                    fn(eobj).then_inc(self.semobj[key], inc)

        with nc.Block() as block:
            @block.tensor
            def _(e):
                run(e, prog['pe'])

            @block.scalar
            def _(e):
                run(e, prog['act'])

            @block.vector
            def _(e):
                run(e, prog['dve'])

            @block.gpsimd
            def _(e):
                run(e, prog['pool'])

            @block.sync
            def _(e):
                run(e, prog['sp'])

    def end_phase(self, release=True):
        self.barrier()
        self.flush()
        if release:
            for b in self.phase_bufs:
                if b.dsem is not None:
                    self.free_dsems.append((b.dsem, b.dcnt))
                    b.dsem = None
            self.phase_bufs = []


def bc(ap, axis, shape):
    return ap.unsqueeze(axis).to_broadcast(list(shape))


def build_program(stop_after=None, dbg=()):
    nc = bass.Bass("TRN2", target_bir_lowering=False)

    def din(name, shape, dt=F32):
        return Buf(name, nc.dram_tensor(name, list(shape), dt, kind="ExternalInput").ap())

    def dscr(name, shape, dt):
        return Buf(name, nc.dram_tensor(name, list(shape), dt, kind="Internal").ap())

    x_all = din("x_all", [NTOK, D])
    x_own = din("x_own", [OWN, D])
    x_halo = din("x_halo", [512, D])
    ctx = din("ctx", [256, D])
    cT = din("cT", [128, 32])
    g1T = din("g1T", [128, 16])
    g2T = din("g2T", [128, 16])
    w_mod = din("w_mod", [D, 6 * D])
    b_mod = din("b_mod", [6 * D])
    w_in = din("w_in", [D, 10240])
    lams = din("lams", [256])
    subg = din("subg", [128])
    na_bias = din("na_bias", [8, 128, 5 * 5 * 128])
    rope_all = din("rope_all", [NTOK, 128])
    rope_own = din("rope_own", [OWN, 128])
    w_a = din("w_a", [1024, D])
    w_b = din("w_b", [1024, D])
    w_o = din("w_o", [D, D])
    w_q = din("w_q", [D, D])
    subT = din("subT", [128, 16 * 128])
    peer_u = din("peer_u", [NTOK, D])
    peer_v = din("peer_v", [NTOK, D])
    final_g = din("final_g", [D])
    out = Buf("out", nc.dram_tensor("out", [OWN, D], F32, kind="ExternalOutput").ap())

    mrow = dscr("mrow", [2, 6 * D], F32)
    KaT_d = dscr("KaT_d", [8, 128, NKEY], BF16)
    Va_d = dscr("Va_d", [NKEY, 1024], BF16)
    QaT_d = dscr("QaT_d", [8, 128, OWN], BF16)
    QbT_d = dscr("QbT_d", [8, 128, OWN], BF16)
    KbT_d = dscr("KbT_d", [8, 128, NLOC], BF16)
    Vb_d = dscr("Vb_d", [NLOC, 1024], BF16)
    GT_d = dscr("GT_d", [32, 128, OWN], BF16)
    OaT_d = dscr("OaT_d", [8, 128, OWN], BF16)
    ObT_d = dscr("ObT_d", [8, 128, OWN], BF16)
    mT_d = dscr("mT_d", [128, 16, OWN], BF16)
    X1_d = dscr("X1_d", [OWN, D], F32)
    UT_d = dscr("UT_d", [128, 128, 16 * 128], BF16)
    VB_d = dscr("VB_d", [NTOK, D], BF16)

    dbg_out = {}
    for name, shape in dbg:
        dbg_out[name] = Buf(name, nc.dram_tensor(name, list(shape), F32, kind="ExternalOutput").ap())

    with ExitStack() as es:
        S = Sched(nc, es)
        ident = S.sb("ident", [128, 128], BF16, True)
        identf = S.sb("identf", [128, 128], F32, True)
        iota_f = S.sb("iota_f", [128, 128], F32, True)
        modv = S.sb("modv", [128, 6, 16], F32, True)
        lamv = S.sb("lamv", [128, 4], F32, True)
        gsub = S.sb("gsub", [128, 128], F32, True)

        def phase_begin():
            S.es_phase = ExitStack()
            return S.es_phase

        with phase_begin():
            S.op('pool', lambda e: e.iota(identf[:, :], [[1, 128]], base=0, channel_multiplier=-1,
                                          allow_small_or_imprecise_dtypes=True), writes=[identf])
            S.op('pool', lambda e: e.tensor_single_scalar(out=identf[:, :], in_=identf[:, :], scalar=0.0,
                                                          op=ALU.is_equal), reads=[identf], writes=[identf])
            S.op('pool', lambda e: e.tensor_copy(out=ident[:, :], in_=identf[:, :]), reads=[identf], writes=[ident])
            S.op('pool', lambda e: e.iota(iota_f[:, :], [[1, 128]], base=0, channel_multiplier=0,
                                          allow_small_or_imprecise_dtypes=True), writes=[iota_f])
            c_sb = S.sb("c_sb", [128, 32], F32)
            sc_sb = S.sb("sc_sb", [128, 16, 2], F32)
            S.dma(c_sb[:, :], cT[:, :], c_sb, reads=[cT], writes=[c_sb])
            S.op('act', lambda e: e.activation(out=sc_sb.t[:, :, :].rearrange("p j r -> p (j r)"), in_=c_sb[:, :],
                                               func=AF.Silu), reads=[c_sb], writes=[sc_sb])
            bm_sb = S.sb("bm_sb", [2, 6 * D], F32)
            m_sb = S.sb("m_sb", [2, 6 * D], F32)
            S.dma(bm_sb[:, :], b_mod.t.partition_broadcast(2), bm_sb, reads=[b_mod], writes=[bm_sb])
            wm = [S.sb("wm%d" % i, [128, 16, 512], F32) for i in range(2)]
            pm = [S.ps("pm%d" % i, [2, 512], F32) for i in range(2)]
            for cb in range(24):
                wb = wm[cb % 2]
                S.dma(wb[:, :, :], w_mod.t[:, cb * 512:(cb + 1) * 512].rearrange("(j p) n -> p j n", p=128), wb,
                      reads=[w_mod], writes=[wb])

                def mm0(e, wb=wb, p=pm[cb % 2]):
                    for j in range(16):
                        ins = e.matmul(p[:, :], lhsT=sc_sb[:, j, :], rhs=wb[:, j, :], start=(j == 0), stop=(j == 15))
                    return ins
                S.op('pe', mm0, reads=[sc_sb, wb], writes=[pm[cb % 2]])
                S.op('dve', lambda e, p=pm[cb % 2], cb=cb: e.tensor_tensor(
                    out=m_sb[:, cb * 512:(cb + 1) * 512], in0=p[:, :], in1=bm_sb[:, cb * 512:(cb + 1) * 512],
                    op=ALU.add), reads=[pm[cb % 2], bm_sb], writes=[m_sb])
            S.dma(mrow[:, :], m_sb[:, :], m_sb, reads=[m_sb], writes=[mrow])
            pt0 = S.ps("pt0", [128, 96, 2], F32)
            modT = S.sb("modT", [128, 96, 2], F32)

            def tr0(e):
                for ch in range(96):
                    ins = e.transpose(out=pt0[:, ch, :], in_=m_sb[0:2, ch * 128:(ch + 1) * 128], identity=identf[0:2, 0:2])
                return ins
            S.op('pe', tr0, reads=[m_sb, identf], writes=[pt0])
            S.op('dve', lambda e: e.tensor_copy(out=modT[:, :, :], in_=pt0[:, :, :]), reads=[pt0], writes=[modT])
            g1_sb = S.sb("g1_sb", [128, 16], F32)
            g2_sb = S.sb("g2_sb", [128, 16], F32)
            S.dma(g1_sb[:, :], g1T[:, :], g1_sb, reads=[g1T], writes=[g1_sb])
            S.dma(g2_sb[:, :], g2T[:, :], g2_sb, reads=[g2T], writes=[g2_sb])
            def mk_a(dst, q, r, g):
                S.op('dve', lambda e: e.scalar_tensor_tensor(out=modv[:, dst, :], in0=modT[:, q * 16:(q + 1) * 16, r],
                                                             scalar=1.0, in1=g[:, :], op0=ALU.add, op1=ALU.mult),
                     reads=[modT, g], writes=[modv])

            def mk_b(dst, q, r):
                S.op('dve', lambda e: e.tensor_copy(out=modv[:, dst, :], in_=modT[:, q * 16:(q + 1) * 16, r]),
                     reads=[modT], writes=[modv])
            mk_a(0, 1, 0, g1_sb); mk_b(1, 0, 0)
            mk_a(2, 1, 1, g1_sb); mk_b(3, 0, 1)
            mk_a(4, 4, 0, g2_sb); mk_b(5, 3, 0)
            lq = S.sb("lq", [128, 4, 64], F32)
            lp = S.sb("lp", [128, 2, 64], F32)
            ls = S.sb("ls", [128, 2], F32)
            le = S.sb("le", [128, 2], F32)
            S.dma(lq.t[:, :, :].rearrange("p a b -> p (a b)"), lams.t.partition_broadcast(128), lq, reads=[lams], writes=[lq])
            S.op('dve', lambda e: e.tensor_tensor(out=lp[:, 0, :], in0=lq[:, 0, :], in1=lq[:, 1, :], op=ALU.mult), reads=[lq], writes=[lp])
            S.op('dve', lambda e: e.tensor_tensor(out=lp[:, 1, :], in0=lq[:, 2, :], in1=lq[:, 3, :], op=ALU.mult), reads=[lq, lp], writes=[lp])
            S.op('dve', lambda e: e.tensor_reduce(out=ls[:, :], in_=lp[:, :, :], axis=AX.X, op=ALU.add), reads=[lp], writes=[ls])
            S.op('act', lambda e: e.activation(out=le[:, :], in_=ls[:, :], func=AF.Exp), reads=[ls], writes=[le])
            S.op('dve', lambda e: e.tensor_tensor(out=lamv[:, 2:3], in0=le[:, 0:1], in1=le[:, 1:2], op=ALU.subtract), reads=[le], writes=[lamv])
            S.op('dve', lambda e: e.tensor_scalar(out=lamv[:, 0:1], in0=lamv[:, 2:3], scalar1=0.2, scalar2=None, op0=ALU.add), reads=[lamv], writes=[lamv])
            S.op('dve', lambda e: e.tensor_scalar(out=lamv[:, 1:2], in0=lamv[:, 0:1], scalar1=-1.0, scalar2=None, op0=ALU.mult), reads=[lamv], writes=[lamv])
            sg = S.sb("sg", [128, 128], F32)
            S.dma(sg[:, :], subg.t.partition_broadcast(128), sg, reads=[subg], writes=[sg])
            S.op('dve', lambda e: e.tensor_scalar(out=gsub[:, :], in0=sg[:, :], scalar1=0.8, scalar2=None, op0=ALU.mult), reads=[sg], writes=[gsub])
            if 'd_m' in dbg_out:
                S.dma(dbg_out['d_m'][:, :], m_sb[:, :], m_sb, reads=[m_sb], writes=[dbg_out['d_m']])
            S.end_phase()
        if stop_after == 0:
            return nc

        def make_norm(nx):
            R = {}
            R['xt'] = [S.sb("n_xt%d" % i, [128, D], F32) for i in range(nx)]
            R['junk'] = S.sb("n_junk", [128, D], BF16)
            R['ssq'] = [S.sb("n_ssq%d" % i, [128, 1], F32) for i in range(2)]
            R['std'] = [S.sb("n_std%d" % i, [128, 1], F32) for i in range(2)]
            R['rstd'] = [S.sb("n_rstd%d" % i, [128, 1], F32) for i in range(2)]
            R['xn'] = [S.sb("n_xn%d" % i, [128, D], BF16) for i in range(2)]
            R['ptr'] = [S.ps("n_ptr%d" % i, [128, 1024], BF16) for i in range(2)]
            R['tmp'] = S.sb("n_tmp", [128, D], F32)
            R['k'] = 0
            return R

        def norm_a1(R, src_ap, srcbuf, preloaded=None):
            k = R['k']
            R['k'] += 1
            if preloaded is None:
                xt = R['xt'][k % len(R['xt'])]
                S.dma(xt[:, :], src_ap, xt, reads=[srcbuf], writes=[xt])
            else:
                xt = preloaded
            ssq, std, rstd, xn = R['ssq'][k % 2], R['std'][k % 2], R['rstd'][k % 2], R['xn'][k % 2]
            junk = R['junk']
            S.op('act', lambda e: e.activation(out=junk[:, :], in_=xt[:, :], func=AF.Square, accum_out=ssq[:, :]),
                 reads=[xt], writes=[junk, ssq])
            S.op('act', lambda e: e.activation(out=std[:, :], in_=ssq[:, :], func=AF.Sqrt, scale=1.0 / D, bias=EPS),
                 reads=[ssq], writes=[std])
            S.op('dve', lambda e: e.reciprocal(out=rstd[:, :], in_=std[:, :]), reads=[std], writes=[rstd])
            S.op('act', lambda e: e.activation(out=xn[:, :], in_=xt[:, :], func=AF.Copy, scale=rstd[:, 0:1]),
                 reads=[xt, rstd], writes=[xn])
            return xn

        def norm_a2(R, xn, va, vb_, out_ap, outbuf):
            tmp = R['tmp']
            for half in range(2):
                ptr = R['ptr'][half]

                def trn(e, ptr=ptr, half=half):
                    for jj in range(8):
                        j = half * 8 + jj
                        ins = e.transpose(out=ptr[:, jj * 128:(jj + 1) * 128], in_=xn[:, j * 128:(j + 1) * 128], identity=ident[:, :])
                    return ins
                S.op('pe', trn, reads=[xn, ident], writes=[ptr])
                S.op('dve', lambda e, ptr=ptr, half=half: e.tensor_tensor(
                    out=tmp.t[:, half * 1024:(half + 1) * 1024].rearrange("p (j t) -> p j t", j=8),
                    in0=ptr.t[:, :].rearrange("p (j t) -> p j t", j=8),
                    in1=bc(modv[:, va, half * 8:(half + 1) * 8], 2, [128, 8, 128]), op=ALU.mult),
                    reads=[ptr, modv], writes=[tmp])
            S.op('pool', lambda e: e.tensor_tensor(
                out=out_ap, in0=tmp.t[:, :].rearrange("p (j t) -> p j t", j=16),
                in1=bc(modv[:, vb_, :], 2, [128, 16, 128]), op=ALU.add), reads=[tmp, modv], writes=[outbuf])

        def norm_tile(R, src_ap, srcbuf, va, vb_, out_ap, outbuf, preloaded=None):
            xn = norm_a1(R, src_ap, srcbuf, preloaded)
            norm_a2(R, xn, va, vb_, out_ap, outbuf)

        with phase_begin():
            Wkv = S.sb("Wkv", [128, 16, 2048], BF16)
            stg = [S.sb("stg%d" % i, [128, 16, 128], F32) for i in range(2)]
            for cb in range(16):
                st = stg[cb % 2]
                S.dma(st[:, :, :], w_in.t[:, 1024 + cb * 128:1024 + (cb + 1) * 128].rearrange("(j p) n -> p j n", p=128), st,
                      reads=[w_in], writes=[st])
                (S.op('act', lambda e, st=st, cb=cb: e.copy(out=Wkv[:, :, cb * 128:(cb + 1) * 128], in_=st[:, :, :]), reads=[st], writes=[Wkv]) if cb % 2 else
                 S.op('dve', lambda e, st=st, cb=cb: e.tensor_copy(out=Wkv[:, :, cb * 128:(cb + 1) * 128], in_=st[:, :, :]), reads=[st], writes=[Wkv]))
            R = make_norm(0)
            hT = [S.sb("hT%d" % i, [128, 16, 128], BF16) for i in range(3)]
            cs = [S.sb("cs%d" % i, [128, 128], F32) for i in range(3)]
            pk = [S.ps("pk%d" % i, [128, 512], F32) for i in range(4)]
            kraw = [[S.sb("kraw%d_%d" % (i, b_), [128, 512], F32) for b_ in range(2)] for i in range(2)]
            kf1 = [S.sb("kf1_%d" % i, [128, 512], F32) for i in range(2)]
            kf2 = [S.sb("kf2_%d" % i, [128, 512], F32) for i in range(2)]
            kb16 = [S.sb("kb16_%d" % i, [128, 1024], BF16) for i in range(2)]
            vb16 = [S.sb("vb16_%d" % i, [128, 1024], BF16) for i in range(2)]
            ptk = S.ps("ptk", [128, 1024], BF16)
            kst = [S.sb("kst%d" % i, [128, 8, 512], BF16) for i in range(2)]
            ntile = 130

            xts = [S.sb("xts%d" % i, [128, D], F32) for i in range(3)]

            def ldx(i):
                if i >= ntile:
                    return
                is_ctx = i >= 128
                src = ctx if is_ctx else x_all
                r0 = (i - 128) * 128 if is_ctx else i * 128
                S.dma(xts[i % 3][:, :], src[r0:r0 + 128, :], xts[i % 3], reads=[src], writes=[xts[i % 3]])

            xn_of = {}

            def stA1(i):
                if i >= ntile:
                    return
                if i == 0:
                    ldx(0)
                    ldx(1)
                ldx(i + 2)
                xn_of[i] = norm_a1(R, None, None, preloaded=xts[i % 3])

            def stA2(i):
                if i >= ntile:
                    return
                is_ctx = i >= 128
                r0 = (i - 128) * 128 if is_ctx else i * 128
                h = hT[i % 3]
                norm_a2(R, xn_of.pop(i), 2 if is_ctx else 0, 3 if is_ctx else 1, h[:, :, :], h)
                if not is_ctx:
                    S.dma(cs[i % 3][:, :], rope_all[r0:r0 + 128, :], cs[i % 3], reads=[rope_all], writes=[cs[i % 3]])

            def stB(i):
                is_ctx = i >= 128
                h = hT[i % 3]
                kb_ = kb16[i % 2]
                vb_ = vb16[i % 2]
                for blk in range(4):
                    p = pk[blk]

                    def mmk(e, p=p, blk=blk):
                        for j in range(16):
                            ins = e.matmul(p[:, :], lhsT=h[:, j, :], rhs=Wkv[:, j, blk * 512:(blk + 1) * 512], start=(j == 0), stop=(j == 15))
                        return ins
                    S.op('pe', mmk, reads=[h, Wkv], writes=[p])
                    if blk >= 2:
                        S.op('act', lambda e, p=p, blk=blk: e.copy(out=vb_[:, (blk - 2) * 512:(blk - 1) * 512], in_=p[:, :]), reads=[p], writes=[vb_])
                    elif is_ctx:
                        S.op('act', lambda e, p=p, blk=blk: e.copy(out=kb_[:, blk * 512:(blk + 1) * 512], in_=p[:, :]), reads=[p], writes=[kb_])
                    else:
                        kr = kraw[i % 2][blk]
                        S.op('act', lambda e, p=p, kr=kr: e.copy(out=kr[:, :], in_=p[:, :]), reads=[p], writes=[kr])
                if not is_ctx:
                    c_ = cs[i % 3]
                    for blk in range(2):
                        kr = kraw[i % 2][blk]
                        t1, t2 = kf1[blk], kf2[blk]
                        S.op('dve', lambda e, kr=kr, t1=t1: e.tensor_tensor(
                            out=t1.t[:, :].rearrange("p (g d) -> p g d", g=8), in0=kr.t[:, :].rearrange("p (g d) -> p g d", g=8),
                            in1=bc(c_[:, 0:64], 1, [128, 8, 64]), op=ALU.mult), reads=[kr, c_], writes=[t1])
                        for ab in range(2):
                            S.op('dve', lambda e, kr=kr, t2=t2, ab=ab: e.tensor_tensor(
                                out=t2.t[:, :].rearrange("p (g r a d) -> p g r a d", g=8, r=2, a=2)[:, :, :, ab, :],
                                in0=kr.t[:, :].rearrange("p (g r a d) -> p g r a d", g=8, r=2, a=2)[:, :, :, 1 - ab, :],
                                in1=bc(c_.t[:, 64:128].rearrange("p (r a d) -> p r a d", r=2, a=2)[:, :, ab, :], 1, [128, 8, 2, 16]),
                                op=ALU.mult), reads=[kr, c_], writes=[t2])
                        S.op('pool', lambda e, t1=t1, t2=t2, blk=blk: e.tensor_tensor(
                            out=kb_[:, blk * 512:(blk + 1) * 512], in0=t1[:, :], in1=t2[:, :], op=ALU.add),
                            reads=[t1, t2], writes=[kb_])
                S.dma(Va_d[i * 128:(i + 1) * 128, :], vb_[:, :], vb_, reads=[vb_], writes=[Va_d])

            def stC(i):
                kb_ = kb16[i % 2]

                def trk(e):
                    for hh in range(8):
                        ins = e.transpose(out=ptk[:, hh * 128:(hh + 1) * 128], in_=kb_[:, hh * 128:(hh + 1) * 128], identity=ident[:, :])
                    return ins
                S.op('pe', trk, reads=[kb_, ident], writes=[ptk])
                ks = kst[(i // 4) % 2]
                S.op('act', lambda e: e.copy(out=ks[:, :, (i % 4) * 128:(i % 4 + 1) * 128],
                                             in_=ptk.t[:, :].rearrange("p (h t) -> p h t", h=8)),
                     reads=[ptk], writes=[ks])
                if i % 4 == 3 or i == ntile - 1:
                    nt = (i % 4 + 1) * 128
                    t0 = (i // 4) * 512
                    S.dma(KaT_d.t[:, :, t0:t0 + nt].rearrange("h p t -> p h t"), ks[:, :, 0:nt], ks, reads=[ks], writes=[KaT_d])

            stA1(0)
            stA2(0)
            stA1(1)
            stA2(1)
            for k_ in range(ntile + 2):
                stA1(k_ + 2)
                if k_ < ntile:
                    stB(k_)
                if 1 <= k_ <= ntile:
                    stC(k_ - 1)
                stA2(k_ + 2)
            S.end_phase()

        with ExitStack() as outer1b:
            S.es_phase = outer1b
            hL = S.sb("hL", [128, 16, NLOC], BF16)
            with phase_begin():
                R = make_norm(2)
                for t in range(22):
                    if t < 2:
                        src, r0 = x_halo, t * 128
                    elif t < 18:
                        src, r0 = x_own, (t - 2) * 128
                    elif t < 20:
                        src, r0 = x_halo, 256 + (t - 18) * 128
                    else:
                        src, r0 = ctx, (t - 20) * 128
                    isc = t >= 20
                    norm_tile(R, src[r0:r0 + 128, :], src, 2 if isc else 0, 3 if isc else 1, hL[:, :, t * 128:(t + 1) * 128], hL)
                S.end_phase(release=False)
            S.es_phase = ExitStack()
            inner1b = S.es_phase
            inner1b.__enter__()
            wst = [S.sb("wst%d" % i, [128, 16, 256], F32) for i in range(2)]
            wbb = [S.sb("wbb%d" % i, [128, 16, 256], BF16) for i in range(2)]
            pp = [S.ps("pp%d" % i, [128, 512], F32) for i in range(4)]
            ptq = S.ps("ptq", [128, 1024], BF16)
            csq = [S.sb("csq%d" % i, [128, 128], F32) for i in range(2)]
            qf1 = [S.sb("qf1_%d" % i, [128, 256], F32) for i in range(2)]
            qf2 = [S.sb("qf2_%d" % i, [128, 256], F32) for i in range(2)]
            q16 = [S.sb("q16_%d" % i, [128, 256], BF16) for i in range(2)]
            stgA = [S.sb("stgA%d" % i, [128, 2, NLOC], BF16) for i in range(2)]
            v16 = [S.sb("v16_%d" % i, [128, 256], BF16) for i in range(3)]
            blocks = []
            for b in range(4):
                blocks.append(('qa', b * 256, b))
            for b in range(4):
                blocks.append(('qb', 3072 + b * 256, b))
            for b in range(4):
                blocks.append(('kb', 4096 + b * 256, b))
            for b in range(4):
                blocks.append(('vb', 5120 + b * 256, b))
            for b in range(16):
                blocks.append(('g', 6144 + b * 256, b))
            kctr = 0
            def ldw(bi):
                if bi >= len(blocks):
                    return
                c0_ = blocks[bi][1]
                ws, wb = wst[bi % 2], wbb[bi % 2]
                S.dma(ws[:, :, :], w_in.t[:, c0_:c0_ + 256].rearrange("(j p) n -> p j n", p=128), ws, reads=[w_in], writes=[ws])
                S.op('dve', lambda e: e.tensor_copy(out=wb[:, :, :], in_=ws[:, :, :]), reads=[ws], writes=[wb])
            ldw(0)
            for bi, (kind, c0, b) in enumerate(blocks):
                ws, wb = wst[bi % 2], wbb[bi % 2]
                ldw(bi + 1)
                sA = stgA[bi % 2]
                if kind == 'qa':
                    for t in range(16):
                        p = pp[t % 4]
                        lt = (t + 2) * 128

                        def mmq(e, p=p, wb=wb, lt=lt):
                            for j in range(16):
                                ins = e.matmul(p[:, 0:256], lhsT=hL[:, j, lt:lt + 128], rhs=wb[:, j, :], start=(j == 0), stop=(j == 15))
                            return ins
                        S.op('pe', mmq, reads=[hL, wb], writes=[p])
                        c_ = csq[t % 2]
                        S.dma(c_[:, :], rope_own[t * 128:(t + 1) * 128, :], c_, reads=[rope_own], writes=[c_])
                        t1, t2, qq = qf1[t % 2], qf2[t % 2], q16[t % 2]
                        S.op('dve', lambda e, p=p, t1=t1, c_=c_: e.tensor_tensor(
                            out=t1.t[:, :].rearrange("p (g d) -> p g d", g=4), in0=p.t[:, 0:256].rearrange("p (g d) -> p g d", g=4),
                            in1=bc(c_[:, 0:64], 1, [128, 4, 64]), op=ALU.mult), reads=[p, c_], writes=[t1])
                        for ab in range(2):
                            S.op('dve', lambda e, p=p, t2=t2, c_=c_, ab=ab: e.tensor_tensor(
                                out=t2.t[:, :].rearrange("p (g r a d) -> p g r a d", g=4, r=2, a=2)[:, :, :, ab, :],
                                in0=p.t[:, 0:256].rearrange("p (g r a d) -> p g r a d", g=4, r=2, a=2)[:, :, :, 1 - ab, :],
                                in1=bc(c_.t[:, 64:128].rearrange("p (r a d) -> p r a d", r=2, a=2)[:, :, ab, :], 1, [128, 4, 2, 16]),
                                op=ALU.mult), reads=[p, c_], writes=[t2])
                        S.op('pool', lambda e, t1=t1, t2=t2, qq=qq: e.tensor_tensor(out=qq[:, :], in0=t1[:, :], in1=t2[:, :], op=ALU.add),
                             reads=[t1, t2], writes=[qq])

                        def trq(e, qq=qq):
                            for hh in range(2):
                                ins = e.transpose(out=ptq[:, hh * 128:(hh + 1) * 128], in_=qq[:, hh * 128:(hh + 1) * 128], identity=ident[:, :])
                            return ins
                        S.op('pe', trq, reads=[qq, ident], writes=[ptq])
                        S.op('act', lambda e, sA=sA, t=t: e.copy(out=sA[:, :, t * 128:(t + 1) * 128],
                                                                 in_=ptq.t[:, 0:256].rearrange("p (h t) -> p h t", h=2)),
                             reads=[ptq], writes=[sA])
                    S.dma(QaT_d.t[2 * b:2 * b + 2, :, :].rearrange("h p t -> p h t"), sA[:, :, 0:OWN], sA, reads=[sA], writes=[QaT_d])
                elif kind in ('qb', 'kb', 'g'):
                    if kind == 'kb':
                        groups = [(g * 512, 512) for g in range(5)] + [(2560, 256)]
                    else:
                        groups = [(256 + g * 512, 512) for g in range(4)]
                    for cc in range(2):
                        for gi, (l0, n) in enumerate(groups):
                            p = pp[kctr % 4]
                            kctr += 1

                            def mmf(e, p=p, wb=wb, cc=cc, l0=l0, n=n):
                                for j in range(16):
                                    ins = e.matmul(p[:, 0:n], lhsT=wb[:, j, cc * 128:(cc + 1) * 128], rhs=hL[:, j, l0:l0 + n], start=(j == 0), stop=(j == 15))
                                return ins
                            S.op('pe', mmf, reads=[hL, wb], writes=[p])
                            o0 = l0 if kind == 'kb' else l0 - 256
                            if kind == 'g':
                                S.op('act', lambda e, p=p, sA=sA, cc=cc, o0=o0, n=n: e.activation(out=sA[:, cc, o0:o0 + n], in_=p[:, 0:n], func=AF.Sigmoid),
                                     reads=[p], writes=[sA])
                            elif kind == 'qb':
                                S.op('act', lambda e, p=p, sA=sA, cc=cc, o0=o0, n=n: e.activation(out=sA[:, cc, o0:o0 + n], in_=p[:, 0:n], func=AF.Copy, scale=128.0 ** -0.5),
                                     reads=[p], writes=[sA])
                            else:
                                S.op('act', lambda e, p=p, sA=sA, cc=cc, o0=o0, n=n: e.copy(out=sA[:, cc, o0:o0 + n], in_=p[:, 0:n]),
                                     reads=[p], writes=[sA])
                    if kind == 'qb':
                        S.dma(QbT_d.t[2 * b:2 * b + 2, :, :].rearrange("h p t -> p h t"), sA[:, :, 0:OWN], sA, reads=[sA], writes=[QbT_d])
                    elif kind == 'kb':
                        S.dma(KbT_d.t[2 * b:2 * b + 2, :, :].rearrange("h p t -> p h t"), sA[:, :, 0:NLOC], sA, reads=[sA], writes=[KbT_d])
                    else:
                        S.dma(GT_d.t[2 * b:2 * b + 2, :, :].rearrange("h p t -> p h t"), sA[:, :, 0:OWN], sA, reads=[sA], writes=[GT_d])
                else:
                    for t in range(22):
                        p = pp[t % 4]

                        def mmv(e, p=p, wb=wb, t=t):
                            for j in range(16):
                                ins = e.matmul(p[:, 0:256], lhsT=hL[:, j, t * 128:(t + 1) * 128], rhs=wb[:, j, :], start=(j == 0), stop=(j == 15))
                            return ins
                        S.op('pe', mmv, reads=[hL, wb], writes=[p])
                        vv = v16[t % 3]
                        S.op('act', lambda e, p=p, vv=vv: e.copy(out=vv[:, :], in_=p[:, 0:256]), reads=[p], writes=[vv])
                        S.dma(Vb_d[t * 128:(t + 1) * 128, b * 256:(b + 1) * 256], vv[:, :], vv, reads=[vv], writes=[Vb_d])
            S.end_phase()
            inner1b.__exit__(None, None, None)

        with phase_begin():
            kT = [S.sb("kT%d" % i, [128, NKEY], BF16) for i in range(2)]
            vA = [S.sb("vA%d" % i, [128, 130, 129], BF16) for i in range(2)]
            qA = [S.sb("qA%d" % i, [128, OWN], BF16) for i in range(2)]
            for i in range(2):
                S.op('pool', lambda e, i=i: e.memset(vA[i][:, :, 128:129], 1.0), writes=[vA[i]])
            pS = [S.ps("pS%d" % i, [128, 2, 512], F32) for i in range(2)]
            pO = [S.ps("pO%d" % i, [128, 512], F32) for i in range(3)]
            pT_ = [S.sb("pT%d" % i, [128, 2, 512], BF16) for i in range(3)]
            ptA = S.ps("ptA", [128, 1024], BF16)
            o0 = S.sb("o0", [128, 128], F32)
            accS = [S.sb("accS%d" % i, [128, 387], F32) for i in range(3)]
            dd = [S.sb("dd%d" % i, [128, 128], F32) for i in range(2)]
            rz = [S.sb("rz%d" % i, [128, 2], F32) for i in range(2)]
            jk = S.sb("jk", [128, 128], F32)
            s2 = [S.sb("s2_%d" % i, [128, 1], F32) for i in range(2)]
            sd = [S.sb("sd_%d" % i, [128, 1], F32) for i in range(2)]
            rs = [S.sb("rs_%d" % i, [128, 1], F32) for i in range(2)]
            oa16 = [S.sb("oa16_%d" % i, [128, 128], BF16) for i in range(2)]
            oaS = [S.sb("oaS%d" % i, [128, OWN], BF16) for i in range(2)]
            NKC = NKEY // 128

            def acc(mi, qt):
                a_ = mi * 4 + qt
                return pO[a_ // 3], a_ % 3
            uf = S.sb("uf", [128, D], F32)
            ub = S.sb("ub", [128, D], BF16)
            us = S.sb("us", [128, D], BF16)
            vf = S.sb("vf", [128, D], F32)
            vb2 = S.sb("vb2", [128, D], BF16)

            def puv_gen():
                for c in range(128):
                    S.dma(uf[:, :], peer_u[c * 128:(c + 1) * 128, :], uf, reads=[peer_u], writes=[uf])
                    S.dma(vf[:, :], peer_v[c * 128:(c + 1) * 128, :], vf, reads=[peer_v], writes=[vf])
                    yield
                    yield
                    S.op('dve', lambda e: e.tensor_copy(out=ub[:, :], in_=uf[:, :]), reads=[uf], writes=[ub])
                    S.op('dve', lambda e: e.tensor_copy(out=vb2[:, :], in_=vf[:, :]), reads=[vf], writes=[vb2])
                    yield
                    yield
                    for half in range(2):
                        def tru(e, half=half):
                            for jj in range(8):
                                j = half * 8 + jj
                                ins = e.transpose(out=ptA[:, jj * 128:(jj + 1) * 128], in_=ub[:, j * 128:(j + 1) * 128], identity=ident[:, :])
                            return ins
                        S.op('pe', tru, reads=[ub, ident], writes=[ptA])
                        yield
                        S.op('dve', lambda e, half=half: e.tensor_copy(out=us[:, half * 1024:(half + 1) * 1024], in_=ptA[:, :]),
                             reads=[ptA], writes=[us])
                        yield
                    S.dma(UT_d[c, :, :], us[:, :], us, reads=[us], writes=[UT_d])
                    S.dma(VB_d[c * 128:(c + 1) * 128, :], vb2[:, :], vb2, reads=[vb2], writes=[VB_d])
            puv = puv_gen()

            def head_loads(h):
                if h >= 8:
                    return
                k_, v_, q_ = kT[h % 2], vA[h % 2], qA[h % 2]
                S.dma(k_[:, :], KaT_d[h, :, :], k_, reads=[KaT_d], writes=[k_])
                for part in range(2):
                    c0, c1 = part * 65, (part + 1) * 65
                    S.dma(v_[:, c0:c1, 0:128], Va_d.t[c0 * 128:c1 * 128, h * 128:(h + 1) * 128].rearrange("(c p) e -> p c e", p=128),
                          v_, reads=[Va_d], writes=[v_])
                S.dma(q_[:, :], QaT_d[h, :, :], q_, reads=[QaT_d], writes=[q_])
            ctr = 0
            head_loads(0)
            for h in range(8):
                k_, v_, q_ = kT[h % 2], vA[h % 2], qA[h % 2]
                head_loads(h + 1)
                oS = oaS[h % 2]
                for qg in range(4):
                    def s_op(kc, k_=k_, q_=q_, qg=qg):
                        p = pS[kc % 2]

                        def f(e):
                            for mi in range(2):
                                ins = e.matmul(p[:, mi, :], lhsT=k_[64 * mi:64 * mi + 64, kc * 128:(kc + 1) * 128],
                                               rhs=q_[64 * mi:64 * mi + 64, qg * 512:(qg + 1) * 512], start=True, stop=True)
                            return ins
                        S.op('pe', f, reads=[k_, q_], writes=[p])
                    def pv_op(kc, v_=v_):
                        pt = pT_[kc % 3]

                        def pv(e):
                            for mi in range(2):
                                for qt in range(4):
                                    ab, ai = acc(mi, qt)
                                    ins = e.matmul(ab[:, ai * 129:ai * 129 + 129], lhsT=pt[:, mi, qt * 128:(qt + 1) * 128], rhs=v_[:, kc, :],
                                                   start=(kc == 0), stop=(kc == NKC - 1))
                            return ins
                        S.op('pe', pv, reads=[pt, v_], writes=pO)
                    s_op(0)
                    for kc in range(NKC):
                        p = pS[kc % 2]
                        pt = pT_[kc % 3]
                        S.op('act', lambda e, p=p, pt=pt: e.activation(out=pt[:, :, :], in_=p[:, :, :], func=AF.Exp), reads=[p], writes=[pt])
                        if kc + 1 < NKC:
                            s_op(kc + 1)
                        if kc >= 1:
                            pv_op(kc - 1)
                        if kc % 4 == 3:
                            next(puv, None)
                    pv_op(NKC - 1)
                    for b_ in range(3):
                        S.op('dve', lambda e, b_=b_: e.tensor_copy(out=accS[b_][:, :], in_=pO[b_][:, 0:387]), reads=[pO[b_]], writes=[accS[b_]])
                    for qt in range(4):
                        ctr += 1
                        r_, d_ = rz[ctr % 2], dd[ctr % 2]
                        a0, i0 = accS[(0 * 4 + qt) // 3], (0 * 4 + qt) % 3
                        a1, i1 = accS[(1 * 4 + qt) // 3], (1 * 4 + qt) % 3
                        S.op('dve', lambda e, a0=a0, i0=i0, r_=r_: e.reciprocal(out=r_[:, 0:1], in_=a0[:, i0 * 129 + 128:i0 * 129 + 129]), reads=[a0], writes=[r_])
                        S.op('dve', lambda e, a1=a1, i1=i1, r_=r_: e.reciprocal(out=r_[:, 1:2], in_=a1[:, i1 * 129 + 128:i1 * 129 + 129]), reads=[a1, r_], writes=[r_])
                        S.op('dve', lambda e, a0=a0, i0=i0, r_=r_: e.tensor_scalar(out=o0[:, :], in0=a0[:, i0 * 129:i0 * 129 + 128], scalar1=r_[:, 0:1], scalar2=None, op0=ALU.mult),
                             reads=[a0, r_], writes=[o0])
                        S.op('dve', lambda e, a1=a1, i1=i1, r_=r_, d_=d_: e.tensor_scalar(out=d_[:, :], in0=a1[:, i1 * 129:i1 * 129 + 128], scalar1=r_[:, 1:2], scalar2=lamv[:, 1:2], op0=ALU.mult, op1=ALU.mult),
                             reads=[a1, r_, lamv], writes=[d_])
                        S.op('dve', lambda e, d_=d_: e.tensor_tensor(out=d_[:, :], in0=d_[:, :], in1=o0[:, :], op=ALU.add),
                             reads=[d_, o0], writes=[d_])
                        a_, b_, c_ = s2[ctr % 2], sd[ctr % 2], rs[ctr % 2]
                        S.op('act', lambda e, d_=d_, a_=a_: e.activation(out=jk[:, :], in_=d_[:, :], func=AF.Square, accum_out=a_[:, :]),
                             reads=[d_], writes=[jk, a_])
                        S.op('act', lambda e, a_=a_, b_=b_: e.activation(out=b_[:, :], in_=a_[:, :], func=AF.Sqrt, scale=1.0 / 128, bias=EPS),
                             reads=[a_], writes=[b_])
                        S.op('dve', lambda e, b_=b_, c_=c_: e.reciprocal(out=c_[:, :], in_=b_[:, :]), reads=[b_], writes=[c_])
                        o16 = oa16[ctr % 2]
                        S.op('dve', lambda e, d_=d_, c_=c_, o16=o16: e.scalar_tensor_tensor(out=o16[:, :], in0=d_[:, :], scalar=c_[:, 0:1], in1=gsub[:, :], op0=ALU.mult, op1=ALU.mult),
                             reads=[d_, c_, gsub], writes=[o16])
                        S.op('pe', lambda e, o16=o16: e.transpose(out=ptA[:, 0:128], in_=o16[:, :], identity=ident[:, :]), reads=[o16, ident], writes=[ptA])
                        tt = qg * 4 + qt
                        S.op('act', lambda e, oS=oS, tt=tt: e.copy(out=oS[:, tt * 128:(tt + 1) * 128], in_=ptA[:, 0:128]), reads=[ptA], writes=[oS])
                S.dma(OaT_d[h, :, :], oS[:, :], oS, reads=[oS], writes=[OaT_d])
            for _ in puv:
                pass
            S.end_phase()

        with phase_begin():
            kB = [S.sb("kB%d" % i, [128, NLOC], BF16) for i in range(2)]
            vB = [S.sb("vB%d" % i, [128, 22, 129], BF16) for i in range(2)]
            qB = [S.sb("qB%d" % i, [128, OWN], BF16) for i in range(2)]
            bia = [S.sb("bia%d" % i, [128, 5, 5, 128], F32) for i in range(2)]
            for i in range(2):
                S.op('pool', lambda e, i=i: e.memset(vB[i][:, :, 128:129], 1.0), writes=[vB[i]])
            pN = [S.ps("pN%d" % i, [128, 8, 128], F32) for i in range(2)]
            pNo = [S.ps("pNo%d" % i, [128, 512], F32) for i in range(2)]
            ptB = S.ps("ptB", [128, 1024], BF16)
            sN = [S.sb("sN%d" % i, [128, 5, 128], F32) for i in range(2)]
            pTn = [S.sb("pTn%d" % i, [128, 7, 128], BF16) for i in range(2)]
            rzb = [S.sb("rzb%d" % i, [128, 1], F32) for i in range(2)]
            ob16 = [S.sb("ob16_%d" % i, [128, 128], BF16) for i in range(2)]
            obS = [S.sb("obS%d" % i, [128, OWN], BF16) for i in range(2)]
            def na_loads(h):
                if h >= 8:
                    return
                k_, v_, q_, bi_ = kB[h % 2], vB[h % 2], qB[h % 2], bia[h % 2]
                S.dma(k_[:, :], KbT_d[h, :, :], k_, reads=[KbT_d], writes=[k_])
                S.dma(v_[:, :, 0:128], Vb_d.t[:, h * 128:(h + 1) * 128].rearrange("(c p) e -> p c e", p=128), v_, reads=[Vb_d], writes=[v_])
                S.dma(q_[:, :], QbT_d[h, :, :], q_, reads=[QbT_d], writes=[q_])
                S.dma(bi_.t[:, :, :, :].rearrange("p a b q -> p (a b q)"), na_bias[h, :, :], bi_, reads=[na_bias], writes=[bi_])
            na_loads(0)
            for h in range(8):
                k_, v_, q_, bi_ = kB[h % 2], vB[h % 2], qB[h % 2], bia[h % 2]
                na_loads(h + 1)
                oS = obS[h % 2]

                def s_stage(j, k_=k_, q_=q_):
                    p = pN[j % 2]

                    def mms(e):
                        for c in range(7):
                            k0 = 128 * j + 128 * c if c < 5 else 2560 + 128 * (c - 5)
                            ins = e.matmul(p[:, c, :], lhsT=k_[:, k0:k0 + 128], rhs=q_[:, j * 128:(j + 1) * 128], start=True, stop=True)
                        return ins
                    S.op('pe', mms, reads=[k_, q_], writes=[p])

                def mid_stage(j, bi_=bi_):
                    slot = 0 if j == 0 else 1 if j == 1 else 3 if j == 14 else 4 if j == 15 else 2
                    p, s_, pt = pN[j % 2], sN[j % 2], pTn[j % 2]
                    S.op('dve', lambda e: e.tensor_tensor(out=s_[:, :, :], in0=p[:, 0:5, :], in1=bi_[:, slot, :, :], op=ALU.add),
                         reads=[p, bi_], writes=[s_])
                    S.op('act', lambda e: e.activation(out=pt[:, 0:5, :], in_=s_[:, :, :], func=AF.Exp), reads=[s_], writes=[pt])
                    S.op('act', lambda e: e.activation(out=pt[:, 5:7, :], in_=p[:, 5:7, :], func=AF.Exp), reads=[p], writes=[pt])

                def o_stage(j, v_=v_):
                    pt, po = pTn[j % 2], pNo[j % 2]

                    def mmo(e):
                        for c in range(7):
                            tile = j + c if c < 5 else 20 + (c - 5)
                            ins = e.matmul(po[:, 0:129], lhsT=pt[:, c, :], rhs=v_[:, tile, :], start=(c == 0), stop=(c == 6))
                        return ins
                    S.op('pe', mmo, reads=[pt, v_], writes=[po])
                    r_, o16 = rzb[j % 2], ob16[j % 2]
                    S.op('dve', lambda e: e.reciprocal(out=r_[:, :], in_=po[:, 128:129]), reads=[po], writes=[r_])
                    S.op('dve', lambda e: e.tensor_scalar(out=o16[:, :], in0=po[:, 0:128], scalar1=r_[:, 0:1], scalar2=None, op0=ALU.mult),
                         reads=[po, r_], writes=[o16])

                def t_stage(j, oS=oS):
                    o16 = ob16[j % 2]
                    S.op('pe', lambda e: e.transpose(out=ptB[:, 0:128], in_=o16[:, :], identity=ident[:, :]), reads=[o16, ident], writes=[ptB])
                    S.op('act', lambda e: e.copy(out=oS[:, j * 128:(j + 1) * 128], in_=ptB[:, 0:128]), reads=[ptB], writes=[oS])
                s_stage(0)
                for j in range(16):
                    if j + 1 < 16:
                        s_stage(j + 1)
                    mid_stage(j)
                    o_stage(j)
                    if j >= 1:
                        t_stage(j - 1)
                t_stage(15)
                S.dma(ObT_d[h, :, :], oS[:, :], oS, reads=[oS], writes=[ObT_d])
            S.end_phase()

        def load_w_bf16(dst, src, nrow_chunks, stgs, ncol=2048, cw=256):
            k = 0
            for c0 in range(0, ncol, cw):
                st = stgs[k % 2]
                S.dma(st[:, 0:nrow_chunks, :], src.t[:, c0:c0 + cw].rearrange("(j p) n -> p j n", p=128), st, reads=[src], writes=[st])
                (S.op('act', lambda e, st=st, c0=c0: e.copy(out=dst[:, :, c0:c0 + cw], in_=st[:, 0:nrow_chunks, :]), reads=[st], writes=[dst]) if k % 2 else
                 S.op('dve', lambda e, st=st, c0=c0: e.tensor_copy(out=dst[:, :, c0:c0 + cw], in_=st[:, 0:nrow_chunks, :]), reads=[st], writes=[dst]))
                k += 1

        with phase_begin():
            wa = S.sb("wa", [128, 8, 2048], BF16)
            wb_ = S.sb("wb_", [128, 8, 2048], BF16)
            stg4 = [S.sb("stg4_%d" % i, [128, 16, 256], F32) for i in range(2)]
            load_w_bf16(wa, w_a, 8, stg4)
            load_w_bf16(wb_, w_b, 8, stg4)
            oa = [S.sb("oa%d" % i, [128, 8, 512], BF16) for i in range(2)]
            ob = [S.sb("ob%d" % i, [128, 8, 512], BF16) for i in range(2)]
            ga = [S.sb("ga%d" % i, [128, 512], BF16) for i in range(2)]
            gb = [S.sb("gb%d" % i, [128, 512], BF16) for i in range(2)]
            pa = [S.ps("pa%d" % i, [128, 512], F32) for i in range(2)]
            pb = [S.ps("pb%d" % i, [128, 512], F32) for i in range(2)]
            t1 = [S.sb("m1_%d" % i, [128, 512], F32) for i in range(2)]
            t2 = [S.sb("m2_%d" % i, [128, 512], F32) for i in range(2)]
            mS = [S.sb("mS%d" % i, [128, 16, 512], BF16) for i in range(2)]
            k = 0
            for tg in range(4):
                oa_, ob_, ms = oa[tg % 2], ob[tg % 2], mS[tg % 2]
                S.dma(oa_[:, :, :], OaT_d.t[:, :, tg * 512:(tg + 1) * 512].rearrange("h p t -> p h t"), oa_, reads=[OaT_d], writes=[oa_])
                S.dma(ob_[:, :, :], ObT_d.t[:, :, tg * 512:(tg + 1) * 512].rearrange("h p t -> p h t"), ob_, reads=[ObT_d], writes=[ob_])
                for fc in range(16):
                    k += 1
                    ga_, gb_, pa_, pb_, t1_, t2_ = ga[k % 2], gb[k % 2], pa[k % 2], pb[k % 2], t1[k % 2], t2[k % 2]
                    S.dma(ga_[:, :], GT_d[fc, :, tg * 512:(tg + 1) * 512], ga_, reads=[GT_d], writes=[ga_])
                    S.dma(gb_[:, :], GT_d[16 + fc, :, tg * 512:(tg + 1) * 512], gb_, reads=[GT_d], writes=[gb_])

                    def mma(e, pa_=pa_, oa_=oa_, fc=fc):
                        for hh in range(8):
                            ins = e.matmul(pa_[:, :], lhsT=wa[:, hh, fc * 128:(fc + 1) * 128], rhs=oa_[:, hh, :], start=(hh == 0), stop=(hh == 7))
                        return ins

                    def mmb(e, pb_=pb_, ob_=ob_, fc=fc):
                        for hh in range(8):
                            ins = e.matmul(pb_[:, :], lhsT=wb_[:, hh, fc * 128:(fc + 1) * 128], rhs=ob_[:, hh, :], start=(hh == 0), stop=(hh == 7))
                        return ins
                    S.op('pe', mma, reads=[wa, oa_], writes=[pa_])
                    S.op('pe', mmb, reads=[wb_, ob_], writes=[pb_])
                    S.op('dve', lambda e, pa_=pa_, ga_=ga_, t1_=t1_: e.tensor_tensor(out=t1_[:, :], in0=pa_[:, :], in1=ga_[:, :], op=ALU.mult), reads=[pa_, ga_], writes=[t1_])
                    S.op('dve', lambda e, pb_=pb_, gb_=gb_, t2_=t2_: e.tensor_tensor(out=t2_[:, :], in0=pb_[:, :], in1=gb_[:, :], op=ALU.mult), reads=[pb_, gb_], writes=[t2_])
                    S.op('pool', lambda e, t1_=t1_, t2_=t2_, ms=ms, fc=fc: e.tensor_tensor(out=ms[:, fc, :], in0=t1_[:, :], in1=t2_[:, :], op=ALU.add), reads=[t1_, t2_], writes=[ms])
                S.dma(mT_d[:, :, tg * 512:(tg + 1) * 512], ms[:, :, :], ms, reads=[ms], writes=[mT_d])
            S.end_phase()

        with phase_begin():
            wo = S.sb("wo", [128, 16, 2048], BF16)
            stg5 = [S.sb("stg5_%d" % i, [128, 16, 256], F32) for i in range(2)]
            load_w_bf16(wo, w_o, 16, stg5)
            gt1 = S.sb("gt1", [128, D], F32)
            S.dma(gt1[:, :], mrow.t[0, 2 * D:3 * D].partition_broadcast(128), gt1, reads=[mrow], writes=[gt1])
            mt = [S.sb("mt%d" % i, [128, 16, 512], BF16) for i in range(2)]
            xo = [S.sb("xo%d" % i, [128, D], F32) for i in range(2)]
            x1 = [S.sb("x1_%d" % i, [128, D], F32) for i in range(2)]
            py = [S.ps("py%d" % i, [128, 512], F32) for i in range(4)]
            for tg in range(4):
                m_ = mt[tg % 2]
                S.dma(m_[:, :, :], mT_d[:, :, tg * 512:(tg + 1) * 512], m_, reads=[mT_d], writes=[m_])
                for tt in range(4):
                    t = tg * 4 + tt
                    xo_, x1_ = xo[t % 2], x1[t % 2]
                    S.dma(xo_[:, :], x_own[t * 128:(t + 1) * 128, :], xo_, reads=[x_own], writes=[xo_])
                    for cb in range(4):
                        def mmy(e, m_=m_, tt=tt, cb=cb):
                            for j in range(16):
                                ins = e.matmul(py[cb][:, :], lhsT=m_[:, j, tt * 128:(tt + 1) * 128], rhs=wo[:, j, cb * 512:(cb + 1) * 512], start=(j == 0), stop=(j == 15))
                            return ins
                        S.op('pe', mmy, reads=[m_, wo], writes=[py[cb]])
                        S.op('dve', lambda e, cb=cb, x1_=x1_: e.tensor_tensor(out=x1_[:, cb * 512:(cb + 1) * 512], in0=py[cb][:, :], in1=gt1[:, cb * 512:(cb + 1) * 512], op=ALU.mult),
                             reads=[py[cb], gt1], writes=[x1_])
                    S.op('pool', lambda e, x1_=x1_, xo_=xo_: e.tensor_tensor(out=x1_[:, :], in0=x1_[:, :], in1=xo_[:, :], op=ALU.add), reads=[x1_, xo_], writes=[x1_])
                    S.dma(X1_d[t * 128:(t + 1) * 128, :], x1_[:, :], x1_, reads=[x1_], writes=[X1_d])
            S.end_phase()

        rt1 = S.sb("rt1", [128, 16, 128], F32, True)
        rt2 = S.sb("rt2", [128, 16, 128], F32, True)
        rtg = S.sb("rtg", [128, 16, 128], F32, True)
        hn_d = dscr("hn_d", [128, 16, OWN], BF16)
        QT_d = dscr("QT_d", [16, 128, OWN], BF16)
        with phase_begin():
            R = make_norm(2)
            hst = [S.sb("hst%d" % i, [128, 16, 512], BF16) for i in range(2)]
            for t in range(16):
                hs = hst[(t // 4) % 2]
                norm_tile(R, X1_d[t * 128:(t + 1) * 128, :], X1_d, 4, 5, hs[:, :, (t % 4) * 128:(t % 4 + 1) * 128], hs)
                if t % 4 == 3:
                    tg = t // 4
                    S.dma(hn_d[:, :, tg * 512:(tg + 1) * 512], hs[:, :, :], hs, reads=[hs], writes=[hn_d])
            S.end_phase()
        with phase_begin():
            hnA = S.sb("hnA", [128, 16, OWN], BF16)
            for tg in range(4):
                S.dma(hnA[:, :, tg * 512:(tg + 1) * 512], hn_d[:, :, tg * 512:(tg + 1) * 512], hnA, reads=[hn_d], writes=[hnA])
            stg6 = [S.sb("stg6_%d" % i, [128, 16, 128], F32) for i in range(2)]
            wqb = [S.sb("wqb%d" % i, [128, 16, 128], BF16) for i in range(2)]
            qst = [S.sb("qst%d" % i, [128, OWN], BF16) for i in range(2)]
            pq = [S.ps("pq%d" % i, [128, 512], F32) for i in range(2)]
            for cq in range(16):
                st, wb = stg6[cq % 2], wqb[cq % 2]
                S.dma(st[:, :, :], w_q.t[:, cq * 128:(cq + 1) * 128].rearrange("(j p) n -> p j n", p=128), st, reads=[w_q], writes=[st])
                S.op('dve', lambda e, st=st, wb=wb: e.tensor_copy(out=wb[:, :, :], in_=st[:, :, :]), reads=[st], writes=[wb])
                qs = qst[cq % 2]
                for tg in range(4):
                    p = pq[tg % 2]

                    def mmq2(e, p=p, wb=wb, tg=tg):
                        for j in range(16):
                            ins = e.matmul(p[:, :], lhsT=wb[:, j, :], rhs=hnA[:, j, tg * 512:(tg + 1) * 512], start=(j == 0), stop=(j == 15))
                        return ins
                    S.op('pe', mmq2, reads=[wb, hnA], writes=[p])
                    S.op('act', lambda e, p=p, qs=qs, tg=tg: e.copy(out=qs[:, tg * 512:(tg + 1) * 512], in_=p[:, :]), reads=[p], writes=[qs])
                S.dma(QT_d[cq, :, :], qs[:, :], qs, reads=[qs], writes=[QT_d])
            S.end_phase()
        with phase_begin():
            sbf = S.sb("sbf", [128, 2048], F32)
            sbk = S.sb("sbk", [128, 16, 128], BF16)
            S.dma(sbf[:, :], subT[:, :], sbf, reads=[subT], writes=[sbf])
            S.op('dve', lambda e: e.tensor_copy(out=sbk.t[:, :, :].rearrange("p a b -> p (a b)"), in_=sbf[:, :]), reads=[sbf], writes=[sbk])
            qT = [S.sb("qT%d" % i, [128, 16, 512], BF16) for i in range(2)]
            psc = [S.ps("psc%d" % i, [128, 4, 128], F32) for i in range(4)]
            ptr_ = S.ps("ptr_", [128, 3, 128], F32)
            s_sb = S.sb("s_sb", [128, 16, 128], F32)
            wk = S.sb("wk", [128, 16, 128], F32)
            top = S.sb("top", [128, 16, 16], F32)
            idx = S.sb("idx", [128, 16, 16], U32)
            idf = S.sb("idf", [128, 16, 16], F32)
            cand = S.sb("cand", [128, 8, 256], F32)
            cw = S.sb("cw", [128, 8, 256], F32)
            best = S.sb("best", [128, 8, 16], F32)
            pos = S.sb("pos", [128, 8, 16], U32)
            pa_u = S.sb("pa_u", [128, 8, 16], U32)
            pb_u = S.sb("pb_u", [128, 8, 16], U32)
            paf = S.sb("paf", [128, 8, 16], F32)
            pbf = S.sb("pbf", [128, 8, 16], F32)
            nb = S.sb("nb", [128, 8], F32)
            ex = S.sb("ex", [128, 8, 16], F32)
            zz = S.sb("zz", [128, 8], F32)
            rzz = S.sb("rzz", [128, 8], F32)
            gg = S.sb("gg", [128, 8, 16], F32)
            oh = S.sb("oh", [128, 8, 16, 16], F32)
            i1f = S.sb("i1f", [128, 8, 16], F32)
            i2f = S.sb("i2f", [128, 8, 16], F32)

            def views(b_, n):
                return [Buf("%s_v%d" % (b_.name, i), b_.t) for i in range(n)]
            s_v = views(s_sb, 4)
            top_v, idx_v, wk_v = views(top, 16), views(idx, 16), views(wk, 16)
            cand_v, best_v, pos_v, cw_v = views(cand, 8), views(best, 8), views(pos, 8), views(cw, 8)
            ex_v, zz_v = views(ex, 8), views(zz, 8)
            for tg in range(4):
                q_ = qT[tg % 2]
                S.dma(q_[:, :, :], QT_d.t[:, :, tg * 512:(tg + 1) * 512].rearrange("c p t -> p c t"), q_, reads=[QT_d], writes=[q_])
                for tt in range(4):
                    t = tg * 4 + tt
                    for g4 in range(4):
                        def mms2(e, q_=q_, tt=tt, g4=g4):
                            for c in range(4):
                                cq = g4 * 4 + c
                                ins = e.matmul(psc[g4][:, c, :], lhsT=q_[:, cq, tt * 128:(tt + 1) * 128], rhs=sbk[:, cq, :], start=True, stop=True)
                            return ins
                        S.op('pe', mms2, reads=[q_, sbk], writes=[psc[g4]])
                        S.op('act', lambda e, g4=g4: e.copy(out=s_sb[:, g4 * 4:(g4 + 1) * 4, :], in_=psc[g4][:, :, :]), reads=[psc[g4]], writes=[s_v[g4]])
                    for cq in range(16):
                        S.op('dve', lambda e, cq=cq: e.max(out=top[:, cq, 0:8], in_=s_sb[:, cq, :]), reads=[s_v[cq // 4]], writes=[top_v[cq]])
                    for cq in range(16):
                        S.op('dve', lambda e, cq=cq: e.max_index(out=idx[:, cq, 0:8], in_max=top[:, cq, 0:8], in_values=s_sb[:, cq, :]), reads=[s_v[cq // 4], top_v[cq]], writes=[idx_v[cq]])
                    for cq in range(16):
                        S.op('dve', lambda e, cq=cq: e.match_replace(out=wk[:, cq, :], in_to_replace=top[:, cq, 0:8], in_values=s_sb[:, cq, :], imm_value=-1e30), reads=[s_v[cq // 4], top_v[cq]], writes=[wk_v[cq]])
                    for cq in range(16):
                        S.op('dve', lambda e, cq=cq: e.max(out=top[:, cq, 8:16], in_=wk[:, cq, :]), reads=[wk_v[cq]], writes=[top_v[cq]])
                    for cq in range(16):
                        S.op('dve', lambda e, cq=cq: e.max_index(out=idx[:, cq, 8:16], in_max=top[:, cq, 8:16], in_values=wk[:, cq, :]), reads=[wk_v[cq], top_v[cq]], writes=[idx_v[cq]])
                    tv = lambda b_: b_.t[:, :, :].rearrange("p (h two) k -> p h two k", two=2)
                    S.op('dve', lambda e: e.tensor_tensor(out=cand.t[:, :, :].rearrange("p h (a b) -> p h a b", a=16),
                                                          in0=bc(tv(top)[:, :, 0, :], 3, [128, 8, 16, 16]),
                                                          in1=bc(tv(top)[:, :, 1, :], 2, [128, 8, 16, 16]), op=ALU.add), reads=top_v, writes=cand_v)
                    for hh in range(8):
                        S.op('dve', lambda e, hh=hh: e.max(out=best[:, hh, 0:8], in_=cand[:, hh, :]), reads=[cand_v[hh]], writes=[best_v[hh]])
                    for hh in range(8):
                        S.op('dve', lambda e, hh=hh: e.max_index(out=pos[:, hh, 0:8], in_max=best[:, hh, 0:8], in_values=cand[:, hh, :]), reads=[cand_v[hh], best_v[hh]], writes=[pos_v[hh]])
                    for hh in range(8):
                        S.op('dve', lambda e, hh=hh: e.match_replace(out=cw[:, hh, :], in_to_replace=best[:, hh, 0:8], in_values=cand[:, hh, :], imm_value=-1e30), reads=[cand_v[hh], best_v[hh]], writes=[cw_v[hh]])
                    for hh in range(8):
                        S.op('dve', lambda e, hh=hh: e.max(out=best[:, hh, 8:16], in_=cw[:, hh, :]), reads=[cw_v[hh]], writes=[best_v[hh]])
                    for hh in range(8):
                        S.op('dve', lambda e, hh=hh: e.max_index(out=pos[:, hh, 8:16], in_max=best[:, hh, 8:16], in_values=cw[:, hh, :]), reads=[cw_v[hh], best_v[hh]], writes=[pos_v[hh]])
                    S.op('dve', lambda e: e.tensor_scalar(out=nb[:, :], in0=best[:, :, 0], scalar1=-1.0, scalar2=None, op0=ALU.mult), reads=best_v, writes=[nb])
                    for hh in range(8):
                        S.op('act', lambda e, hh=hh: e.activation(out=ex[:, hh, :], in_=best[:, hh, :], func=AF.Exp, bias=nb[:, hh:hh + 1], accum_out=zz[:, hh:hh + 1]),
                             reads=[best_v[hh], nb], writes=[ex_v[hh], zz_v[hh]])
                    S.op('dve', lambda e: e.reciprocal(out=rzz[:, :], in_=zz[:, :]), reads=zz_v, writes=[rzz])
                    S.op('dve', lambda e: e.tensor_tensor(out=gg[:, :, :], in0=ex[:, :, :], in1=bc(rzz[:, :], 2, [128, 8, 16]), op=ALU.mult), reads=ex_v + [rzz], writes=[gg])
                    S.op('dve', lambda e: e.tensor_single_scalar(out=pa_u[:, :, :], in_=pos[:, :, :], scalar=4, op=ALU.logical_shift_right), reads=pos_v, writes=[pa_u])
                    S.op('dve', lambda e: e.tensor_single_scalar(out=pb_u[:, :, :], in_=pos[:, :, :], scalar=15, op=ALU.bitwise_and), reads=pos_v, writes=[pb_u])
                    S.op('dve', lambda e: e.tensor_copy(out=paf[:, :, :], in_=pa_u[:, :, :]), reads=[pa_u], writes=[paf])
                    S.op('dve', lambda e: e.tensor_copy(out=pbf[:, :, :], in_=pb_u[:, :, :]), reads=[pb_u], writes=[pbf])
                    S.op('dve', lambda e: e.tensor_copy(out=idf[:, :, :], in_=idx[:, :, :]), reads=idx_v, writes=[idf])
                    for (sel, two, dst) in ((paf, 0, i1f), (pbf, 1, i2f)):
                        S.op('dve', lambda e, sel=sel: e.tensor_tensor(out=oh[:, :, :, :], in0=bc(bc(iota_f[:, 0:16], 1, [128, 16, 16]), 1, [128, 8, 16, 16]),
                                                                       in1=bc(sel[:, :, :], 3, [128, 8, 16, 16]), op=ALU.is_equal), reads=[sel, iota_f], writes=[oh])
                        S.op('dve', lambda e, two=two: e.tensor_tensor(out=oh[:, :, :, :], in0=oh[:, :, :, :],
                                                                       in1=bc(tv(idf)[:, :, two, :], 2, [128, 8, 16, 16]), op=ALU.mult), reads=[oh, idf], writes=[oh])
                        S.op('dve', lambda e, dst=dst: e.tensor_reduce(out=dst[:, :, :], in_=oh[:, :, :, :], axis=AX.X, op=ALU.add), reads=[oh], writes=[dst])

                    def trr(e):
                        e.transpose(out=ptr_[:, 0, :], in_=i1f.t[:, :, :].rearrange("p h k -> p (h k)"), identity=identf[:, :])
                        e.transpose(out=ptr_[:, 1, :], in_=i2f.t[:, :, :].rearrange("p h k -> p (h k)"), identity=identf[:, :])
                        return e.transpose(out=ptr_[:, 2, :], in_=gg.t[:, :, :].rearrange("p h k -> p (h k)"), identity=identf[:, :])
                    S.op('pe', trr, reads=[i1f, i2f, gg, identf], writes=[ptr_])
                    S.op('act', lambda e, t=t: e.copy(out=rt1[:, t, :], in_=ptr_[:, 0, :]), reads=[ptr_], writes=[rt1])
                    S.op('act', lambda e, t=t: e.copy(out=rt2[:, t, :], in_=ptr_[:, 1, :]), reads=[ptr_], writes=[rt2])
                    S.op('act', lambda e, t=t: e.copy(out=rtg[:, t, :], in_=ptr_[:, 2, :]), reads=[ptr_], writes=[rtg])
            S.end_phase()

        WTp = S.sb("WTp", [128, 256, 128], BF16, True)
        PT_d = dscr("PT_d", [16, 128, 8 * 256], BF16)
        kkc = [0]

        def wb_dve(p, sbi, Aoh, Boh, part=None):
            t = 2 * p + sbi // 8
            n0 = (sbi % 8) * 16
            A_, B_ = Aoh[sbi % 2], Boh[sbi % 2]
            if part in (None, 0):
                S.op('dve', lambda e: e.tensor_tensor(out=A_[:, :, :], in0=bc(iota_f[:, :], 1, [128, 16, 128]),
                                                      in1=bc(rt1[:, t, n0:n0 + 16], 2, [128, 16, 128]), op=ALU.is_equal),
                     reads=[iota_f, rt1], writes=[A_])
            if part in (None, 1):
                S.op('dve', lambda e: e.tensor_tensor(out=A_[:, :, :], in0=A_[:, :, :],
                                                      in1=bc(rtg[:, t, n0:n0 + 16], 2, [128, 16, 128]), op=ALU.mult),
                     reads=[A_, rtg], writes=[A_])
            if part in (None, 2):
                S.op('dve', lambda e: e.tensor_tensor(out=B_[:, :, :], in0=bc(iota_f[:, :], 1, [128, 16, 128]),
                                                      in1=bc(rt2[:, t, n0:n0 + 16], 2, [128, 16, 128]), op=ALU.is_equal),
                     reads=[iota_f, rt2], writes=[B_])

        def wb_pe(p, sbi, q4, Aoh, Boh, pW):
            A_, B_ = Aoh[sbi % 2], Boh[sbi % 2]
            kkc[0] += 1
            pw = pW[kkc[0] % 2]

            def mmw(e):
                for n in range(4):
                    ins = e.matmul(pw[:, n, :], lhsT=B_[:, q4 * 4 + n, :], rhs=A_[:, q4 * 4 + n, :], start=True, stop=True)
                return ins
            S.op('pe', mmw, reads=[A_, B_], writes=[pw])
            nn = (sbi // 8) * 128 + (sbi % 8) * 16 + q4 * 4
            S.op('dve', lambda e: e.tensor_copy(out=WTp[:, nn:nn + 4, :], in_=pw[:, :, :]), reads=[pw], writes=[WTp])

        gt2 = S.sb("gt2", [128, D], F32, True)
        fg = S.sb("fg", [128, D], F32, True)
        x1t = S.sb("x1t", [128, D], F32, True)
        xf = [S.sb("xf%d" % i, [128, D], F32, True) for i in range(2)]
        jk2 = S.sb("jk2", [128, D], BF16, True)
        fs = [S.sb("fs%d" % i, [128, 1], F32, True) for i in range(2)]
        fd = [S.sb("fd%d" % i, [128, 1], F32, True) for i in range(2)]
        fr = [S.sb("fr%d" % i, [128, 1], F32, True) for i in range(2)]

        def epi_compute(p, a_):
            t = 2 * p + a_
            xf_ = xf[a_]
            S.dma(x1t[:, :], X1_d[t * 128:(t + 1) * 128, :], x1t, reads=[X1_d], writes=[x1t])
            S.op('pool', lambda e: e.tensor_tensor(out=xf_[:, :], in0=xf_[:, :], in1=x1t[:, :], op=ALU.add), reads=[xf_, x1t], writes=[xf_])
            fa_, fb_, fc_ = fs[a_], fd[a_], fr[a_]
            S.op('act', lambda e: e.activation(out=jk2[:, :], in_=xf_[:, :], func=AF.Square, accum_out=fa_[:, :]), reads=[xf_], writes=[jk2, fa_])
            S.op('act', lambda e: e.activation(out=fb_[:, :], in_=fa_[:, :], func=AF.Sqrt, scale=1.0 / D, bias=EPS), reads=[fa_], writes=[fb_])
            S.op('dve', lambda e: e.reciprocal(out=fc_[:, :], in_=fb_[:, :]), reads=[fb_], writes=[fc_])
            S.op('dve', lambda e: e.scalar_tensor_tensor(out=xf_[:, :], in0=xf_[:, :], scalar=fc_[:, 0:1], in1=fg[:, :], op0=ALU.mult, op1=ALU.mult),
                 reads=[xf_, fc_, fg], writes=[xf_])

        def epi_store(p, a_):
            t = 2 * p + a_
            xf_ = xf[a_]
            S.dma(out[t * 128:(t + 1) * 128, :], xf_[:, :], xf_, reads=[xf_], writes=[out])

        def epilogue(p):
            for a_ in range(2):
                epi_compute(p, a_)
                epi_store(p, a_)

        with phase_begin():
            S.dma(gt2[:, :], mrow.t[0, 5 * D:6 * D].partition_broadcast(128), gt2, reads=[mrow], writes=[gt2])
            S.dma(fg[:, :], final_g.t.partition_broadcast(128), fg, reads=[final_g], writes=[fg])
            Aoh = [S.sb("Aoh%d" % i, [128, 16, 128], BF16) for i in range(2)]
            Boh = [S.sb("Boh%d" % i, [128, 16, 128], BF16) for i in range(2)]
            pW = [S.ps("pW%d" % i, [128, 4, 128], F32) for i in range(2)]
            for sbi in range(16):
                wb_dve(0, sbi, Aoh, Boh)
                for q4 in range(4):
                    wb_pe(0, sbi, q4, Aoh, Boh, pW)
            S.end_phase()

        for p in range(8):
            with phase_begin():
                hnP = S.sb("hnP", [128, 16, 256], BF16)
                S.dma(hnP[:, :, :], hn_d[:, :, p * 256:(p + 1) * 256], hnP, reads=[hn_d], writes=[hnP])
                NB = 4
                ut = [S.sb("ut%d" % i, [128, 16, 128], BF16) for i in range(NB)]
                gl = [S.sb("gl%d" % i, [128, 256], F32) for i in range(2)]
                pst = [S.sb("pst%d" % i, [128, 8, 256], BF16) for i in range(2)]
                pSe = [S.ps("pSe%d" % i, [128, 512], F32) for i in range(2)]

                def ldu(c):
                    if c >= 128:
                        return
                    u_ = ut[c % NB]
                    S.dma(u_.t[:, :, :].rearrange("p j e -> p (j e)"), UT_d[c, :, :], u_, reads=[UT_d], writes=[u_])

                def s_grp(c):
                    u_ = ut[c % NB]
                    ps_ = pSe[c % 2]

                    def mmse(e):
                        for j in range(16):
                            ins = e.matmul(ps_[:, 0:256], lhsT=u_[:, j, :], rhs=hnP[:, j, :], start=(j == 0), stop=(j == 15))
                        return ins
                    S.op('pe', mmse, reads=[u_, hnP], writes=[ps_])
                ldu(0)
                ldu(1)
                ldu(2)
                s_grp(0)
                for c in range(128):
                    if p >= 1:
                        if c == 8:
                            epi_compute(p - 1, 0)
                        if c == 40:
                            epi_store(p - 1, 0)
                            epi_compute(p - 1, 1)
                        if c == 72:
                            epi_store(p - 1, 1)
                    ldu(c + 3)
                    if c + 1 < 128:
                        s_grp(c + 1)
                    ps_ = pSe[c % 2]
                    g_ = gl[c % 2]
                    st_ = pst[(c // 8) % 2]
                    S.op('act', lambda e, ps_=ps_, g_=g_: e.activation(out=g_[:, :], in_=ps_[:, 0:256], func=AF.Gelu_apprx_tanh), reads=[ps_], writes=[g_])
                    S.op('dve', lambda e, g_=g_, st_=st_, c=c: e.tensor_tensor(out=st_[:, c % 8, :], in0=g_[:, :], in1=WTp[:, :, c], op=ALU.mult),
                         reads=[g_, WTp], writes=[st_])
                    if c % 8 == 7:
                        S.dma(PT_d[c // 8, :, :], st_.t[:, :, :].rearrange("p a n -> p (a n)"), st_, reads=[st_], writes=[PT_d])
                S.end_phase()
            with phase_begin():
                ptl = [S.sb("ptl%d" % i, [128, 8, 256], BF16) for i in range(2)]
                vvl = [S.sb("vvl%d" % i, [128, 2, 1024], BF16) for i in range(4)]
                Aoh = [S.sb("Aoh%d" % i, [128, 16, 128], BF16) for i in range(2)]
                Boh = [S.sb("Boh%d" % i, [128, 16, 128], BF16) for i in range(2)]
                pOu = [S.ps("pOu%d" % i, [128, 512], F32) for i in range(4)]
                pW = [S.ps("pW%d" % i, [128, 4, 128], F32) for i in range(2)]

                def ldp(g):
                    S.dma(ptl[g % 2].t[:, :, :].rearrange("p a n -> p (a n)"), PT_d[g % 16, :, :], ptl[g % 2], reads=[PT_d], writes=[ptl[g % 2]])

                def ldv(g, half):
                    if g >= 64:
                        return
                    c0 = g * 2
                    S.dma(vvl[g % 4][:, :, :], VB_d.t[c0 * 128:(c0 + 2) * 128, half * 1024:(half + 1) * 1024].rearrange("(t e) d -> e t d", e=128),
                          vvl[g % 4], reads=[VB_d], writes=[vvl[g % 4]])
                for half in range(2):
                    ldp(half * 16)
                    ldv(0, half)
                    ldv(1, half)
                    ldv(2, half)
                    for c in range(128):
                        gp = half * 16 + c // 8
                        gv = c // 2
                        if c % 8 == 0 and c + 8 < 128:
                            ldp(gp + 1)
                        if c % 2 == 0:
                            ldv(gv + 3, half)
                        pl_, vl_ = ptl[gp % 2], vvl[gv % 4]

                        def mmv2(e, pl_=pl_, vl_=vl_, c=c):
                            for a_ in range(2):
                                for cbh in range(2):
                                    ins = e.matmul(pOu[a_ * 2 + cbh][:, :], lhsT=pl_[:, c % 8, a_ * 128:(a_ + 1) * 128],
                                                   rhs=vl_[:, c % 2, cbh * 512:(cbh + 1) * 512], start=(c == 0), stop=(c == 127))
                            return ins
                        S.op('pe', mmv2, reads=[pl_, vl_], writes=pOu)
                        if p + 1 < 8:
                            s_ = half * 128 + c
                            if s_ == 0:
                                wb_dve(p + 1, 0, Aoh, Boh)
                            if s_ % 16 in (3, 7, 11, 15):
                                wb_pe(p + 1, s_ // 16, (s_ % 16 - 3) // 4, Aoh, Boh, pW)
                            if s_ % 16 in (4, 8, 12) and s_ // 16 + 1 < 16:
                                wb_dve(p + 1, s_ // 16 + 1, Aoh, Boh, part=(s_ % 16) // 4 - 1)
                    for a_ in range(2):
                        for cbh in range(2):
                            col = half * 1024 + cbh * 512
                            S.op('dve', lambda e, a_=a_, cbh=cbh, col=col: e.tensor_tensor(out=xf[a_][:, col:col + 512], in0=pOu[a_ * 2 + cbh][:, :], in1=gt2[:, col:col + 512], op=ALU.mult),
                                 reads=[pOu[a_ * 2 + cbh], gt2], writes=[xf[a_]])
                if p == 7:
                    epilogue(7)
                S.end_phase()
    return nc


def _rope_tables():
    t = np.arange(NTOK)
    row = (t // 64).astype(np.float32)
    col = (t % 64).astype(np.float32)
    freqs = (10000.0 ** (-np.arange(16, dtype=np.float32) / 16)).astype(np.float32)
    ar = row[:, None] * freqs[None, :]
    ac = col[:, None] * freqs[None, :]
    ang = np.concatenate([ar, ar, ac, ac], axis=-1).astype(np.float32)
    cos = np.cos(ang).astype(np.float32)
    sin = np.sin(ang).astype(np.float32)
    sgn = np.concatenate([-np.ones(16), np.ones(16), -np.ones(16), np.ones(16)]).astype(np.float32)
    return np.concatenate([cos, sin * sgn[None, :]], axis=1).astype(np.float32)


def _local_rows(c):
    base = 32 * c - 4
    rows = [base + i for i in range(40)]
    if c == 0:
        rows[0:4] = [6, 7, 8, 9]
    if c == NCORE - 1:
        rows[36:40] = [248, 249, 246, 247]
    return rows


def _na_bias(rpb, c):
    rows = _local_rows(c)
    outb = np.full((8, 5, 5, 128, 128), NEG, np.float32)
    qc = np.arange(64)
    cstart = np.clip(qc - 8, 0, 48)
    for slot, j in enumerate([0, 1, 2, 14, 15]):
        seen = set()
        for kr in range(10):
            gk = rows[2 * j + kr]
            if gk in seen or gk < 0 or gk > 255:
                continue
            seen.add(gk)
            for a in range(2):
                gq = rows[2 * j + 4 + a]
                rs = min(max(gq - 4, 0), 248)
                if not (rs <= gk < rs + 8):
                    continue
                dr = gk - gq + 7
                kc = np.arange(64)
                valid = (kc[:, None] >= cstart[None, :]) & (kc[:, None] < cstart[None, :] + 16)
                dc = kc[:, None] - qc[None, :] + 15
                vals = rpb[:, dr, :][:, np.clip(dc, 0, 30)]
                blockv = np.where(valid[None], vals, NEG).astype(np.float32)
                chunk, kin = (kr * 64) // 128, (kr * 64) % 128
                outb[:, slot, chunk, kin:kin + 64, a * 64:(a + 1) * 64] = blockv
    return np.ascontiguousarray(outb.transpose(0, 3, 1, 2, 4)).reshape(8, 128, 5 * 5 * 128)


def kernel(x, c, ctx, c_ctx, w_mod, b_mod, norm1_g, norm2_g, w_in, lambda_q1, lambda_k1, lambda_q2, lambda_k2,
           subln_g, na_rpb, w_branch_a, w_branch_b, w_out, peer_w_q, peer_subkeys, peer_u, peer_v, final_g):
    f = lambda a: np.ascontiguousarray(np.asarray(a, dtype=np.float32))
    x2 = f(x)[0]
    rope = _rope_tables()
    rope_q = rope * np.float32(0.125)
    cT = np.stack([f(c)[0].reshape(16, 128).T, f(c_ctx).reshape(16, 128).T], axis=-1).reshape(128, 32)
    shared = {
        "x_all": x2, "ctx": f(ctx)[0], "cT": f(cT),
        "g1T": f(f(norm1_g)[0].reshape(16, 128).T), "g2T": f(f(norm2_g)[0].reshape(16, 128).T),
        "w_mod": f(w_mod)[0], "b_mod": f(b_mod)[0], "w_in": f(w_in)[0],
        "lams": f(np.concatenate([f(lambda_q1)[0], f(lambda_k1)[0], f(lambda_q2)[0], f(lambda_k2)[0]])),
        "subg": f(subln_g)[0], "rope_all": rope,
        "w_a": f(w_branch_a)[0], "w_b": f(w_branch_b)[0], "w_o": f(w_out)[0], "w_q": f(peer_w_q)[0],
        "subT": f(f(peer_subkeys)[0].transpose(3, 0, 1, 2).reshape(128, 16 * 128)),
        "peer_u": f(peer_u)[0], "peer_v": f(peer_v)[0], "final_g": f(final_g),
    }
    rpb = f(na_rpb)[0]
    xg = x2.reshape(256, 64, D)
    in_maps = []
    for ci in range(NCORE):
        rows = _local_rows(ci)
        halo = np.concatenate([xg[rows[0:4]].reshape(256, D), xg[rows[36:40]].reshape(256, D)], axis=0)
        m = dict(shared)
        m["x_own"] = f(x2[ci * OWN:(ci + 1) * OWN])
        m["x_halo"] = f(halo)
        m["na_bias"] = _na_bias(rpb, ci)
        m["rope_own"] = f(rope_q[ci * OWN:(ci + 1) * OWN])
        in_maps.append(m)
    nc = build_program()
    res = run_bass_kernel_spmd(nc, in_maps, core_ids=list(range(NCORE)))
    outs = [np.asarray(r["out"], dtype=np.float32) for r in res.results]
    return np.concatenate(outs, axis=0).reshape(1, NTOK, D)
```

```python
import numpy as np
from contextlib import ExitStack
import concourse.bass as bass
import concourse.mybir as mybir
from concourse.bass_utils import run_bass_kernel_spmd

F32 = mybir.dt.float32
BF16 = mybir.dt.bfloat16
U32 = mybir.dt.uint32
AF = mybir.ActivationFunctionType
ALU = mybir.AluOpType
AX = mybir.AxisListType

EPOCH = 12000
SAME_ENG_SYNC = {'pe': False, 'act': True, 'dve': True, 'pool': True, 'sp': False}

D = 2048
NTOK = 16384
NCORE = 8
OWN = 2048
NLOC = 2816
NKEY = NTOK + 256
EPS = 1e-6
NEG = -30000.0


class Buf:
    def __init__(self, name, t):
        self.name = name
        self.t = t
        self.w = {}
        self.r = {}
        self.dsem = None
        self.dcnt = 0

    def __getitem__(self, idx):
        return self.t[idx]


def _merge(d, s):
    for k, v in s.items():
        if d.get(k, 0) < v:
            d[k] = v


class Sched:
    CE = ['pe', 'act', 'dve', 'pool']
    ENG = ['pe', 'act', 'dve', 'pool', 'sp']

    def __init__(self, nc, es):
        self.nc = nc
        self.es = es
        self.es_phase = None
        self.prog = {e: [] for e in self.ENG}
        self.cnt = {e: 0 for e in self.CE}
        self.waited = {e: {} for e in self.ENG}
        self.semobj = {}
        self.cur = {}
        self.nd = 0
        self.nops = 0
        self.free_dsems = []
        self.phase_bufs = []

    def _sem(self, key):
        if key not in self.semobj:
            name = 's_' + '_'.join(str(x) for x in key)
            self.semobj[key] = self.es.enter_context(self.nc.semaphore(name))
        return self.semobj[key]

    def sb(self, name, shape, dt, persistent=False):
        st = self.es if persistent else self.es_phase
        self.nd_names = getattr(self, 'nd_names', 0) + 1
        name = "%s_%d" % (name, self.nd_names)
        b = Buf(name, st.enter_context(self.nc.sbuf_tensor(name, list(shape), dt)))
        if not persistent:
            self.phase_bufs.append(b)
        return b

    def ps(self, name, shape, dt):
        self.nd_names = getattr(self, 'nd_names', 0) + 1
        name = "%s_%d" % (name, self.nd_names)
        return Buf(name, self.es_phase.enter_context(self.nc.psum_tensor(name, list(shape), dt)))

    def _deps(self, eng, reads, writes, skip=None):
        deps = {}
        for b in reads:
            _merge(deps, b.w)
        for b in writes:
            _merge(deps, b.w)
            _merge(deps, b.r)
        waits = []
        for k, v in deps.items():
            if skip is not None and k == skip:
                continue
            if k[0] == eng and not SAME_ENG_SYNC[eng]:
                continue
            if self.waited[eng].get(k, 0) >= v:
                continue
            self.waited[eng][k] = v
            waits.append((k, v))
        return waits

    def op(self, eng, fn, reads=(), writes=()):
        waits = self._deps(eng, reads, writes)
        self.cnt[eng] += 1
        c = self.cnt[eng]
        key = (eng, (c - 1) // EPOCH)
        val = (c - 1) % EPOCH + 1
        self._sem(key)
        self.cur[key] = val
        self.prog[eng].append((waits, fn, key, 1))
        for b in reads:
            b.r[key] = max(b.r.get(key, 0), val)
        for b in writes:
            b.w[key] = max(b.w.get(key, 0), val)
            b.r = {}
        self.nops += 1

    def dma(self, out_ap, in_ap, sembuf, reads=(), writes=(), **kw):
        if sembuf.dsem is None:
            if self.free_dsems:
                sembuf.dsem, sembuf.dcnt = self.free_dsems.pop()
            else:
                sembuf.dsem = ('d', self.nd)
                self.nd += 1
                self._sem(sembuf.dsem)
        key = sembuf.dsem
        waits = self._deps('sp', reads, writes, skip=key)
        sembuf.dcnt += 16
        val = sembuf.dcnt
        self.cur[key] = val

        def fn(e, out_ap=out_ap, in_ap=in_ap, kw=kw):
            return e.dma_start(out=out_ap, in_=in_ap, **kw)
        self.prog['sp'].append((waits, fn, key, 16))
        for b in reads:
            b.r[key] = max(b.r.get(key, 0), val)
        for b in writes:
            b.w[key] = max(b.w.get(key, 0), val)
            b.r = {}
        self.nops += 1

    def barrier(self):
        for e in self.ENG:
            waits = []
            for k, v in self.cur.items():
                if k[0] == e:
                    continue
                if self.waited[e].get(k, 0) >= v:
                    continue
                self.waited[e][k] = v
                waits.append((k, v))
            if waits:
                self.prog[e].append((waits, None, None, 0))

    def flush(self):
        nc = self.nc
        prog = self.prog
        self.prog = {e: [] for e in self.ENG}

        def run(eobj, lst):
            for waits, fn, key, inc in lst:
                for k, v in waits:
                    eobj.wait_ge(self.semobj[k], v)
                if fn is not None:
                    fn(eobj).then_inc(self.semobj[key], inc)

        with nc.Block() as block:
            @block.tensor
            def _(e):
                run(e, prog['pe'])

            @block.scalar
            def _(e):
                run(e, prog['act'])

            @block.vector
            def _(e):
                run(e, prog['dve'])

            @block.gpsimd
            def _(e):
                run(e, prog['pool'])

            @block.sync
            def _(e):
                run(e, prog['sp'])

    def end_phase(self, release=True):
        self.barrier()
        self.flush()
        if release:
            for b in self.phase_bufs:
                if b.dsem is not None:
                    self.free_dsems.append((b.dsem, b.dcnt))
                    b.dsem = None
            self.phase_bufs = []


def bc(ap, axis, shape):
    return ap.unsqueeze(axis).to_broadcast(list(shape))


def build_program(stop_after=None, dbg=()):
    nc = bass.Bass("TRN2", target_bir_lowering=False)

    def din(name, shape, dt=F32):
        return Buf(name, nc.dram_tensor(name, list(shape), dt, kind="ExternalInput").ap())

    def dscr(name, shape, dt):
        return Buf(name, nc.dram_tensor(name, list(shape), dt, kind="Internal").ap())

    x_all = din("x_all", [NTOK, D])
    x_own = din("x_own", [OWN, D])
    x_halo = din("x_halo", [512, D])
    ctx = din("ctx", [256, D])
    cT = din("cT", [128, 32])
    g1T = din("g1T", [128, 16])
    g2T = din("g2T", [128, 16])
    w_mod = din("w_mod", [D, 6 * D])
    b_mod = din("b_mod", [6 * D])
    w_in = din("w_in", [D, 10240])
    lams = din("lams", [256])
    subg = din("subg", [128])
    na_bias = din("na_bias", [8, 128, 5 * 5 * 128])
    rope_all = din("rope_all", [NTOK, 128])
    rope_own = din("rope_own", [OWN, 128])
    w_a = din("w_a", [1024, D])
    w_b = din("w_b", [1024, D])
    w_o = din("w_o", [D, D])
    w_q = din("w_q", [D, D])
    subT = din("subT", [128, 16 * 128])
    peer_u = din("peer_u", [NTOK, D])
    peer_v = din("peer_v", [NTOK, D])
    final_g = din("final_g", [D])
    out = Buf("out", nc.dram_tensor("out", [OWN, D], F32, kind="ExternalOutput").ap())

    mrow = dscr("mrow", [2, 6 * D], F32)
    KaT_d = dscr("KaT_d", [8, 128, NKEY], BF16)
    Va_d = dscr("Va_d", [NKEY, 1024], BF16)
    QaT_d = dscr("QaT_d", [8, 128, OWN], BF16)
    QbT_d = dscr("QbT_d", [8, 128, OWN], BF16)
    KbT_d = dscr("KbT_d", [8, 128, NLOC], BF16)
    Vb_d = dscr("Vb_d", [NLOC, 1024], BF16)
    GT_d = dscr("GT_d", [32, 128, OWN], BF16)
    OaT_d = dscr("OaT_d", [8, 128, OWN], BF16)
    ObT_d = dscr("ObT_d", [8, 128, OWN], BF16)
    mT_d = dscr("mT_d", [128, 16, OWN], BF16)
    X1_d = dscr("X1_d", [OWN, D], F32)
    UT_d = dscr("UT_d", [128, 128, 16 * 128], BF16)
    VB_d = dscr("VB_d", [NTOK, D], BF16)

    dbg_out = {}
    for name, shape in dbg:
        dbg_out[name] = Buf(name, nc.dram_tensor(name, list(shape), F32, kind="ExternalOutput").ap())

    with ExitStack() as es:
        S = Sched(nc, es)
        ident = S.sb("ident", [128, 128], BF16, True)
        identf = S.sb("identf", [128, 128], F32, True)
        iota_f = S.sb("iota_f", [128, 128], F32, True)
        modv = S.sb("modv", [128, 6, 16], F32, True)
        lamv = S.sb("lamv", [128, 4], F32, True)
        gsub = S.sb("gsub", [128, 128], F32, True)

        def phase_begin():
            S.es_phase = ExitStack()
            return S.es_phase

        with phase_begin():
            S.op('pool', lambda e: e.iota(identf[:, :], [[1, 128]], base=0, channel_multiplier=-1,
                                          allow_small_or_imprecise_dtypes=True), writes=[identf])
            S.op('pool', lambda e: e.tensor_single_scalar(out=identf[:, :], in_=identf[:, :], scalar=0.0,
                                                          op=ALU.is_equal), reads=[identf], writes=[identf])
            S.op('pool', lambda e: e.tensor_copy(out=ident[:, :], in_=identf[:, :]), reads=[identf], writes=[ident])
            S.op('pool', lambda e: e.iota(iota_f[:, :], [[1, 128]], base=0, channel_multiplier=0,
                                          allow_small_or_imprecise_dtypes=True), writes=[iota_f])
            c_sb = S.sb("c_sb", [128, 32], F32)
            sc_sb = S.sb("sc_sb", [128, 16, 2], F32)
            S.dma(c_sb[:, :], cT[:, :], c_sb, reads=[cT], writes=[c_sb])
            S.op('act', lambda e: e.activation(out=sc_sb.t[:, :, :].rearrange("p j r -> p (j r)"), in_=c_sb[:, :],
                                               func=AF.Silu), reads=[c_sb], writes=[sc_sb])
            bm_sb = S.sb("bm_sb", [2, 6 * D], F32)
            m_sb = S.sb("m_sb", [2, 6 * D], F32)
            S.dma(bm_sb[:, :], b_mod.t.partition_broadcast(2), bm_sb, reads=[b_mod], writes=[bm_sb])
            wm = [S.sb("wm%d" % i, [128, 16, 512], F32) for i in range(2)]
            pm = [S.ps("pm%d" % i, [2, 512], F32) for i in range(2)]
            for cb in range(24):
                wb = wm[cb % 2]
                S.dma(wb[:, :, :], w_mod.t[:, cb * 512:(cb + 1) * 512].rearrange("(j p) n -> p j n", p=128), wb,
                      reads=[w_mod], writes=[wb])

                def mm0(e, wb=wb, p=pm[cb % 2]):
                    for j in range(16):
                        ins = e.matmul(p[:, :], lhsT=sc_sb[:, j, :], rhs=wb[:, j, :], start=(j == 0), stop=(j == 15))
                    return ins
                S.op('pe', mm0, reads=[sc_sb, wb], writes=[pm[cb % 2]])
                S.op('dve', lambda e, p=pm[cb % 2], cb=cb: e.tensor_tensor(
                    out=m_sb[:, cb * 512:(cb + 1) * 512], in0=p[:, :], in1=bm_sb[:, cb * 512:(cb + 1) * 512],
                    op=ALU.add), reads=[pm[cb % 2], bm_sb], writes=[m_sb])
            S.dma(mrow[:, :], m_sb[:, :], m_sb, reads=[m_sb], writes=[mrow])
            pt0 = S.ps("pt0", [128, 96, 2], F32)
            modT = S.sb("modT", [128, 96, 2], F32)

            def tr0(e):
                for ch in range(96):
                    ins = e.transpose(out=pt0[:, ch, :], in_=m_sb[0:2, ch * 128:(ch + 1) * 128], identity=identf[0:2, 0:2])
                return ins
            S.op('pe', tr0, reads=[m_sb, identf], writes=[pt0])
            S.op('dve', lambda e: e.tensor_copy(out=modT[:, :, :], in_=pt0[:, :, :]), reads=[pt0], writes=[modT])
            g1_sb = S.sb("g1_sb", [128, 16], F32)
            g2_sb = S.sb("g2_sb", [128, 16], F32)
            S.dma(g1_sb[:, :], g1T[:, :], g1_sb, reads=[g1T], writes=[g1_sb])
            S.dma(g2_sb[:, :], g2T[:, :], g2_sb, reads=[g2T], writes=[g2_sb])
            def mk_a(dst, q, r, g):
                S.op('dve', lambda e: e.scalar_tensor_tensor(out=modv[:, dst, :], in0=modT[:, q * 16:(q + 1) * 16, r],
                                                             scalar=1.0, in1=g[:, :], op0=ALU.add, op1=ALU.mult),
                     reads=[modT, g], writes=[modv])

            def mk_b(dst, q, r):
                S.op('dve', lambda e: e.tensor_copy(out=modv[:, dst, :], in_=modT[:, q * 16:(q + 1) * 16, r]),
                     reads=[modT], writes=[modv])
            mk_a(0, 1, 0, g1_sb); mk_b(1, 0, 0)
            mk_a(2, 1, 1, g1_sb); mk_b(3, 0, 1)
            mk_a(4, 4, 0, g2_sb); mk_b(5, 3, 0)
            lq = S.sb("lq", [128, 4, 64], F32)
            lp = S.sb("lp", [128, 2, 64], F32)
            ls = S.sb("ls", [128, 2], F32)
            le = S.sb("le", [128, 2], F32)
            S.dma(lq.t[:, :, :].rearrange("p a b -> p (a b)"), lams.t.partition_broadcast(128), lq, reads=[lams], writes=[lq])
            S.op('dve', lambda e: e.tensor_tensor(out=lp[:, 0, :], in0=lq[:, 0, :], in1=lq[:, 1, :], op=ALU.mult), reads=[lq], writes=[lp])
            S.op('dve', lambda e: e.tensor_tensor(out=lp[:, 1, :], in0=lq[:, 2, :], in1=lq[:, 3, :], op=ALU.mult), reads=[lq, lp], writes=[lp])
            S.op('dve', lambda e: e.tensor_reduce(out=ls[:, :], in_=lp[:, :, :], axis=AX.X, op=ALU.add), reads=[lp], writes=[ls])
            S.op('act', lambda e: e.activation(out=le[:, :], in_=ls[:, :], func=AF.Exp), reads=[ls], writes=[le])
            S.op('dve', lambda e: e.tensor_tensor(out=lamv[:, 2:3], in0=le[:, 0:1], in1=le[:, 1:2], op=ALU.subtract), reads=[le], writes=[lamv])
            S.op('dve', lambda e: e.tensor_scalar(out=lamv[:, 0:1], in0=lamv[:, 2:3], scalar1=0.2, scalar2=None, op0=ALU.add), reads=[lamv], writes=[lamv])
            S.op('dve', lambda e: e.tensor_scalar(out=lamv[:, 1:2], in0=lamv[:, 0:1], scalar1=-1.0, scalar2=None, op0=ALU.mult), reads=[lamv], writes=[lamv])
            sg = S.sb("sg", [128, 128], F32)
            S.dma(sg[:, :], subg.t.partition_broadcast(128), sg, reads=[subg], writes=[sg])
            S.op('dve', lambda e: e.tensor_scalar(out=gsub[:, :], in0=sg[:, :], scalar1=0.8, scalar2=None, op0=ALU.mult), reads=[sg], writes=[gsub])
            if 'd_m' in dbg_out:
                S.dma(dbg_out['d_m'][:, :], m_sb[:, :], m_sb, reads=[m_sb], writes=[dbg_out['d_m']])
            S.end_phase()
        if stop_after == 0:
            return nc

        def make_norm(nx):
            R = {}
            R['xt'] = [S.sb("n_xt%d" % i, [128, D], F32) for i in range(nx)]
            R['junk'] = S.sb("n_junk", [128, D], BF16)
            R['ssq'] = [S.sb("n_ssq%d" % i, [128, 1], F32) for i in range(2)]
            R['std'] = [S.sb("n_std%d" % i, [128, 1], F32) for i in range(2)]
            R['rstd'] = [S.sb("n_rstd%d" % i, [128, 1], F32) for i in range(2)]
            R['xn'] = [S.sb("n_xn%d" % i, [128, D], BF16) for i in range(2)]
            R['ptr'] = [S.ps("n_ptr%d" % i, [128, 1024], BF16) for i in range(2)]
            R['tmp'] = S.sb("n_tmp", [128, D], F32)
            R['k'] = 0
            return R

        def norm_a1(R, src_ap, srcbuf, preloaded=None):
            k = R['k']
            R['k'] += 1
            if preloaded is None:
                xt = R['xt'][k % len(R['xt'])]
                S.dma(xt[:, :], src_ap, xt, reads=[srcbuf], writes=[xt])
            else:
                xt = preloaded
            ssq, std, rstd, xn = R['ssq'][k % 2], R['std'][k % 2], R['rstd'][k % 2], R['xn'][k % 2]
            junk = R['junk']
            S.op('act', lambda e: e.activation(out=junk[:, :], in_=xt[:, :], func=AF.Square, accum_out=ssq[:, :]),
                 reads=[xt], writes=[junk, ssq])
            S.op('act', lambda e: e.activation(out=std[:, :], in_=ssq[:, :], func=AF.Sqrt, scale=1.0 / D, bias=EPS),
                 reads=[ssq], writes=[std])
            S.op('dve', lambda e: e.reciprocal(out=rstd[:, :], in_=std[:, :]), reads=[std], writes=[rstd])
            S.op('act', lambda e: e.activation(out=xn[:, :], in_=xt[:, :], func=AF.Copy, scale=rstd[:, 0:1]),
                 reads=[xt, rstd], writes=[xn])
            return xn

        def norm_a2(R, xn, va, vb_, out_ap, outbuf):
            tmp = R['tmp']
            for half in range(2):
                ptr = R['ptr'][half]

                def trn(e, ptr=ptr, half=half):
                    for jj in range(8):
                        j = half * 8 + jj
                        ins = e.transpose(out=ptr[:, jj * 128:(jj + 1) * 128], in_=xn[:, j * 128:(j + 1) * 128], identity=ident[:, :])
                    return ins
                S.op('pe', trn, reads=[xn, ident], writes=[ptr])
                S.op('dve', lambda e, ptr=ptr, half=half: e.tensor_tensor(
                    out=tmp.t[:, half * 1024:(half + 1) * 1024].rearrange("p (j t) -> p j t", j=8),
                    in0=ptr.t[:, :].rearrange("p (j t) -> p j t", j=8),
                    in1=bc(modv[:, va, half * 8:(half + 1) * 8], 2, [128, 8, 128]), op=ALU.mult),
                    reads=[ptr, modv], writes=[tmp])
            S.op('pool', lambda e: e.tensor_tensor(
                out=out_ap, in0=tmp.t[:, :].rearrange("p (j t) -> p j t", j=16),
                in1=bc(modv[:, vb_, :], 2, [128, 16, 128]), op=ALU.add), reads=[tmp, modv], writes=[outbuf])

        def norm_tile(R, src_ap, srcbuf, va, vb_, out_ap, outbuf, preloaded=None):
            xn = norm_a1(R, src_ap, srcbuf, preloaded)
            norm_a2(R, xn, va, vb_, out_ap, outbuf)

        with phase_begin():
            Wkv = S.sb("Wkv", [128, 16, 2048], BF16)
            stg = [S.sb("stg%d" % i, [128, 16, 128], F32) for i in range(2)]
            for cb in range(16):
                st = stg[cb % 2]
                S.dma(st[:, :, :], w_in.t[:, 1024 + cb * 128:1024 + (cb + 1) * 128].rearrange("(j p) n -> p j n", p=128), st,
                      reads=[w_in], writes=[st])
                (S.op('act', lambda e, st=st, cb=cb: e.copy(out=Wkv[:, :, cb * 128:(cb + 1) * 128], in_=st[:, :, :]), reads=[st], writes=[Wkv]) if cb % 2 else
                 S.op('dve', lambda e, st=st, cb=cb: e.tensor_copy(out=Wkv[:, :, cb * 128:(cb + 1) * 128], in_=st[:, :, :]), reads=[st], writes=[Wkv]))
            R = make_norm(0)
            hT = [S.sb("hT%d" % i, [128, 16, 128], BF16) for i in range(3)]
            cs = [S.sb("cs%d" % i, [128, 128], F32) for i in range(3)]
            pk = [S.ps("pk%d" % i, [128, 512], F32) for i in range(4)]
            kraw = [[S.sb("kraw%d_%d" % (i, b_), [128, 512], F32) for b_ in range(2)] for i in range(2)]
            kf1 = [S.sb("kf1_%d" % i, [128, 512], F32) for i in range(2)]
            kf2 = [S.sb("kf2_%d" % i, [128, 512], F32) for i in range(2)]
            kb16 = [S.sb("kb16_%d" % i, [128, 1024], BF16) for i in range(2)]
            vb16 = [S.sb("vb16_%d" % i, [128, 1024], BF16) for i in range(2)]
            ptk = S.ps("ptk", [128, 1024], BF16)
            kst = [S.sb("kst%d" % i, [128, 8, 512], BF16) for i in range(2)]
            ntile = 130

            xts = [S.sb("xts%d" % i, [128, D], F32) for i in range(3)]

            def ldx(i):
                if i >= ntile:
                    return
                is_ctx = i >= 128
                src = ctx if is_ctx else x_all
                r0 = (i - 128) * 128 if is_ctx else i * 128
                S.dma(xts[i % 3][:, :], src[r0:r0 + 128, :], xts[i % 3], reads=[src], writes=[xts[i % 3]])

            xn_of = {}

            def stA1(i):
                if i >= ntile:
                    return
                if i == 0:
                    ldx(0)
                    ldx(1)
                ldx(i + 2)
                xn_of[i] = norm_a1(R, None, None, preloaded=xts[i % 3])

            def stA2(i):
                if i >= ntile:
                    return
                is_ctx = i >= 128
                r0 = (i - 128) * 128 if is_ctx else i * 128
                h = hT[i % 3]
                norm_a2(R, xn_of.pop(i), 2 if is_ctx else 0, 3 if is_ctx else 1, h[:, :, :], h)
                if not is_ctx:
                    S.dma(cs[i % 3][:, :], rope_all[r0:r0 + 128, :], cs[i % 3], reads=[rope_all], writes=[cs[i % 3]])

            def stB(i):
                is_ctx = i >= 128
                h = hT[i % 3]
                kb_ = kb16[i % 2]
                vb_ = vb16[i % 2]
                for blk in range(4):
                    p = pk[blk]

                    def mmk(e, p=p, blk=blk):
                        for j in range(16):
                            ins = e.matmul(p[:, :], lhsT=h[:, j, :], rhs=Wkv[:, j, blk * 512:(blk + 1) * 512], start=(j == 0), stop=(j == 15))
                        return ins
                    S.op('pe', mmk, reads=[h, Wkv], writes=[p])
                    if blk >= 2:
                        S.op('act', lambda e, p=p, blk=blk: e.copy(out=vb_[:, (blk - 2) * 512:(blk - 1) * 512], in_=p[:, :]), reads=[p], writes=[vb_])
                    elif is_ctx:
                        S.op('act', lambda e, p=p, blk=blk: e.copy(out=kb_[:, blk * 512:(blk + 1) * 512], in_=p[:, :]), reads=[p], writes=[kb_])
                    else:
                        kr = kraw[i % 2][blk]
                        S.op('act', lambda e, p=p, kr=kr: e.copy(out=kr[:, :], in_=p[:, :]), reads=[p], writes=[kr])
                if not is_ctx:
                    c_ = cs[i % 3]
                    for blk in range(2):
                        kr = kraw[i % 2][blk]
                        t1, t2 = kf1[blk], kf2[blk]
                        S.op('dve', lambda e, kr=kr, t1=t1: e.tensor_tensor(
                            out=t1.t[:, :].rearrange("p (g d) -> p g d", g=8), in0=kr.t[:, :].rearrange("p (g d) -> p g d", g=8),
                            in1=bc(c_[:, 0:64], 1, [128, 8, 64]), op=ALU.mult), reads=[kr, c_], writes=[t1])
                        for ab in range(2):
                            S.op('dve', lambda e, kr=kr, t2=t2, ab=ab: e.tensor_tensor(
                                out=t2.t[:, :].rearrange("p (g r a d) -> p g r a d", g=8, r=2, a=2)[:, :, :, ab, :],
                                in0=kr.t[:, :].rearrange("p (g r a d) -> p g r a d", g=8, r=2, a=2)[:, :, :, 1 - ab, :],
                                in1=bc(c_.t[:, 64:128].rearrange("p (r a d) -> p r a d", r=2, a=2)[:, :, ab, :], 1, [128, 8, 2, 16]),
                                op=ALU.mult), reads=[kr, c_], writes=[t2])
                        S.op('pool', lambda e, t1=t1, t2=t2, blk=blk: e.tensor_tensor(
                            out=kb_[:, blk * 512:(blk + 1) * 512], in0=t1[:, :], in1=t2[:, :], op=ALU.add),
                            reads=[t1, t2], writes=[kb_])
                S.dma(Va_d[i * 128:(i + 1) * 128, :], vb_[:, :], vb_, reads=[vb_], writes=[Va_d])

            def stC(i):
                kb_ = kb16[i % 2]

                def trk(e):
                    for hh in range(8):
                        ins = e.transpose(out=ptk[:, hh * 128:(hh + 1) * 128], in_=kb_[:, hh * 128:(hh + 1) * 128], identity=ident[:, :])
                    return ins
                S.op('pe', trk, reads=[kb_, ident], writes=[ptk])
                ks = kst[(i // 4) % 2]
                S.op('act', lambda e: e.copy(out=ks[:, :, (i % 4) * 128:(i % 4 + 1) * 128],
                                             in_=ptk.t[:, :].rearrange("p (h t) -> p h t", h=8)),
                     reads=[ptk], writes=[ks])
                if i % 4 == 3 or i == ntile - 1:
                    nt = (i % 4 + 1) * 128
                    t0 = (i // 4) * 512
                    S.dma(KaT_d.t[:, :, t0:t0 + nt].rearrange("h p t -> p h t"), ks[:, :, 0:nt], ks, reads=[ks], writes=[KaT_d])

            stA1(0)
            stA2(0)
            stA1(1)
            stA2(1)
            for k_ in range(ntile + 2):
                stA1(k_ + 2)
                if k_ < ntile:
                    stB(k_)
                if 1 <= k_ <= ntile:
                    stC(k_ - 1)
                stA2(k_ + 2)
            S.end_phase()

        with ExitStack() as outer1b:
            S.es_phase = outer1b
            hL = S.sb("hL", [128, 16, NLOC], BF16)
            with phase_begin():
                R = make_norm(2)
                for t in range(22):
                    if t < 2:
                        src, r0 = x_halo, t * 128
                    elif t < 18:
                        src, r0 = x_own, (t - 2) * 128
                    elif t < 20:
                        src, r0 = x_halo, 256 + (t - 18) * 128
                    else:
                        src, r0 = ctx, (t - 20) * 128
                    isc = t >= 20
                    norm_tile(R, src[r0:r0 + 128, :], src, 2 if isc else 0, 3 if isc else 1, hL[:, :, t * 128:(t + 1) * 128], hL)
                S.end_phase(release=False)
            S.es_phase = ExitStack()
            inner1b = S.es_phase
            inner1b.__enter__()
            wst = [S.sb("wst%d" % i, [128, 16, 256], F32) for i in range(2)]
            wbb = [S.sb("wbb%d" % i, [128, 16, 256], BF16) for i in range(2)]
            pp = [S.ps("pp%d" % i, [128, 512], F32) for i in range(4)]
            ptq = S.ps("ptq", [128, 1024], BF16)
            csq = [S.sb("csq%d" % i, [128, 128], F32) for i in range(2)]
            qf1 = [S.sb("qf1_%d" % i, [128, 256], F32) for i in range(2)]
            qf2 = [S.sb("qf2_%d" % i, [128, 256], F32) for i in range(2)]
            q16 = [S.sb("q16_%d" % i, [128, 256], BF16) for i in range(2)]
            stgA = [S.sb("stgA%d" % i, [128, 2, NLOC], BF16) for i in range(2)]
            v16 = [S.sb("v16_%d" % i, [128, 256], BF16) for i in range(3)]
            blocks = []
            for b in range(4):
                blocks.append(('qa', b * 256, b))
            for b in range(4):
                blocks.append(('qb', 3072 + b * 256, b))
            for b in range(4):
                blocks.append(('kb', 4096 + b * 256, b))
            for b in range(4):
                blocks.append(('vb', 5120 + b * 256, b))
            for b in range(16):
                blocks.append(('g', 6144 + b * 256, b))
            kctr = 0
            def ldw(bi):
                if bi >= len(blocks):
                    return
                c0_ = blocks[bi][1]
                ws, wb = wst[bi % 2], wbb[bi % 2]
                S.dma(ws[:, :, :], w_in.t[:, c0_:c0_ + 256].rearrange("(j p) n -> p j n", p=128), ws, reads=[w_in], writes=[ws])
                S.op('dve', lambda e: e.tensor_copy(out=wb[:, :, :], in_=ws[:, :, :]), reads=[ws], writes=[wb])
            ldw(0)
            for bi, (kind, c0, b) in enumerate(blocks):
                ws, wb = wst[bi % 2], wbb[bi % 2]
                ldw(bi + 1)
                sA = stgA[bi % 2]
                if kind == 'qa':
                    for t in range(16):
                        p = pp[t % 4]
                        lt = (t + 2) * 128

                        def mmq(e, p=p, wb=wb, lt=lt):
                            for j in range(16):
                                ins = e.matmul(p[:, 0:256], lhsT=hL[:, j, lt:lt + 128], rhs=wb[:, j, :], start=(j == 0), stop=(j == 15))
                            return ins
                        S.op('pe', mmq, reads=[hL, wb], writes=[p])
                        c_ = csq[t % 2]
                        S.dma(c_[:, :], rope_own[t * 128:(t + 1) * 128, :], c_, reads=[rope_own], writes=[c_])
                        t1, t2, qq = qf1[t % 2], qf2[t % 2], q16[t % 2]
                        S.op('dve', lambda e, p=p, t1=t1, c_=c_: e.tensor_tensor(
                            out=t1.t[:, :].rearrange("p (g d) -> p g d", g=4), in0=p.t[:, 0:256].rearrange("p (g d) -> p g d", g=4),
                            in1=bc(c_[:, 0:64], 1, [128, 4, 64]), op=ALU.mult), reads=[p, c_], writes=[t1])
                        for ab in range(2):
                            S.op('dve', lambda e, p=p, t2=t2, c_=c_, ab=ab: e.tensor_tensor(
                                out=t2.t[:, :].rearrange("p (g r a d) -> p g r a d", g=4, r=2, a=2)[:, :, :, ab, :],
                                in0=p.t[:, 0:256].rearrange("p (g r a d) -> p g r a d", g=4, r=2, a=2)[:, :, :, 1 - ab, :],
                                in1=bc(c_.t[:, 64:128].rearrange("p (r a d) -> p r a d", r=2, a=2)[:, :, ab, :], 1, [128, 4, 2, 16]),
                                op=ALU.mult), reads=[p, c_], writes=[t2])
                        S.op('pool', lambda e, t1=t1, t2=t2, qq=qq: e.tensor_tensor(out=qq[:, :], in0=t1[:, :], in1=t2[:, :], op=ALU.add),
                             reads=[t1, t2], writes=[qq])

                        def tr_stage(t_, qq_, sA=sA):
                            def trq(e):
                                for hh in range(2):
                                    ins = e.transpose(out=ptq[:, hh * 128:(hh + 1) * 128], in_=qq_[:, hh * 128:(hh + 1) * 128], identity=ident[:, :])
                                return ins
                            S.op('pe', trq, reads=[qq_, ident], writes=[ptq])
                            S.op('act', lambda e: e.copy(out=sA[:, :, t_ * 128:(t_ + 1) * 128],
                                                         in_=ptq.t[:, 0:256].rearrange("p (h t) -> p h t", h=2)),
                                 reads=[ptq], writes=[sA])
                        if t >= 1:
                            tr_stage(t - 1, q16[(t - 1) % 2])
                        if t == 15:
                            tr_stage(15, qq)
                    S.dma(QaT_d.t[2 * b:2 * b + 2, :, :].rearrange("h p t -> p h t"), sA[:, :, 0:OWN], sA, reads=[sA], writes=[QaT_d])
                elif kind in ('qb', 'kb', 'g'):
                    if kind == 'kb':
                        groups = [(g * 512, 512) for g in range(5)] + [(2560, 256)]
                    else:
                        groups = [(256 + g * 512, 512) for g in range(4)]
                    for cc in range(2):
                        for gi, (l0, n) in enumerate(groups):
                            p = pp[kctr % 4]
                            kctr += 1

                            def mmf(e, p=p, wb=wb, cc=cc, l0=l0, n=n):
                                for j in range(16):
                                    ins = e.matmul(p[:, 0:n], lhsT=wb[:, j, cc * 128:(cc + 1) * 128], rhs=hL[:, j, l0:l0 + n], start=(j == 0), stop=(j == 15))
                                return ins
                            S.op('pe', mmf, reads=[hL, wb], writes=[p])
                            o0 = l0 if kind == 'kb' else l0 - 256
                            if kind == 'g':
                                S.op('act', lambda e, p=p, sA=sA, cc=cc, o0=o0, n=n: e.activation(out=sA[:, cc, o0:o0 + n], in_=p[:, 0:n], func=AF.Sigmoid),
                                     reads=[p], writes=[sA])
                            elif kind == 'qb':
                                S.op('act', lambda e, p=p, sA=sA, cc=cc, o0=o0, n=n: e.activation(out=sA[:, cc, o0:o0 + n], in_=p[:, 0:n], func=AF.Copy, scale=128.0 ** -0.5),
                                     reads=[p], writes=[sA])
                            else:
                                S.op('act', lambda e, p=p, sA=sA, cc=cc, o0=o0, n=n: e.copy(out=sA[:, cc, o0:o0 + n], in_=p[:, 0:n]),
                                     reads=[p], writes=[sA])
                    if kind == 'qb':
                        S.dma(QbT_d.t[2 * b:2 * b + 2, :, :].rearrange("h p t -> p h t"), sA[:, :, 0:OWN], sA, reads=[sA], writes=[QbT_d])
                    elif kind == 'kb':
                        S.dma(KbT_d.t[2 * b:2 * b + 2, :, :].rearrange("h p t -> p h t"), sA[:, :, 0:NLOC], sA, reads=[sA], writes=[KbT_d])
                    else:
                        S.dma(GT_d.t[2 * b:2 * b + 2, :, :].rearrange("h p t -> p h t"), sA[:, :, 0:OWN], sA, reads=[sA], writes=[GT_d])
                else:
                    for t in range(22):
                        p = pp[t % 4]

                        def mmv(e, p=p, wb=wb, t=t):
                            for j in range(16):
                                ins = e.matmul(p[:, 0:256], lhsT=hL[:, j, t * 128:(t + 1) * 128], rhs=wb[:, j, :], start=(j == 0), stop=(j == 15))
                            return ins
                        S.op('pe', mmv, reads=[hL, wb], writes=[p])
                        vv = v16[t % 3]
                        S.op('act', lambda e, p=p, vv=vv: e.copy(out=vv[:, :], in_=p[:, 0:256]), reads=[p], writes=[vv])
                        S.dma(Vb_d[t * 128:(t + 1) * 128, b * 256:(b + 1) * 256], vv[:, :], vv, reads=[vv], writes=[Vb_d])
            S.end_phase()
            inner1b.__exit__(None, None, None)

        with phase_begin():
            kT = [S.sb("kT%d" % i, [128, NKEY], BF16) for i in range(2)]
            vA = [S.sb("vA%d" % i, [128, 130, 129], BF16) for i in range(2)]
            qA = [S.sb("qA%d" % i, [128, OWN], BF16) for i in range(2)]
            for i in range(2):
                S.op('pool', lambda e, i=i: e.memset(vA[i][:, :, 128:129], 1.0), writes=[vA[i]])
            pS = [S.ps("pS%d" % i, [128, 2, 512], F32) for i in range(2)]
            pO = [S.ps("pO%d" % i, [128, 512], F32) for i in range(3)]
            pT_ = [S.sb("pT%d" % i, [128, 2, 512], BF16) for i in range(3)]
            ptA = S.ps("ptA", [128, 1024], BF16)
            o0 = S.sb("o0", [128, 128], F32)
            accS = [S.sb("accS%d" % i, [128, 387], F32) for i in range(3)]
            dd = [S.sb("dd%d" % i, [128, 128], F32) for i in range(2)]
            rz = [S.sb("rz%d" % i, [128, 2], F32) for i in range(2)]
            jk = S.sb("jk", [128, 128], F32)
            s2 = [S.sb("s2_%d" % i, [128, 1], F32) for i in range(2)]
            sd = [S.sb("sd_%d" % i, [128, 1], F32) for i in range(2)]
            rs = [S.sb("rs_%d" % i, [128, 1], F32) for i in range(2)]
            oa16 = [S.sb("oa16_%d" % i, [128, 128], BF16) for i in range(2)]
            oaS = [S.sb("oaS%d" % i, [128, OWN], BF16) for i in range(2)]
            NKC = NKEY // 128

            def acc(mi, qt):
                a_ = mi * 4 + qt
                return pO[a_ // 3], a_ % 3
            uf = S.sb("uf", [128, D], F32)
            ub = S.sb("ub", [128, D], BF16)
            us = S.sb("us", [128, D], BF16)
            vf = S.sb("vf", [128, D], F32)
            vb2 = S.sb("vb2", [128, D], BF16)

            def puv_gen():
                for c in range(128):
                    S.dma(uf[:, :], peer_u[c * 128:(c + 1) * 128, :], uf, reads=[peer_u], writes=[uf])
                    S.dma(vf[:, :], peer_v[c * 128:(c + 1) * 128, :], vf, reads=[peer_v], writes=[vf])
                    yield
                    yield
                    S.op('dve', lambda e: e.tensor_copy(out=ub[:, :], in_=uf[:, :]), reads=[uf], writes=[ub])
                    S.op('dve', lambda e: e.tensor_copy(out=vb2[:, :], in_=vf[:, :]), reads=[vf], writes=[vb2])
                    yield
                    yield
                    for half in range(2):
                        def tru(e, half=half):
                            for jj in range(8):
                                j = half * 8 + jj
                                ins = e.transpose(out=ptA[:, jj * 128:(jj + 1) * 128], in_=ub[:, j * 128:(j + 1) * 128], identity=ident[:, :])
                            return ins
                        S.op('pe', tru, reads=[ub, ident], writes=[ptA])
                        yield
                        S.op('dve', lambda e, half=half: e.tensor_copy(out=us[:, half * 1024:(half + 1) * 1024], in_=ptA[:, :]),
                             reads=[ptA], writes=[us])
                        yield
                    S.dma(UT_d[c, :, :], us[:, :], us, reads=[us], writes=[UT_d])
                    S.dma(VB_d[c * 128:(c + 1) * 128, :], vb2[:, :], vb2, reads=[vb2], writes=[VB_d])
            puv = puv_gen()

            def head_loads(h):
                if h >= 8:
                    return
                k_, v_, q_ = kT[h % 2], vA[h % 2], qA[h % 2]
                S.dma(k_[:, :], KaT_d[h, :, :], k_, reads=[KaT_d], writes=[k_])
                for part in range(2):
                    c0, c1 = part * 65, (part + 1) * 65
                    S.dma(v_[:, c0:c1, 0:128], Va_d.t[c0 * 128:c1 * 128, h * 128:(h + 1) * 128].rearrange("(c p) e -> p c e", p=128),
                          v_, reads=[Va_d], writes=[v_])
                S.dma(q_[:, :], QaT_d[h, :, :], q_, reads=[QaT_d], writes=[q_])
            ctr = 0
            head_loads(0)
            for h in range(8):
                k_, v_, q_ = kT[h % 2], vA[h % 2], qA[h % 2]
                head_loads(h + 1)
                oS = oaS[h % 2]
                for qg in range(4):
                    def s_op(kc, k_=k_, q_=q_, qg=qg):
                        p = pS[kc % 2]

                        def f(e):
                            for mi in range(2):
                                ins = e.matmul(p[:, mi, :], lhsT=k_[64 * mi:64 * mi + 64, kc * 128:(kc + 1) * 128],
                                               rhs=q_[64 * mi:64 * mi + 64, qg * 512:(qg + 1) * 512], start=True, stop=True)
                            return ins
                        S.op('pe', f, reads=[k_, q_], writes=[p])
                    def pv_op(kc, v_=v_):
                        pt = pT_[kc % 3]

                        def pv(e):
                            for mi in range(2):
                                for qt in range(4):
                                    ab, ai = acc(mi, qt)
                                    ins = e.matmul(ab[:, ai * 129:ai * 129 + 129], lhsT=pt[:, mi, qt * 128:(qt + 1) * 128], rhs=v_[:, kc, :],
                                                   start=(kc == 0 and ai == 0), stop=(kc == NKC - 1), skip_group_check=True)
                            return ins
                        S.op('pe', pv, reads=[pt, v_], writes=pO)
                    s_op(0)
                    for kc in range(NKC):
                        p = pS[kc % 2]
                        pt = pT_[kc % 3]
                        S.op('act', lambda e, p=p, pt=pt: e.activation(out=pt[:, :, :], in_=p[:, :, :], func=AF.Exp), reads=[p], writes=[pt])
                        if kc + 1 < NKC:
                            s_op(kc + 1)
                        if kc >= 1:
                            pv_op(kc - 1)
                        if kc % 4 == 3:
                            next(puv, None)
                    pv_op(NKC - 1)
                    for b_ in range(3):
                        S.op('dve', lambda e, b_=b_: e.tensor_copy(out=accS[b_][:, :], in_=pO[b_][:, 0:387]), reads=[pO[b_]], writes=[accS[b_]])
                    for qt in range(4):
                        ctr += 1
                        r_, d_ = rz[ctr % 2], dd[ctr % 2]
                        a0, i0 = accS[(0 * 4 + qt) // 3], (0 * 4 + qt) % 3
                        a1, i1 = accS[(1 * 4 + qt) // 3], (1 * 4 + qt) % 3
                        S.op('dve', lambda e, a0=a0, i0=i0, r_=r_: e.reciprocal(out=r_[:, 0:1], in_=a0[:, i0 * 129 + 128:i0 * 129 + 129]), reads=[a0], writes=[r_])
                        S.op('dve', lambda e, a1=a1, i1=i1, r_=r_: e.reciprocal(out=r_[:, 1:2], in_=a1[:, i1 * 129 + 128:i1 * 129 + 129]), reads=[a1, r_], writes=[r_])
                        S.op('dve', lambda e, a0=a0, i0=i0, r_=r_: e.tensor_scalar(out=o0[:, :], in0=a0[:, i0 * 129:i0 * 129 + 128], scalar1=r_[:, 0:1], scalar2=None, op0=ALU.mult),
                             reads=[a0, r_], writes=[o0])
                        S.op('dve', lambda e, a1=a1, i1=i1, r_=r_, d_=d_: e.tensor_scalar(out=d_[:, :], in0=a1[:, i1 * 129:i1 * 129 + 128], scalar1=r_[:, 1:2], scalar2=lamv[:, 1:2], op0=ALU.mult, op1=ALU.mult),
                             reads=[a1, r_, lamv], writes=[d_])
                        S.op('dve', lambda e, d_=d_: e.tensor_tensor(out=d_[:, :], in0=d_[:, :], in1=o0[:, :], op=ALU.add),
                             reads=[d_, o0], writes=[d_])
                        a_, b_, c_ = s2[ctr % 2], sd[ctr % 2], rs[ctr % 2]
                        S.op('act', lambda e, d_=d_, a_=a_: e.activation(out=jk[:, :], in_=d_[:, :], func=AF.Square, accum_out=a_[:, :]),
                             reads=[d_], writes=[jk, a_])
                        S.op('act', lambda e, a_=a_, b_=b_: e.activation(out=b_[:, :], in_=a_[:, :], func=AF.Sqrt, scale=1.0 / 128, bias=EPS),
                             reads=[a_], writes=[b_])
                        S.op('dve', lambda e, b_=b_, c_=c_: e.reciprocal(out=c_[:, :], in_=b_[:, :]), reads=[b_], writes=[c_])
                        o16 = oa16[ctr % 2]
                        S.op('dve', lambda e, d_=d_, c_=c_, o16=o16: e.scalar_tensor_tensor(out=o16[:, :], in0=d_[:, :], scalar=c_[:, 0:1], in1=gsub[:, :], op0=ALU.mult, op1=ALU.mult),
                             reads=[d_, c_, gsub], writes=[o16])
                        S.op('pe', lambda e, o16=o16: e.transpose(out=ptA[:, 0:128], in_=o16[:, :], identity=ident[:, :]), reads=[o16, ident], writes=[ptA])
                        tt = qg * 4 + qt
                        S.op('act', lambda e, oS=oS, tt=tt: e.copy(out=oS[:, tt * 128:(tt + 1) * 128], in_=ptA[:, 0:128]), reads=[ptA], writes=[oS])
                S.dma(OaT_d[h, :, :], oS[:, :], oS, reads=[oS], writes=[OaT_d])
            for _ in puv:
                pass
            S.end_phase()

        with phase_begin():
            kB = [S.sb("kB%d" % i, [128, NLOC], BF16) for i in range(2)]
            vB = [S.sb("vB%d" % i, [128, 22, 129], BF16) for i in range(2)]
            qB = [S.sb("qB%d" % i, [128, OWN], BF16) for i in range(2)]
            bia = [S.sb("bia%d" % i, [128, 5, 5, 128], F32) for i in range(2)]
            for i in range(2):
                S.op('pool', lambda e, i=i: e.memset(vB[i][:, :, 128:129], 1.0), writes=[vB[i]])
            pN = [S.ps("pN%d" % i, [128, 8, 128], F32) for i in range(2)]
            pNo = [S.ps("pNo%d" % i, [128, 512], F32) for i in range(2)]
            ptB = S.ps("ptB", [128, 1024], BF16)
            sN = [S.sb("sN%d" % i, [128, 5, 128], F32) for i in range(2)]
            pTn = [S.sb("pTn%d" % i, [128, 7, 128], BF16) for i in range(2)]
            rzb = [S.sb("rzb%d" % i, [128, 1], F32) for i in range(2)]
            ob16 = [S.sb("ob16_%d" % i, [128, 128], BF16) for i in range(2)]
            obS = [S.sb("obS%d" % i, [128, OWN], BF16) for i in range(2)]
            def na_loads(h):
                if h >= 8:
                    return
                k_, v_, q_, bi_ = kB[h % 2], vB[h % 2], qB[h % 2], bia[h % 2]
                S.dma(k_[:, :], KbT_d[h, :, :], k_, reads=[KbT_d], writes=[k_])
                S.dma(v_[:, :, 0:128], Vb_d.t[:, h * 128:(h + 1) * 128].rearrange("(c p) e -> p c e", p=128), v_, reads=[Vb_d], writes=[v_])
                S.dma(q_[:, :], QbT_d[h, :, :], q_, reads=[QbT_d], writes=[q_])
                S.dma(bi_.t[:, :, :, :].rearrange("p a b q -> p (a b q)"), na_bias[h, :, :], bi_, reads=[na_bias], writes=[bi_])
            na_loads(0)
            for h in range(8):
                k_, v_, q_, bi_ = kB[h % 2], vB[h % 2], qB[h % 2], bia[h % 2]
                na_loads(h + 1)
                oS = obS[h % 2]

                def s_stage(j, k_=k_, q_=q_):
                    p = pN[j % 2]

                    def mms(e):
                        for c in range(7):
                            k0 = 128 * j + 128 * c if c < 5 else 2560 + 128 * (c - 5)
                            ins = e.matmul(p[:, c, :], lhsT=k_[:, k0:k0 + 128], rhs=q_[:, j * 128:(j + 1) * 128], start=True, stop=True)
                        return ins
                    S.op('pe', mms, reads=[k_, q_], writes=[p])

                def mid_stage(j, bi_=bi_):
                    slot = 0 if j == 0 else 1 if j == 1 else 3 if j == 14 else 4 if j == 15 else 2
                    p, s_, pt = pN[j % 2], sN[j % 2], pTn[j % 2]
                    S.op('dve', lambda e: e.tensor_tensor(out=s_[:, :, :], in0=p[:, 0:5, :], in1=bi_[:, slot, :, :], op=ALU.add),
                         reads=[p, bi_], writes=[s_])
                    S.op('act', lambda e: e.activation(out=pt[:, 0:5, :], in_=s_[:, :, :], func=AF.Exp), reads=[s_], writes=[pt])
                    S.op('act', lambda e: e.activation(out=pt[:, 5:7, :], in_=p[:, 5:7, :], func=AF.Exp), reads=[p], writes=[pt])

                def o_stage(j, v_=v_):
                    pt, po = pTn[j % 2], pNo[j % 2]

                    def mmo(e):
                        for c in range(7):
                            tile = j + c if c < 5 else 20 + (c - 5)
                            ins = e.matmul(po[:, 0:129], lhsT=pt[:, c, :], rhs=v_[:, tile, :], start=(c == 0), stop=(c == 6))
                        return ins
                    S.op('pe', mmo, reads=[pt, v_], writes=[po])
                    r_, o16 = rzb[j % 2], ob16[j % 2]
                    S.op('dve', lambda e: e.reciprocal(out=r_[:, :], in_=po[:, 128:129]), reads=[po], writes=[r_])
                    S.op('dve', lambda e: e.tensor_scalar(out=o16[:, :], in0=po[:, 0:128], scalar1=r_[:, 0:1], scalar2=None, op0=ALU.mult),
                         reads=[po, r_], writes=[o16])

                def t_stage(j, oS=oS):
                    o16 = ob16[j % 2]
                    S.op('pe', lambda e: e.transpose(out=ptB[:, 0:128], in_=o16[:, :], identity=ident[:, :]), reads=[o16, ident], writes=[ptB])
                    S.op('act', lambda e: e.copy(out=oS[:, j * 128:(j + 1) * 128], in_=ptB[:, 0:128]), reads=[ptB], writes=[oS])
                s_stage(0)
                for j in range(16):
                    if j + 1 < 16:
                        s_stage(j + 1)
                    mid_stage(j)
                    o_stage(j)
                    if j >= 1:
                        t_stage(j - 1)
                t_stage(15)
                S.dma(ObT_d[h, :, :], oS[:, :], oS, reads=[oS], writes=[ObT_d])
            S.end_phase()

        def load_w_bf16(dst, src, nrow_chunks, stgs, ncol=2048, cw=256):
            k = 0
            for c0 in range(0, ncol, cw):
                st = stgs[k % 2]
                S.dma(st[:, 0:nrow_chunks, :], src.t[:, c0:c0 + cw].rearrange("(j p) n -> p j n", p=128), st, reads=[src], writes=[st])
                (S.op('act', lambda e, st=st, c0=c0: e.copy(out=dst[:, :, c0:c0 + cw], in_=st[:, 0:nrow_chunks, :]), reads=[st], writes=[dst]) if k % 2 else
                 S.op('dve', lambda e, st=st, c0=c0: e.tensor_copy(out=dst[:, :, c0:c0 + cw], in_=st[:, 0:nrow_chunks, :]), reads=[st], writes=[dst]))
                k += 1

        with phase_begin():
            wa = S.sb("wa", [128, 8, 2048], BF16)
            wb_ = S.sb("wb_", [128, 8, 2048], BF16)
            stg4 = [S.sb("stg4_%d" % i, [128, 16, 256], F32) for i in range(2)]
            load_w_bf16(wa, w_a, 8, stg4)
            load_w_bf16(wb_, w_b, 8, stg4)
            oa = [S.sb("oa%d" % i, [128, 8, 512], BF16) for i in range(2)]
            ob = [S.sb("ob%d" % i, [128, 8, 512], BF16) for i in range(2)]
            ga = [S.sb("ga%d" % i, [128, 512], BF16) for i in range(2)]
            gb = [S.sb("gb%d" % i, [128, 512], BF16) for i in range(2)]
            pa = [S.ps("pa%d" % i, [128, 512], F32) for i in range(2)]
            pb = [S.ps("pb%d" % i, [128, 512], F32) for i in range(2)]
            t1 = [S.sb("m1_%d" % i, [128, 512], F32) for i in range(2)]
            t2 = [S.sb("m2_%d" % i, [128, 512], F32) for i in range(2)]
            mS = [S.sb("mS%d" % i, [128, 16, 512], BF16) for i in range(2)]
            k = 0
            for tg in range(4):
                oa_, ob_, ms = oa[tg % 2], ob[tg % 2], mS[tg % 2]
                S.dma(oa_[:, :, :], OaT_d.t[:, :, tg * 512:(tg + 1) * 512].rearrange("h p t -> p h t"), oa_, reads=[OaT_d], writes=[oa_])
                S.dma(ob_[:, :, :], ObT_d.t[:, :, tg * 512:(tg + 1) * 512].rearrange("h p t -> p h t"), ob_, reads=[ObT_d], writes=[ob_])
                for fc in range(16):
                    k += 1
                    ga_, gb_, pa_, pb_, t1_, t2_ = ga[k % 2], gb[k % 2], pa[k % 2], pb[k % 2], t1[k % 2], t2[k % 2]
                    S.dma(ga_[:, :], GT_d[fc, :, tg * 512:(tg + 1) * 512], ga_, reads=[GT_d], writes=[ga_])
                    S.dma(gb_[:, :], GT_d[16 + fc, :, tg * 512:(tg + 1) * 512], gb_, reads=[GT_d], writes=[gb_])

                    def mma(e, pa_=pa_, oa_=oa_, fc=fc):
                        for hh in range(8):
                            ins = e.matmul(pa_[:, :], lhsT=wa[:, hh, fc * 128:(fc + 1) * 128], rhs=oa_[:, hh, :], start=(hh == 0), stop=(hh == 7))
                        return ins

                    def mmb(e, pb_=pb_, ob_=ob_, fc=fc):
                        for hh in range(8):
                            ins = e.matmul(pb_[:, :], lhsT=wb_[:, hh, fc * 128:(fc + 1) * 128], rhs=ob_[:, hh, :], start=(hh == 0), stop=(hh == 7))
                        return ins
                    S.op('pe', mma, reads=[wa, oa_], writes=[pa_])
                    S.op('pe', mmb, reads=[wb_, ob_], writes=[pb_])
                    S.op('dve', lambda e, pa_=pa_, ga_=ga_, t1_=t1_: e.tensor_tensor(out=t1_[:, :], in0=pa_[:, :], in1=ga_[:, :], op=ALU.mult), reads=[pa_, ga_], writes=[t1_])
                    S.op('dve', lambda e, pb_=pb_, gb_=gb_, t2_=t2_: e.tensor_tensor(out=t2_[:, :], in0=pb_[:, :], in1=gb_[:, :], op=ALU.mult), reads=[pb_, gb_], writes=[t2_])
                    S.op('pool', lambda e, t1_=t1_, t2_=t2_, ms=ms, fc=fc: e.tensor_tensor(out=ms[:, fc, :], in0=t1_[:, :], in1=t2_[:, :], op=ALU.add), reads=[t1_, t2_], writes=[ms])
                S.dma(mT_d[:, :, tg * 512:(tg + 1) * 512], ms[:, :, :], ms, reads=[ms], writes=[mT_d])
            S.end_phase()

        with phase_begin():
            wo = S.sb("wo", [128, 16, 2048], BF16)
            stg5 = [S.sb("stg5_%d" % i, [128, 16, 256], F32) for i in range(2)]
            load_w_bf16(wo, w_o, 16, stg5)
            gt1 = S.sb("gt1", [128, D], F32)
            S.dma(gt1[:, :], mrow.t[0, 2 * D:3 * D].partition_broadcast(128), gt1, reads=[mrow], writes=[gt1])
            mt = [S.sb("mt%d" % i, [128, 16, 512], BF16) for i in range(2)]
            xo = [S.sb("xo%d" % i, [128, D], F32) for i in range(2)]
            x1 = [S.sb("x1_%d" % i, [128, D], F32) for i in range(2)]
            py = [S.ps("py%d" % i, [128, 512], F32) for i in range(4)]
            for tg in range(4):
                m_ = mt[tg % 2]
                S.dma(m_[:, :, :], mT_d[:, :, tg * 512:(tg + 1) * 512], m_, reads=[mT_d], writes=[m_])
                for tt in range(4):
                    t = tg * 4 + tt
                    xo_, x1_ = xo[t % 2], x1[t % 2]
                    S.dma(xo_[:, :], x_own[t * 128:(t + 1) * 128, :], xo_, reads=[x_own], writes=[xo_])
                    for cb in range(4):
                        def mmy(e, m_=m_, tt=tt, cb=cb):
                            for j in range(16):
                                ins = e.matmul(py[cb][:, :], lhsT=m_[:, j, tt * 128:(tt + 1) * 128], rhs=wo[:, j, cb * 512:(cb + 1) * 512], start=(j == 0), stop=(j == 15))
                            return ins
                        S.op('pe', mmy, reads=[m_, wo], writes=[py[cb]])
                        S.op('dve', lambda e, cb=cb, x1_=x1_: e.tensor_tensor(out=x1_[:, cb * 512:(cb + 1) * 512], in0=py[cb][:, :], in1=gt1[:, cb * 512:(cb + 1) * 512], op=ALU.mult),
                             reads=[py[cb], gt1], writes=[x1_])
                    S.op('pool', lambda e, x1_=x1_, xo_=xo_: e.tensor_tensor(out=x1_[:, :], in0=x1_[:, :], in1=xo_[:, :], op=ALU.add), reads=[x1_, xo_], writes=[x1_])
                    S.dma(X1_d[t * 128:(t + 1) * 128, :], x1_[:, :], x1_, reads=[x1_], writes=[X1_d])
            S.end_phase()

        rt1 = S.sb("rt1", [128, 16, 128], F32, True)
        rt2 = S.sb("rt2", [128, 16, 128], F32, True)
        rtg = S.sb("rtg", [128, 16, 128], F32, True)
        hn_d = dscr("hn_d", [128, 16, OWN], BF16)
        QT_d = dscr("QT_d", [16, 128, OWN], BF16)
        with phase_begin():
            R = make_norm(2)
            hst = [S.sb("hst%d" % i, [128, 16, 512], BF16) for i in range(2)]
            for t in range(16):
                hs = hst[(t // 4) % 2]
                norm_tile(R, X1_d[t * 128:(t + 1) * 128, :], X1_d, 4, 5, hs[:, :, (t % 4) * 128:(t % 4 + 1) * 128], hs)
                if t % 4 == 3:
                    tg = t // 4
                    S.dma(hn_d[:, :, tg * 512:(tg + 1) * 512], hs[:, :, :], hs, reads=[hs], writes=[hn_d])
            S.end_phase()
        with phase_begin():
            hnA = S.sb("hnA", [128, 16, OWN], BF16)
            for tg in range(4):
                S.dma(hnA[:, :, tg * 512:(tg + 1) * 512], hn_d[:, :, tg * 512:(tg + 1) * 512], hnA, reads=[hn_d], writes=[hnA])
            stg6 = [S.sb("stg6_%d" % i, [128, 16, 128], F32) for i in range(2)]
            wqb = [S.sb("wqb%d" % i, [128, 16, 128], BF16) for i in range(2)]
            qst = [S.sb("qst%d" % i, [128, OWN], BF16) for i in range(2)]
            pq = [S.ps("pq%d" % i, [128, 512], F32) for i in range(2)]
            def ldq(cq):
                if cq >= 16:
                    return
                st, wb = stg6[cq % 2], wqb[cq % 2]
                S.dma(st[:, :, :], w_q.t[:, cq * 128:(cq + 1) * 128].rearrange("(j p) n -> p j n", p=128), st, reads=[w_q], writes=[st])
                S.op('dve', lambda e: e.tensor_copy(out=wb[:, :, :], in_=st[:, :, :]), reads=[st], writes=[wb])
            ldq(0)
            for cq in range(16):
                st, wb = stg6[cq % 2], wqb[cq % 2]
                ldq(cq + 1)
                qs = qst[cq % 2]
                for tg in range(4):
                    p = pq[tg % 2]

                    def mmq2(e, p=p, wb=wb, tg=tg):
                        for j in range(16):
                            ins = e.matmul(p[:, :], lhsT=wb[:, j, :], rhs=hnA[:, j, tg * 512:(tg + 1) * 512], start=(j == 0), stop=(j == 15))
                        return ins
                    S.op('pe', mmq2, reads=[wb, hnA], writes=[p])
                    S.op('act', lambda e, p=p, qs=qs, tg=tg: e.copy(out=qs[:, tg * 512:(tg + 1) * 512], in_=p[:, :]), reads=[p], writes=[qs])
                S.dma(QT_d[cq, :, :], qs[:, :], qs, reads=[qs], writes=[QT_d])
            S.end_phase()
        with phase_begin():
            sbf = S.sb("sbf", [128, 2048], F32)
            sbk = S.sb("sbk", [128, 16, 128], BF16)
            S.dma(sbf[:, :], subT[:, :], sbf, reads=[subT], writes=[sbf])
            S.op('dve', lambda e: e.tensor_copy(out=sbk.t[:, :, :].rearrange("p a b -> p (a b)"), in_=sbf[:, :]), reads=[sbf], writes=[sbk])
            qT = [S.sb("qT%d" % i, [128, 16, 512], BF16) for i in range(2)]
            psc = [S.ps("psc%d" % i, [128, 4, 128], F32) for i in range(4)]
            ptr_ = S.ps("ptr_", [128, 3, 128], F32)
            s_sb = S.sb("s_sb", [128, 16, 128], F32)
            wk = S.sb("wk", [128, 16, 128], F32)
            top = S.sb("top", [128, 16, 16], F32)
            idx = S.sb("idx", [128, 16, 16], U32)
            idf = S.sb("idf", [128, 16, 16], F32)
            cand = S.sb("cand", [128, 8, 256], F32)
            cw = S.sb("cw", [128, 8, 256], F32)
            best = S.sb("best", [128, 8, 16], F32)
            pos = S.sb("pos", [128, 8, 16], U32)
            pa_u = S.sb("pa_u", [128, 8, 16], U32)
            pb_u = S.sb("pb_u", [128, 8, 16], U32)
            paf = S.sb("paf", [128, 8, 16], F32)
            pbf = S.sb("pbf", [128, 8, 16], F32)
            nb = S.sb("nb", [128, 8], F32)
            ex = S.sb("ex", [128, 8, 16], F32)
            zz = S.sb("zz", [128, 8], F32)
            rzz = S.sb("rzz", [128, 8], F32)
            gg = S.sb("gg", [128, 8, 16], F32)
            oh = S.sb("oh", [128, 8, 16, 16], F32)
            i1f = S.sb("i1f", [128, 8, 16], F32)
            i2f = S.sb("i2f", [128, 8, 16], F32)

            def views(b_, n):
                return [Buf("%s_v%d" % (b_.name, i), b_.t) for i in range(n)]
            s_v = views(s_sb, 4)
            top_v, idx_v, wk_v = views(top, 16), views(idx, 16), views(wk, 16)
            cand_v, best_v, pos_v, cw_v = views(cand, 8), views(best, 8), views(pos, 8), views(cw, 8)
            ex_v, zz_v = views(ex, 8), views(zz, 8)
            for tg in range(4):
                q_ = qT[tg % 2]
                S.dma(q_[:, :, :], QT_d.t[:, :, tg * 512:(tg + 1) * 512].rearrange("c p t -> p c t"), q_, reads=[QT_d], writes=[q_])
                for tt in range(4):
                    t = tg * 4 + tt
                    for g4 in range(4):
                        def mms2(e, q_=q_, tt=tt, g4=g4):
                            for c in range(4):
                                cq = g4 * 4 + c
                                ins = e.matmul(psc[g4][:, c, :], lhsT=q_[:, cq, tt * 128:(tt + 1) * 128], rhs=sbk[:, cq, :], start=True, stop=True)
                            return ins
                        S.op('pe', mms2, reads=[q_, sbk], writes=[psc[g4]])
                        S.op('act', lambda e, g4=g4: e.copy(out=s_sb[:, g4 * 4:(g4 + 1) * 4, :], in_=psc[g4][:, :, :]), reads=[psc[g4]], writes=[s_v[g4]])
                    for cq in range(16):
                        S.op('dve', lambda e, cq=cq: e.max(out=top[:, cq, 0:8], in_=s_sb[:, cq, :]), reads=[s_v[cq // 4]], writes=[top_v[cq]])
                    for cq in range(16):
                        S.op('dve', lambda e, cq=cq: e.max_index(out=idx[:, cq, 0:8], in_max=top[:, cq, 0:8], in_values=s_sb[:, cq, :]), reads=[s_v[cq // 4], top_v[cq]], writes=[idx_v[cq]])
                    for cq in range(16):
                        S.op('dve', lambda e, cq=cq: e.match_replace(out=wk[:, cq, :], in_to_replace=top[:, cq, 0:8], in_values=s_sb[:, cq, :], imm_value=-1e30), reads=[s_v[cq // 4], top_v[cq]], writes=[wk_v[cq]])
                    for cq in range(16):
                        S.op('dve', lambda e, cq=cq: e.max(out=top[:, cq, 8:16], in_=wk[:, cq, :]), reads=[wk_v[cq]], writes=[top_v[cq]])
                    for cq in range(16):
                        S.op('dve', lambda e, cq=cq: e.max_index(out=idx[:, cq, 8:16], in_max=top[:, cq, 8:16], in_values=wk[:, cq, :]), reads=[wk_v[cq], top_v[cq]], writes=[idx_v[cq]])
                    tv = lambda b_: b_.t[:, :, :].rearrange("p (h two) k -> p h two k", two=2)
                    S.op('dve', lambda e: e.tensor_tensor(out=cand.t[:, :, :].rearrange("p h (a b) -> p h a b", a=16),
                                                          in0=bc(tv(top)[:, :, 0, :], 3, [128, 8, 16, 16]),
                                                          in1=bc(tv(top)[:, :, 1, :], 2, [128, 8, 16, 16]), op=ALU.add), reads=top_v, writes=cand_v)
                    for hh in range(8):
                        S.op('dve', lambda e, hh=hh: e.max(out=best[:, hh, 0:8], in_=cand[:, hh, :]), reads=[cand_v[hh]], writes=[best_v[hh]])
                    for hh in range(8):
                        S.op('dve', lambda e, hh=hh: e.max_index(out=pos[:, hh, 0:8], in_max=best[:, hh, 0:8], in_values=cand[:, hh, :]), reads=[cand_v[hh], best_v[hh]], writes=[pos_v[hh]])
                    for hh in range(8):
                        S.op('dve', lambda e, hh=hh: e.match_replace(out=cw[:, hh, :], in_to_replace=best[:, hh, 0:8], in_values=cand[:, hh, :], imm_value=-1e30), reads=[cand_v[hh], best_v[hh]], writes=[cw_v[hh]])
                    for hh in range(8):
                        S.op('dve', lambda e, hh=hh: e.max(out=best[:, hh, 8:16], in_=cw[:, hh, :]), reads=[cw_v[hh]], writes=[best_v[hh]])
                    for hh in range(8):
                        S.op('dve', lambda e, hh=hh: e.max_index(out=pos[:, hh, 8:16], in_max=best[:, hh, 8:16], in_values=cw[:, hh, :]), reads=[cw_v[hh], best_v[hh]], writes=[pos_v[hh]])
                    S.op('dve', lambda e: e.tensor_scalar(out=nb[:, :], in0=best[:, :, 0], scalar1=-1.0, scalar2=None, op0=ALU.mult), reads=best_v, writes=[nb])
                    for hh in range(8):
                        S.op('act', lambda e, hh=hh: e.activation(out=ex[:, hh, :], in_=best[:, hh, :], func=AF.Exp, bias=nb[:, hh:hh + 1], accum_out=zz[:, hh:hh + 1]),
                             reads=[best_v[hh], nb], writes=[ex_v[hh], zz_v[hh]])
                    S.op('dve', lambda e: e.reciprocal(out=rzz[:, :], in_=zz[:, :]), reads=zz_v, writes=[rzz])
                    S.op('dve', lambda e: e.tensor_tensor(out=gg[:, :, :], in0=ex[:, :, :], in1=bc(rzz[:, :], 2, [128, 8, 16]), op=ALU.mult), reads=ex_v + [rzz], writes=[gg])
                    S.op('dve', lambda e: e.tensor_single_scalar(out=pa_u[:, :, :], in_=pos[:, :, :], scalar=4, op=ALU.logical_shift_right), reads=pos_v, writes=[pa_u])
                    S.op('dve', lambda e: e.tensor_single_scalar(out=pb_u[:, :, :], in_=pos[:, :, :], scalar=15, op=ALU.bitwise_and), reads=pos_v, writes=[pb_u])
                    S.op('dve', lambda e: e.tensor_copy(out=paf[:, :, :], in_=pa_u[:, :, :]), reads=[pa_u], writes=[paf])
                    S.op('dve', lambda e: e.tensor_copy(out=pbf[:, :, :], in_=pb_u[:, :, :]), reads=[pb_u], writes=[pbf])
                    S.op('dve', lambda e: e.tensor_copy(out=idf[:, :, :], in_=idx[:, :, :]), reads=idx_v, writes=[idf])
                    for (sel, two, dst) in ((paf, 0, i1f), (pbf, 1, i2f)):
                        S.op('dve', lambda e, sel=sel: e.tensor_tensor(out=oh[:, :, :, :], in0=bc(bc(iota_f[:, 0:16], 1, [128, 16, 16]), 1, [128, 8, 16, 16]),
                                                                       in1=bc(sel[:, :, :], 3, [128, 8, 16, 16]), op=ALU.is_equal), reads=[sel, iota_f], writes=[oh])
                        S.op('dve', lambda e, two=two: e.tensor_tensor(out=oh[:, :, :, :], in0=oh[:, :, :, :],
                                                                       in1=bc(tv(idf)[:, :, two, :], 2, [128, 8, 16, 16]), op=ALU.mult), reads=[oh, idf], writes=[oh])
                        S.op('dve', lambda e, dst=dst: e.tensor_reduce(out=dst[:, :, :], in_=oh[:, :, :, :], axis=AX.X, op=ALU.add), reads=[oh], writes=[dst])

                    def trr(e):
                        e.transpose(out=ptr_[:, 0, :], in_=i1f.t[:, :, :].rearrange("p h k -> p (h k)"), identity=identf[:, :])
                        e.transpose(out=ptr_[:, 1, :], in_=i2f.t[:, :, :].rearrange("p h k -> p (h k)"), identity=identf[:, :])
                        return e.transpose(out=ptr_[:, 2, :], in_=gg.t[:, :, :].rearrange("p h k -> p (h k)"), identity=identf[:, :])
                    S.op('pe', trr, reads=[i1f, i2f, gg, identf], writes=[ptr_])
                    S.op('act', lambda e, t=t: e.copy(out=rt1[:, t, :], in_=ptr_[:, 0, :]), reads=[ptr_], writes=[rt1])
                    S.op('act', lambda e, t=t: e.copy(out=rt2[:, t, :], in_=ptr_[:, 1, :]), reads=[ptr_], writes=[rt2])
                    S.op('act', lambda e, t=t: e.copy(out=rtg[:, t, :], in_=ptr_[:, 2, :]), reads=[ptr_], writes=[rtg])
            S.end_phase()

        WTp = S.sb("WTp", [128, 256, 128], BF16, True)
        PT_d = dscr("PT_d", [16, 128, 8 * 256], BF16)
        kkc = [0]

        def wb_dve(p, sbi, Aoh, Boh, part=None):
            t = 2 * p + sbi // 8
            n0 = (sbi % 8) * 16
            A_, B_ = Aoh[sbi % 2], Boh[sbi % 2]
            if part in (None, 0):
                S.op('dve', lambda e: e.tensor_tensor(out=A_[:, :, :], in0=bc(iota_f[:, :], 1, [128, 16, 128]),
                                                      in1=bc(rt1[:, t, n0:n0 + 16], 2, [128, 16, 128]), op=ALU.is_equal),
                     reads=[iota_f, rt1], writes=[A_])
            if part in (None, 1):
                S.op('dve', lambda e: e.tensor_tensor(out=A_[:, :, :], in0=A_[:, :, :],
                                                      in1=bc(rtg[:, t, n0:n0 + 16], 2, [128, 16, 128]), op=ALU.mult),
                     reads=[A_, rtg], writes=[A_])
            if part in (None, 2):
                S.op('dve', lambda e: e.tensor_tensor(out=B_[:, :, :], in0=bc(iota_f[:, :], 1, [128, 16, 128]),
                                                      in1=bc(rt2[:, t, n0:n0 + 16], 2, [128, 16, 128]), op=ALU.is_equal),
                     reads=[iota_f, rt2], writes=[B_])

        def wb_pe(p, sbi, q4, Aoh, Boh, pW):
            A_, B_ = Aoh[sbi % 2], Boh[sbi % 2]
            kkc[0] += 1
            pw = pW[kkc[0] % 2]

            def mmw(e):
                for n in range(4):
                    ins = e.matmul(pw[:, n, :], lhsT=B_[:, q4 * 4 + n, :], rhs=A_[:, q4 * 4 + n, :], start=True, stop=True)
                return ins
            S.op('pe', mmw, reads=[A_, B_], writes=[pw])
            nn = (sbi // 8) * 128 + (sbi % 8) * 16 + q4 * 4
            S.op('dve', lambda e: e.tensor_copy(out=WTp[:, nn:nn + 4, :], in_=pw[:, :, :]), reads=[pw], writes=[WTp])

        gt2 = S.sb("gt2", [128, D], F32, True)
        fg = S.sb("fg", [128, D], F32, True)
        x1t = S.sb("x1t", [128, D], F32, True)
        xf = [S.sb("xf%d" % i, [128, D], F32, True) for i in range(2)]
        jk2 = S.sb("jk2", [128, D], BF16, True)
        fs = [S.sb("fs%d" % i, [128, 1], F32, True) for i in range(2)]
        fd = [S.sb("fd%d" % i, [128, 1], F32, True) for i in range(2)]
        fr = [S.sb("fr%d" % i, [128, 1], F32, True) for i in range(2)]

        def epi_compute(p, a_):
            t = 2 * p + a_
            xf_ = xf[a_]
            S.dma(x1t[:, :], X1_d[t * 128:(t + 1) * 128, :], x1t, reads=[X1_d], writes=[x1t])
            S.op('pool', lambda e: e.tensor_tensor(out=xf_[:, :], in0=xf_[:, :], in1=x1t[:, :], op=ALU.add), reads=[xf_, x1t], writes=[xf_])
            fa_, fb_, fc_ = fs[a_], fd[a_], fr[a_]
            S.op('act', lambda e: e.activation(out=jk2[:, :], in_=xf_[:, :], func=AF.Square, accum_out=fa_[:, :]), reads=[xf_], writes=[jk2, fa_])
            S.op('act', lambda e: e.activation(out=fb_[:, :], in_=fa_[:, :], func=AF.Sqrt, scale=1.0 / D, bias=EPS), reads=[fa_], writes=[fb_])
            S.op('dve', lambda e: e.reciprocal(out=fc_[:, :], in_=fb_[:, :]), reads=[fb_], writes=[fc_])
            S.op('dve', lambda e: e.scalar_tensor_tensor(out=xf_[:, :], in0=xf_[:, :], scalar=fc_[:, 0:1], in1=fg[:, :], op0=ALU.mult, op1=ALU.mult),
                 reads=[xf_, fc_, fg], writes=[xf_])

        def epi_store(p, a_):
            t = 2 * p + a_
            xf_ = xf[a_]
            S.dma(out[t * 128:(t + 1) * 128, :], xf_[:, :], xf_, reads=[xf_], writes=[out])

        def epilogue(p):
            for a_ in range(2):
                epi_compute(p, a_)
                epi_store(p, a_)

        with phase_begin():
            S.dma(gt2[:, :], mrow.t[0, 5 * D:6 * D].partition_broadcast(128), gt2, reads=[mrow], writes=[gt2])
            S.dma(fg[:, :], final_g.t.partition_broadcast(128), fg, reads=[final_g], writes=[fg])
            Aoh = [S.sb("Aoh%d" % i, [128, 16, 128], BF16) for i in range(2)]
            Boh = [S.sb("Boh%d" % i, [128, 16, 128], BF16) for i in range(2)]
            pW = [S.ps("pW%d" % i, [128, 4, 128], F32) for i in range(2)]
            for sbi in range(16):
                wb_dve(0, sbi, Aoh, Boh)
                for q4 in range(4):
                    wb_pe(0, sbi, q4, Aoh, Boh, pW)
            S.end_phase()

        for p in range(8):
            with phase_begin():
                hnP = S.sb("hnP", [128, 16, 256], BF16)
                S.dma(hnP[:, :, :], hn_d[:, :, p * 256:(p + 1) * 256], hnP, reads=[hn_d], writes=[hnP])
                NB = 4
                ut = [S.sb("ut%d" % i, [128, 16, 128], BF16) for i in range(NB)]
                gl = [S.sb("gl%d" % i, [128, 256], F32) for i in range(2)]
                pst = [S.sb("pst%d" % i, [128, 8, 256], BF16) for i in range(2)]
                pSe = [S.ps("pSe%d" % i, [128, 512], F32) for i in range(2)]

                def ldu(c):
                    if c >= 128:
                        return
                    u_ = ut[c % NB]
                    S.dma(u_.t[:, :, :].rearrange("p j e -> p (j e)"), UT_d[c, :, :], u_, reads=[UT_d], writes=[u_])

                def s_grp(c):
                    u_ = ut[c % NB]
                    ps_ = pSe[c % 2]

                    def mmse(e):
                        for j in range(16):
                            ins = e.matmul(ps_[:, 0:256], lhsT=u_[:, j, :], rhs=hnP[:, j, :], start=(j == 0), stop=(j == 15))
                        return ins
                    S.op('pe', mmse, reads=[u_, hnP], writes=[ps_])
                ldu(0)
                ldu(1)
                ldu(2)
                s_grp(0)
                for c in range(128):
                    if p >= 1:
                        if c == 8:
                            epi_compute(p - 1, 0)
                        if c == 40:
                            epi_store(p - 1, 0)
                            epi_compute(p - 1, 1)
                        if c == 72:
                            epi_store(p - 1, 1)
                    ldu(c + 3)
                    if c + 1 < 128:
                        s_grp(c + 1)
                    ps_ = pSe[c % 2]
                    g_ = gl[c % 2]
                    st_ = pst[(c // 8) % 2]
                    S.op('act', lambda e, ps_=ps_, g_=g_: e.activation(out=g_[:, :], in_=ps_[:, 0:256], func=AF.Gelu_apprx_tanh), reads=[ps_], writes=[g_])
                    S.op('dve', lambda e, g_=g_, st_=st_, c=c: e.tensor_tensor(out=st_[:, c % 8, :], in0=g_[:, :], in1=WTp[:, :, c], op=ALU.mult),
                         reads=[g_, WTp], writes=[st_])
                    if c % 8 == 7:
                        S.dma(PT_d[c // 8, :, :], st_.t[:, :, :].rearrange("p a n -> p (a n)"), st_, reads=[st_], writes=[PT_d])
                S.end_phase()
            with phase_begin():
                ptl = [S.sb("ptl%d" % i, [128, 8, 256], BF16) for i in range(2)]
                vvl = [S.sb("vvl%d" % i, [128, 2, 1024], BF16) for i in range(4)]
                Aoh = [S.sb("Aoh%d" % i, [128, 16, 128], BF16) for i in range(2)]
                Boh = [S.sb("Boh%d" % i, [128, 16, 128], BF16) for i in range(2)]
                pOu = [S.ps("pOu%d" % i, [128, 512], F32) for i in range(4)]
                pW = [S.ps("pW%d" % i, [128, 4, 128], F32) for i in range(2)]

                def ldp(g):
                    S.dma(ptl[g % 2].t[:, :, :].rearrange("p a n -> p (a n)"), PT_d[g % 16, :, :], ptl[g % 2], reads=[PT_d], writes=[ptl[g % 2]])

                def ldv(g, half):
                    if g >= 64:
                        return
                    c0 = g * 2
                    S.dma(vvl[g % 4][:, :, :], VB_d.t[c0 * 128:(c0 + 2) * 128, half * 1024:(half + 1) * 1024].rearrange("(t e) d -> e t d", e=128),
                          vvl[g % 4], reads=[VB_d], writes=[vvl[g % 4]])
                for half in range(2):
                    ldp(half * 16)
                    ldv(0, half)
                    ldv(1, half)
                    ldv(2, half)
                    for c in range(128):
                        gp = half * 16 + c // 8
                        gv = c // 2
                        if c % 8 == 0 and c + 8 < 128:
                            ldp(gp + 1)
                        if c % 2 == 0:
                            ldv(gv + 3, half)
                        pl_, vl_ = ptl[gp % 2], vvl[gv % 4]

                        def mmv2(e, pl_=pl_, vl_=vl_, c=c):
                            for a_ in range(2):
                                for cbh in range(2):
                                    ins = e.matmul(pOu[a_ * 2 + cbh][:, :], lhsT=pl_[:, c % 8, a_ * 128:(a_ + 1) * 128],
                                                   rhs=vl_[:, c % 2, cbh * 512:(cbh + 1) * 512], start=(c == 0), stop=(c == 127))
                            return ins
                        S.op('pe', mmv2, reads=[pl_, vl_], writes=pOu)
                        if p + 1 < 8:
                            s_ = half * 128 + c
                            if s_ == 0:
                                wb_dve(p + 1, 0, Aoh, Boh)
                            if s_ % 16 in (3, 7, 11, 15):
                                wb_pe(p + 1, s_ // 16, (s_ % 16 - 3) // 4, Aoh, Boh, pW)
                            if s_ % 16 in (4, 8, 12) and s_ // 16 + 1 < 16:
                                wb_dve(p + 1, s_ // 16 + 1, Aoh, Boh, part=(s_ % 16) // 4 - 1)
                    for a_ in range(2):
                        for cbh in range(2):
                            col = half * 1024 + cbh * 512
                            S.op('dve', lambda e, a_=a_, cbh=cbh, col=col: e.tensor_tensor(out=xf[a_][:, col:col + 512], in0=pOu[a_ * 2 + cbh][:, :], in1=gt2[:, col:col + 512], op=ALU.mult),
                                 reads=[pOu[a_ * 2 + cbh], gt2], writes=[xf[a_]])
                if p == 7:
                    epilogue(7)
                S.end_phase()
    return nc


def _rope_tables():
    t = np.arange(NTOK)
    row = (t // 64).astype(np.float32)
    col = (t % 64).astype(np.float32)
    freqs = (10000.0 ** (-np.arange(16, dtype=np.float32) / 16)).astype(np.float32)
    ar = row[:, None] * freqs[None, :]
    ac = col[:, None] * freqs[None, :]
    ang = np.concatenate([ar, ar, ac, ac], axis=-1).astype(np.float32)
    cos = np.cos(ang).astype(np.float32)
    sin = np.sin(ang).astype(np.float32)
    sgn = np.concatenate([-np.ones(16), np.ones(16), -np.ones(16), np.ones(16)]).astype(np.float32)
    return np.concatenate([cos, sin * sgn[None, :]], axis=1).astype(np.float32)


def _local_rows(c):
    base = 32 * c - 4
    rows = [base + i for i in range(40)]
    if c == 0:
        rows[0:4] = [6, 7, 8, 9]
    if c == NCORE - 1:
        rows[36:40] = [248, 249, 246, 247]
    return rows


def _na_bias(rpb, c):
    rows = _local_rows(c)
    outb = np.full((8, 5, 5, 128, 128), NEG, np.float32)
    qc = np.arange(64)
    cstart = np.clip(qc - 8, 0, 48)
    for slot, j in enumerate([0, 1, 2, 14, 15]):
        seen = set()
        for kr in range(10):
            gk = rows[2 * j + kr]
            if gk in seen or gk < 0 or gk > 255:
                continue
            seen.add(gk)
            for a in range(2):
                gq = rows[2 * j + 4 + a]
                rs = min(max(gq - 4, 0), 248)
                if not (rs <= gk < rs + 8):
                    continue
                dr = gk - gq + 7
                kc = np.arange(64)
                valid = (kc[:, None] >= cstart[None, :]) & (kc[:, None] < cstart[None, :] + 16)
                dc = kc[:, None] - qc[None, :] + 15
                vals = rpb[:, dr, :][:, np.clip(dc, 0, 30)]
                blockv = np.where(valid[None], vals, NEG).astype(np.float32)
                chunk, kin = (kr * 64) // 128, (kr * 64) % 128
                outb[:, slot, chunk, kin:kin + 64, a * 64:(a + 1) * 64] = blockv
    return np.ascontiguousarray(outb.transpose(0, 3, 1, 2, 4)).reshape(8, 128, 5 * 5 * 128)


def kernel(x, c, ctx, c_ctx, w_mod, b_mod, norm1_g, norm2_g, w_in, lambda_q1, lambda_k1, lambda_q2, lambda_k2,
           subln_g, na_rpb, w_branch_a, w_branch_b, w_out, peer_w_q, peer_subkeys, peer_u, peer_v, final_g):
    f = lambda a: np.ascontiguousarray(np.asarray(a, dtype=np.float32))
    x2 = f(x)[0]
    rope = _rope_tables()
    rope_q = rope * np.float32(0.125)
    cT = np.stack([f(c)[0].reshape(16, 128).T, f(c_ctx).reshape(16, 128).T], axis=-1).reshape(128, 32)
    shared = {
        "x_all": x2, "ctx": f(ctx)[0], "cT": f(cT),
        "g1T": f(f(norm1_g)[0].reshape(16, 128).T), "g2T": f(f(norm2_g)[0].reshape(16, 128).T),
        "w_mod": f(w_mod)[0], "b_mod": f(b_mod)[0], "w_in": f(w_in)[0],
        "lams": f(np.concatenate([f(lambda_q1)[0], f(lambda_k1)[0], f(lambda_q2)[0], f(lambda_k2)[0]])),
        "subg": f(subln_g)[0], "rope_all": rope,
        "w_a": f(w_branch_a)[0], "w_b": f(w_branch_b)[0], "w_o": f(w_out)[0], "w_q": f(peer_w_q)[0],
        "subT": f(f(peer_subkeys)[0].transpose(3, 0, 1, 2).reshape(128, 16 * 128)),
        "peer_u": f(peer_u)[0], "peer_v": f(peer_v)[0], "final_g": f(final_g),
    }
    rpb = f(na_rpb)[0]
    xg = x2.reshape(256, 64, D)
    in_maps = []
    for ci in range(NCORE):
        rows = _local_rows(ci)
        halo = np.concatenate([xg[rows[0:4]].reshape(256, D), xg[rows[36:40]].reshape(256, D)], axis=0)
        m = dict(shared)
        m["x_own"] = f(x2[ci * OWN:(ci + 1) * OWN])
        m["x_halo"] = f(halo)
        m["na_bias"] = _na_bias(rpb, ci)
        m["rope_own"] = f(rope_q[ci * OWN:(ci + 1) * OWN])
        in_maps.append(m)
    nc = build_program()
    res = run_bass_kernel_spmd(nc, in_maps, core_ids=list(range(NCORE)))
    outs = [np.asarray(r["out"], dtype=np.float32) for r in res.results]
    return np.concatenate(outs, axis=0).reshape(1, NTOK, D)
```

```python
import numpy as np
from contextlib import ExitStack
import concourse.bass as bass
import concourse.mybir as mybir
from concourse.bass_utils import run_bass_kernel_spmd

F32 = mybir.dt.float32
BF16 = mybir.dt.bfloat16
U32 = mybir.dt.uint32
AF = mybir.ActivationFunctionType
ALU = mybir.AluOpType
AX = mybir.AxisListType

EPOCH = 12000
SAME_ENG_SYNC = {'pe': False, 'act': True, 'dve': True, 'pool': True, 'sp': False}

D = 2048
NTOK = 16384
NCORE = 8
OWN = 2048
NLOC = 2816
NKEY = NTOK + 256
EPS = 1e-6
NEG = -30000.0


class Buf:
    def __init__(self, name, t):
        self.name = name
        self.t = t
        self.w = {}
        self.r = {}
        self.dsem = None
        self.dcnt = 0

    def __getitem__(self, idx):
        return self.t[idx]


def _merge(d, s):
    for k, v in s.items():
        if d.get(k, 0) < v:
            d[k] = v


class Sched:
    CE = ['pe', 'act', 'dve', 'pool']
    ENG = ['pe', 'act', 'dve', 'pool', 'sp']

    def __init__(self, nc, es):
        self.nc = nc
        self.es = es
        self.es_phase = None
        self.prog = {e: [] for e in self.ENG}
        self.cnt = {e: 0 for e in self.CE}
        self.waited = {e: {} for e in self.ENG}
        self.semobj = {}
        self.cur = {}
        self.nd = 0
        self.nops = 0
        self.free_dsems = []
        self.phase_bufs = []

    def _sem(self, key):
        if key not in self.semobj:
            name = 's_' + '_'.join(str(x) for x in key)
            self.semobj[key] = self.es.enter_context(self.nc.semaphore(name))
        return self.semobj[key]

    def sb(self, name, shape, dt, persistent=False):
        st = self.es if persistent else self.es_phase
        self.nd_names = getattr(self, 'nd_names', 0) + 1
        name = "%s_%d" % (name, self.nd_names)
        b = Buf(name, st.enter_context(self.nc.sbuf_tensor(name, list(shape), dt)))
        if not persistent:
            self.phase_bufs.append(b)
        return b

    def ps(self, name, shape, dt):
        self.nd_names = getattr(self, 'nd_names', 0) + 1
        name = "%s_%d" % (name, self.nd_names)
        return Buf(name, self.es_phase.enter_context(self.nc.psum_tensor(name, list(shape), dt)))

    def _deps(self, eng, reads, writes, skip=None):
        deps = {}
        for b in reads:
            _merge(deps, b.w)
        for b in writes:
            _merge(deps, b.w)
            _merge(deps, b.r)
        waits = []
        for k, v in deps.items():
            if skip is not None and k == skip:
                continue
            if k[0] == eng and not SAME_ENG_SYNC[eng]:
                continue
            if self.waited[eng].get(k, 0) >= v:
                continue
            self.waited[eng][k] = v
            waits.append((k, v))
        return waits

    def op(self, eng, fn, reads=(), writes=()):
        waits = self._deps(eng, reads, writes)
        self.cnt[eng] += 1
        c = self.cnt[eng]
        key = (eng, (c - 1) // EPOCH)
        val = (c - 1) % EPOCH + 1
        self._sem(key)
        self.cur[key] = val
        self.prog[eng].append((waits, fn, key, 1))
        for b in reads:
            b.r[key] = max(b.r.get(key, 0), val)
        for b in writes:
            b.w[key] = max(b.w.get(key, 0), val)
            b.r = {}
        self.nops += 1

    def dma(self, out_ap, in_ap, sembuf, reads=(), writes=(), **kw):
        if sembuf.dsem is None:
            if self.free_dsems:
                sembuf.dsem, sembuf.dcnt = self.free_dsems.pop()
            else:
                sembuf.dsem = ('d', self.nd)
                self.nd += 1
                self._sem(sembuf.dsem)
        key = sembuf.dsem
        waits = self._deps('sp', reads, writes, skip=key)
        sembuf.dcnt += 16
        val = sembuf.dcnt
        self.cur[key] = val

        def fn(e, out_ap=out_ap, in_ap=in_ap, kw=kw):
            return e.dma_start(out=out_ap, in_=in_ap, **kw)
        self.prog['sp'].append((waits, fn, key, 16))
        for b in reads:
            b.r[key] = max(b.r.get(key, 0), val)
        for b in writes:
            b.w[key] = max(b.w.get(key, 0), val)
            b.r = {}
        self.nops += 1

    def barrier(self):
        for e in self.ENG:
            waits = []
            for k, v in self.cur.items():
                if k[0] == e:
                    continue
                if self.waited[e].get(k, 0) >= v:
                    continue
                self.waited[e][k] = v
                waits.append((k, v))
            if waits:
                self.prog[e].append((waits, None, None, 0))

    def flush(self):
        nc = self.nc
        prog = self.prog
        self.prog = {e: [] for e in self.ENG}

        def run(eobj, lst):
            for waits, fn, key, inc in lst:
                for k, v in waits:
                    eobj.wait_ge(self.semobj[k], v)
                if fn is not None:
                    fn(eobj).then_inc(self.semobj[key], inc)

        with nc.Block() as block:
            @block.tensor
            def _(e):
                run(e, prog['pe'])

            @block.scalar
            def _(e):
                run(e, prog['act'])

            @block.vector
            def _(e):
                run(e, prog['dve'])

            @block.gpsimd
            def _(e):
                run(e, prog['pool'])

            @block.sync
            def _(e):
                run(e, prog['sp'])

    def end_phase(self, release=True):
        self.barrier()
        self.flush()
        if release:
            for b in self.phase_bufs:
                if b.dsem is not None:
                    self.free_dsems.append((b.dsem, b.dcnt))
                    b.dsem = None
            self.phase_bufs = []


def bc(ap, axis, shape):
    return ap.unsqueeze(axis).to_broadcast(list(shape))


def build_program(stop_after=None, dbg=()):
    nc = bass.Bass("TRN2", target_bir_lowering=False)

    def din(name, shape, dt=F32):
        return Buf(name, nc.dram_tensor(name, list(shape), dt, kind="ExternalInput").ap())

    def dscr(name, shape, dt):
        return Buf(name, nc.dram_tensor(name, list(shape), dt, kind="Internal").ap())

    x_all = din("x_all", [NTOK, D])
    x_own = din("x_own", [OWN, D])
    x_halo = din("x_halo", [512, D])
    ctx = din("ctx", [256, D])
    cT = din("cT", [128, 32])
    g1T = din("g1T", [128, 16])
    g2T = din("g2T", [128, 16])
    w_mod = din("w_mod", [D, 6 * D])
    b_mod = din("b_mod", [6 * D])
    w_in = din("w_in", [D, 10240])
    lams = din("lams", [256])
    subg = din("subg", [128])
    na_bias = din("na_bias", [8, 128, 5 * 5 * 128])
    rope_all = din("rope_all", [NTOK, 128])
    rope_own = din("rope_own", [OWN, 128])
    w_a = din("w_a", [1024, D])
    w_b = din("w_b", [1024, D])
    w_o = din("w_o", [D, D])
    w_q = din("w_q", [D, D])
    subT = din("subT", [128, 16 * 128])
    peer_u = din("peer_u", [NTOK, D])
    peer_v = din("peer_v", [NTOK, D])
    final_g = din("final_g", [D])
    out = Buf("out", nc.dram_tensor("out", [OWN, D], F32, kind="ExternalOutput").ap())

    mrow = dscr("mrow", [2, 6 * D], F32)
    KaT_d = dscr("KaT_d", [8, 128, NKEY], BF16)
    Va_d = dscr("Va_d", [NKEY, 1024], BF16)
    QaT_d = dscr("QaT_d", [8, 128, OWN], BF16)
    QbT_d = dscr("QbT_d", [8, 128, OWN], BF16)
    KbT_d = dscr("KbT_d", [8, 128, NLOC], BF16)
    Vb_d = dscr("Vb_d", [NLOC, 1024], BF16)
    GT_d = dscr("GT_d", [32, 128, OWN], BF16)
    OaT_d = dscr("OaT_d", [8, 128, OWN], BF16)
    ObT_d = dscr("ObT_d", [8, 128, OWN], BF16)
    mT_d = dscr("mT_d", [128, 16, OWN], BF16)
    X1_d = dscr("X1_d", [OWN, D], F32)
    UT_d = dscr("UT_d", [128, 128, 16 * 128], BF16)
    VB_d = dscr("VB_d", [NTOK, D], BF16)

    dbg_out = {}
    for name, shape in dbg:
        dbg_out[name] = Buf(name, nc.dram_tensor(name, list(shape), F32, kind="ExternalOutput").ap())

    with ExitStack() as es:
        S = Sched(nc, es)
        ident = S.sb("ident", [128, 128], BF16, True)
        identf = S.sb("identf", [128, 128], F32, True)
        iota_f = S.sb("iota_f", [128, 128], F32, True)
        modv = S.sb("modv", [128, 6, 16], F32, True)
        lamv = S.sb("lamv", [128, 4], F32, True)
        gsub = S.sb("gsub", [128, 128], F32, True)

        def phase_begin():
            S.es_phase = ExitStack()
            return S.es_phase

        with phase_begin():
            S.op('pool', lambda e: e.iota(identf[:, :], [[1, 128]], base=0, channel_multiplier=-1,
                                          allow_small_or_imprecise_dtypes=True), writes=[identf])
            S.op('pool', lambda e: e.tensor_single_scalar(out=identf[:, :], in_=identf[:, :], scalar=0.0,
                                                          op=ALU.is_equal), reads=[identf], writes=[identf])
            S.op('pool', lambda e: e.tensor_copy(out=ident[:, :], in_=identf[:, :]), reads=[identf], writes=[ident])
            S.op('pool', lambda e: e.iota(iota_f[:, :], [[1, 128]], base=0, channel_multiplier=0,
                                          allow_small_or_imprecise_dtypes=True), writes=[iota_f])
            c_sb = S.sb("c_sb", [128, 32], F32)
            sc_sb = S.sb("sc_sb", [128, 16, 2], F32)
            S.dma(c_sb[:, :], cT[:, :], c_sb, reads=[cT], writes=[c_sb])
            S.op('act', lambda e: e.activation(out=sc_sb.t[:, :, :].rearrange("p j r -> p (j r)"), in_=c_sb[:, :],
                                               func=AF.Silu), reads=[c_sb], writes=[sc_sb])
            bm_sb = S.sb("bm_sb", [2, 6 * D], F32)
            m_sb = S.sb("m_sb", [2, 6 * D], F32)
            S.dma(bm_sb[:, :], b_mod.t.partition_broadcast(2), bm_sb, reads=[b_mod], writes=[bm_sb])
            wm = [S.sb("wm%d" % i, [128, 16, 512], F32) for i in range(2)]
            pm = [S.ps("pm%d" % i, [2, 512], F32) for i in range(2)]
            for cb in range(24):
                wb = wm[cb % 2]
                S.dma(wb[:, :, :], w_mod.t[:, cb * 512:(cb + 1) * 512].rearrange("(j p) n -> p j n", p=128), wb,
                      reads=[w_mod], writes=[wb])

                def mm0(e, wb=wb, p=pm[cb % 2]):
                    for j in range(16):
                        ins = e.matmul(p[:, :], lhsT=sc_sb[:, j, :], rhs=wb[:, j, :], start=(j == 0), stop=(j == 15))
                    return ins
                S.op('pe', mm0, reads=[sc_sb, wb], writes=[pm[cb % 2]])
                S.op('dve', lambda e, p=pm[cb % 2], cb=cb: e.tensor_tensor(
                    out=m_sb[:, cb * 512:(cb + 1) * 512], in0=p[:, :], in1=bm_sb[:, cb * 512:(cb + 1) * 512],
                    op=ALU.add), reads=[pm[cb % 2], bm_sb], writes=[m_sb])
            S.dma(mrow[:, :], m_sb[:, :], m_sb, reads=[m_sb], writes=[mrow])
            pt0 = S.ps("pt0", [128, 96, 2], F32)
            modT = S.sb("modT", [128, 96, 2], F32)

            def tr0(e):
                for ch in range(96):
                    ins = e.transpose(out=pt0[:, ch, :], in_=m_sb[0:2, ch * 128:(ch + 1) * 128], identity=identf[0:2, 0:2])
                return ins
            S.op('pe', tr0, reads=[m_sb, identf], writes=[pt0])
            S.op('dve', lambda e: e.tensor_copy(out=modT[:, :, :], in_=pt0[:, :, :]), reads=[pt0], writes=[modT])
            g1_sb = S.sb("g1_sb", [128, 16], F32)
            g2_sb = S.sb("g2_sb", [128, 16], F32)
            S.dma(g1_sb[:, :], g1T[:, :], g1_sb, reads=[g1T], writes=[g1_sb])
            S.dma(g2_sb[:, :], g2T[:, :], g2_sb, reads=[g2T], writes=[g2_sb])
            def mk_a(dst, q, r, g):
                S.op('dve', lambda e: e.scalar_tensor_tensor(out=modv[:, dst, :], in0=modT[:, q * 16:(q + 1) * 16, r],
                                                             scalar=1.0, in1=g[:, :], op0=ALU.add, op1=ALU.mult),
                     reads=[modT, g], writes=[modv])

            def mk_b(dst, q, r):
                S.op('dve', lambda e: e.tensor_copy(out=modv[:, dst, :], in_=modT[:, q * 16:(q + 1) * 16, r]),
                     reads=[modT], writes=[modv])
            mk_a(0, 1, 0, g1_sb); mk_b(1, 0, 0)
            mk_a(2, 1, 1, g1_sb); mk_b(3, 0, 1)
            mk_a(4, 4, 0, g2_sb); mk_b(5, 3, 0)
            lq = S.sb("lq", [128, 4, 64], F32)
            lp = S.sb("lp", [128, 2, 64], F32)
            ls = S.sb("ls", [128, 2], F32)
            le = S.sb("le", [128, 2], F32)
            S.dma(lq.t[:, :, :].rearrange("p a b -> p (a b)"), lams.t.partition_broadcast(128), lq, reads=[lams], writes=[lq])
            S.op('dve', lambda e: e.tensor_tensor(out=lp[:, 0, :], in0=lq[:, 0, :], in1=lq[:, 1, :], op=ALU.mult), reads=[lq], writes=[lp])
            S.op('dve', lambda e: e.tensor_tensor(out=lp[:, 1, :], in0=lq[:, 2, :], in1=lq[:, 3, :], op=ALU.mult), reads=[lq, lp], writes=[lp])
            S.op('dve', lambda e: e.tensor_reduce(out=ls[:, :], in_=lp[:, :, :], axis=AX.X, op=ALU.add), reads=[lp], writes=[ls])
            S.op('act', lambda e: e.activation(out=le[:, :], in_=ls[:, :], func=AF.Exp), reads=[ls], writes=[le])
            S.op('dve', lambda e: e.tensor_tensor(out=lamv[:, 2:3], in0=le[:, 0:1], in1=le[:, 1:2], op=ALU.subtract), reads=[le], writes=[lamv])
            S.op('dve', lambda e: e.tensor_scalar(out=lamv[:, 0:1], in0=lamv[:, 2:3], scalar1=0.2, scalar2=None, op0=ALU.add), reads=[lamv], writes=[lamv])
            S.op('dve', lambda e: e.tensor_scalar(out=lamv[:, 1:2], in0=lamv[:, 0:1], scalar1=-1.0, scalar2=None, op0=ALU.mult), reads=[lamv], writes=[lamv])
            sg = S.sb("sg", [128, 128], F32)
            S.dma(sg[:, :], subg.t.partition_broadcast(128), sg, reads=[subg], writes=[sg])
            S.op('dve', lambda e: e.tensor_scalar(out=gsub[:, :], in0=sg[:, :], scalar1=0.8, scalar2=None, op0=ALU.mult), reads=[sg], writes=[gsub])
            if 'd_m' in dbg_out:
                S.dma(dbg_out['d_m'][:, :], m_sb[:, :], m_sb, reads=[m_sb], writes=[dbg_out['d_m']])
            S.end_phase()
        if stop_after == 0:
            return nc

        def make_norm(nx):
            R = {}
            R['xt'] = [S.sb("n_xt%d" % i, [128, D], F32) for i in range(nx)]
            R['junk'] = S.sb("n_junk", [128, D], BF16)
            R['ssq'] = [S.sb("n_ssq%d" % i, [128, 1], F32) for i in range(2)]
            R['std'] = [S.sb("n_std%d" % i, [128, 1], F32) for i in range(2)]
            R['rstd'] = [S.sb("n_rstd%d" % i, [128, 1], F32) for i in range(2)]
            R['xn'] = [S.sb("n_xn%d" % i, [128, D], BF16) for i in range(2)]
            R['ptr'] = [S.ps("n_ptr%d" % i, [128, 1024], BF16) for i in range(2)]
            R['tmp'] = S.sb("n_tmp", [128, D], F32)
            R['k'] = 0
            return R

        def norm_a1(R, src_ap, srcbuf, preloaded=None):
            k = R['k']
            R['k'] += 1
            if preloaded is None:
                xt = R['xt'][k % len(R['xt'])]
                S.dma(xt[:, :], src_ap, xt, reads=[srcbuf], writes=[xt])
            else:
                xt = preloaded
            ssq, std, rstd, xn = R['ssq'][k % 2], R['std'][k % 2], R['rstd'][k % 2], R['xn'][k % 2]
            junk = R['junk']
            S.op('act', lambda e: e.activation(out=junk[:, :], in_=xt[:, :], func=AF.Square, accum_out=ssq[:, :]),
                 reads=[xt], writes=[junk, ssq])
            S.op('act', lambda e: e.activation(out=std[:, :], in_=ssq[:, :], func=AF.Sqrt, scale=1.0 / D, bias=EPS),
                 reads=[ssq], writes=[std])
            S.op('dve', lambda e: e.reciprocal(out=rstd[:, :], in_=std[:, :]), reads=[std], writes=[rstd])
            S.op('act', lambda e: e.activation(out=xn[:, :], in_=xt[:, :], func=AF.Copy, scale=rstd[:, 0:1]),
                 reads=[xt, rstd], writes=[xn])
            return xn

        def norm_a2(R, xn, va, vb_, out_ap, outbuf):
            tmp = R['tmp']
            for half in range(2):
                ptr = R['ptr'][half]

                def trn(e, ptr=ptr, half=half):
                    for jj in range(8):
                        j = half * 8 + jj
                        ins = e.transpose(out=ptr[:, jj * 128:(jj + 1) * 128], in_=xn[:, j * 128:(j + 1) * 128], identity=ident[:, :])
                    return ins
                S.op('pe', trn, reads=[xn, ident], writes=[ptr])
                S.op('dve', lambda e, ptr=ptr, half=half: e.tensor_tensor(
                    out=tmp.t[:, half * 1024:(half + 1) * 1024].rearrange("p (j t) -> p j t", j=8),
                    in0=ptr.t[:, :].rearrange("p (j t) -> p j t", j=8),
                    in1=bc(modv[:, va, half * 8:(half + 1) * 8], 2, [128, 8, 128]), op=ALU.mult),
                    reads=[ptr, modv], writes=[tmp])
            S.op('pool', lambda e: e.tensor_tensor(
                out=out_ap, in0=tmp.t[:, :].rearrange("p (j t) -> p j t", j=16),
                in1=bc(modv[:, vb_, :], 2, [128, 16, 128]), op=ALU.add), reads=[tmp, modv], writes=[outbuf])

        def norm_tile(R, src_ap, srcbuf, va, vb_, out_ap, outbuf, preloaded=None):
            xn = norm_a1(R, src_ap, srcbuf, preloaded)
            norm_a2(R, xn, va, vb_, out_ap, outbuf)

        with phase_begin():
            Wkv = S.sb("Wkv", [128, 16, 2048], BF16)
            stg = [S.sb("stg%d" % i, [128, 16, 128], F32) for i in range(2)]
            for cb in range(16):
                st = stg[cb % 2]
                S.dma(st[:, :, :], w_in.t[:, 1024 + cb * 128:1024 + (cb + 1) * 128].rearrange("(j p) n -> p j n", p=128), st,
                      reads=[w_in], writes=[st])
                (S.op('act', lambda e, st=st, cb=cb: e.copy(out=Wkv[:, :, cb * 128:(cb + 1) * 128], in_=st[:, :, :]), reads=[st], writes=[Wkv]) if cb % 2 else
                 S.op('dve', lambda e, st=st, cb=cb: e.tensor_copy(out=Wkv[:, :, cb * 128:(cb + 1) * 128], in_=st[:, :, :]), reads=[st], writes=[Wkv]))
            R = make_norm(0)
            hT = [S.sb("hT%d" % i, [128, 16, 128], BF16) for i in range(3)]
            cs = [S.sb("cs%d" % i, [128, 128], F32) for i in range(3)]
            pk = [S.ps("pk%d" % i, [128, 512], F32) for i in range(4)]
            kraw = [[S.sb("kraw%d_%d" % (i, b_), [128, 512], F32) for b_ in range(2)] for i in range(2)]
            kf1 = [S.sb("kf1_%d" % i, [128, 512], F32) for i in range(2)]
            kf2 = [S.sb("kf2_%d" % i, [128, 512], F32) for i in range(2)]
            kb16 = [S.sb("kb16_%d" % i, [128, 1024], BF16) for i in range(2)]
            vb16 = [S.sb("vb16_%d" % i, [128, 1024], BF16) for i in range(2)]
            ptk = S.ps("ptk", [128, 1024], BF16)
            kst = [S.sb("kst%d" % i, [128, 8, 512], BF16) for i in range(2)]
            ntile = 130

            xts = [S.sb("xts%d" % i, [128, D], F32) for i in range(3)]

            def ldx(i):
                if i >= ntile:
                    return
                is_ctx = i >= 128
                src = ctx if is_ctx else x_all
                r0 = (i - 128) * 128 if is_ctx else i * 128
                S.dma(xts[i % 3][:, :], src[r0:r0 + 128, :], xts[i % 3], reads=[src], writes=[xts[i % 3]])

            xn_of = {}

            def stA1(i):
                if i >= ntile:
                    return
                if i == 0:
                    ldx(0)
                    ldx(1)
                ldx(i + 2)
                xn_of[i] = norm_a1(R, None, None, preloaded=xts[i % 3])

            def stA2(i):
                if i >= ntile:
                    return
                is_ctx = i >= 128
                r0 = (i - 128) * 128 if is_ctx else i * 128
                h = hT[i % 3]
                norm_a2(R, xn_of.pop(i), 2 if is_ctx else 0, 3 if is_ctx else 1, h[:, :, :], h)
                if not is_ctx:
                    S.dma(cs[i % 3][:, :], rope_all[r0:r0 + 128, :], cs[i % 3], reads=[rope_all], writes=[cs[i % 3]])

            def stB(i):
                is_ctx = i >= 128
                h = hT[i % 3]
                kb_ = kb16[i % 2]
                vb_ = vb16[i % 2]
                for blk in range(4):
                    p = pk[blk]

                    def mmk(e, p=p, blk=blk):
                        for j in range(16):
                            ins = e.matmul(p[:, :], lhsT=h[:, j, :], rhs=Wkv[:, j, blk * 512:(blk + 1) * 512], start=(j == 0), stop=(j == 15))
                        return ins
                    S.op('pe', mmk, reads=[h, Wkv], writes=[p])
                    if blk >= 2:
                        S.op('act', lambda e, p=p, blk=blk: e.copy(out=vb_[:, (blk - 2) * 512:(blk - 1) * 512], in_=p[:, :]), reads=[p], writes=[vb_])
                    elif is_ctx:
                        S.op('act', lambda e, p=p, blk=blk: e.copy(out=kb_[:, blk * 512:(blk + 1) * 512], in_=p[:, :]), reads=[p], writes=[kb_])
                    else:
                        kr = kraw[i % 2][blk]
                        S.op('act', lambda e, p=p, kr=kr: e.copy(out=kr[:, :], in_=p[:, :]), reads=[p], writes=[kr])
                if not is_ctx:
                    c_ = cs[i % 3]
                    for blk in range(2):
                        kr = kraw[i % 2][blk]
                        t1, t2 = kf1[blk], kf2[blk]
                        S.op('dve', lambda e, kr=kr, t1=t1: e.tensor_tensor(
                            out=t1.t[:, :].rearrange("p (g d) -> p g d", g=8), in0=kr.t[:, :].rearrange("p (g d) -> p g d", g=8),
                            in1=bc(c_[:, 0:64], 1, [128, 8, 64]), op=ALU.mult), reads=[kr, c_], writes=[t1])
                        for ab in range(2):
                            S.op('dve', lambda e, kr=kr, t2=t2, ab=ab: e.tensor_tensor(
                                out=t2.t[:, :].rearrange("p (g r a d) -> p g r a d", g=8, r=2, a=2)[:, :, :, ab, :],
                                in0=kr.t[:, :].rearrange("p (g r a d) -> p g r a d", g=8, r=2, a=2)[:, :, :, 1 - ab, :],
                                in1=bc(c_.t[:, 64:128].rearrange("p (r a d) -> p r a d", r=2, a=2)[:, :, ab, :], 1, [128, 8, 2, 16]),
                                op=ALU.mult), reads=[kr, c_], writes=[t2])
                        S.op('pool', lambda e, t1=t1, t2=t2, blk=blk: e.tensor_tensor(
                            out=kb_[:, blk * 512:(blk + 1) * 512], in0=t1[:, :], in1=t2[:, :], op=ALU.add),
                            reads=[t1, t2], writes=[kb_])
                S.dma(Va_d[i * 128:(i + 1) * 128, :], vb_[:, :], vb_, reads=[vb_], writes=[Va_d])

            def stC(i):
                kb_ = kb16[i % 2]

                def trk(e):
                    for hh in range(8):
                        ins = e.transpose(out=ptk[:, hh * 128:(hh + 1) * 128], in_=kb_[:, hh * 128:(hh + 1) * 128], identity=ident[:, :])
                    return ins
                S.op('pe', trk, reads=[kb_, ident], writes=[ptk])
                ks = kst[(i // 4) % 2]
                S.op('act', lambda e: e.copy(out=ks[:, :, (i % 4) * 128:(i % 4 + 1) * 128],
                                             in_=ptk.t[:, :].rearrange("p (h t) -> p h t", h=8)),
                     reads=[ptk], writes=[ks])
                if i % 4 == 3 or i == ntile - 1:
                    nt = (i % 4 + 1) * 128
                    t0 = (i // 4) * 512
                    S.dma(KaT_d.t[:, :, t0:t0 + nt].rearrange("h p t -> p h t"), ks[:, :, 0:nt], ks, reads=[ks], writes=[KaT_d])

            stA1(0)
            stA2(0)
            stA1(1)
            stA2(1)
            for k_ in range(ntile + 2):
                stA1(k_ + 2)
                if k_ < ntile:
                    stB(k_)
                if 1 <= k_ <= ntile:
                    stC(k_ - 1)
                stA2(k_ + 2)
            S.end_phase()

        with ExitStack() as outer1b:
            S.es_phase = outer1b
            hL = S.sb("hL", [128, 16, NLOC], BF16)
            with phase_begin():
                R = make_norm(2)
                for t in range(22):
                    if t < 2:
                        src, r0 = x_halo, t * 128
                    elif t < 18:
                        src, r0 = x_own, (t - 2) * 128
                    elif t < 20:
                        src, r0 = x_halo, 256 + (t - 18) * 128
                    else:
                        src, r0 = ctx, (t - 20) * 128
                    isc = t >= 20
                    norm_tile(R, src[r0:r0 + 128, :], src, 2 if isc else 0, 3 if isc else 1, hL[:, :, t * 128:(t + 1) * 128], hL)
                S.end_phase(release=False)
            S.es_phase = ExitStack()
            inner1b = S.es_phase
            inner1b.__enter__()
            wst = [S.sb("wst%d" % i, [128, 16, 256], F32) for i in range(2)]
            wbb = [S.sb("wbb%d" % i, [128, 16, 256], BF16) for i in range(2)]
            pp = [S.ps("pp%d" % i, [128, 512], F32) for i in range(4)]
            ptq = S.ps("ptq", [128, 1024], BF16)
            csq = [S.sb("csq%d" % i, [128, 128], F32) for i in range(2)]
            qf1 = [S.sb("qf1_%d" % i, [128, 256], F32) for i in range(2)]
            qf2 = [S.sb("qf2_%d" % i, [128, 256], F32) for i in range(2)]
            q16 = [S.sb("q16_%d" % i, [128, 256], BF16) for i in range(2)]
            stgA = [S.sb("stgA%d" % i, [128, 2, NLOC], BF16) for i in range(2)]
            v16 = [S.sb("v16_%d" % i, [128, 256], BF16) for i in range(3)]
            blocks = []
            for b in range(4):
                blocks.append(('qa', b * 256, b))
            for b in range(4):
                blocks.append(('qb', 3072 + b * 256, b))
            for b in range(4):
                blocks.append(('kb', 4096 + b * 256, b))
            for b in range(4):
                blocks.append(('vb', 5120 + b * 256, b))
            for b in range(16):
                blocks.append(('g', 6144 + b * 256, b))
            kctr = 0
            def ldw(bi):
                if bi >= len(blocks):
                    return
                c0_ = blocks[bi][1]
                ws, wb = wst[bi % 2], wbb[bi % 2]
                S.dma(ws[:, :, :], w_in.t[:, c0_:c0_ + 256].rearrange("(j p) n -> p j n", p=128), ws, reads=[w_in], writes=[ws])
                S.op('dve', lambda e: e.tensor_copy(out=wb[:, :, :], in_=ws[:, :, :]), reads=[ws], writes=[wb])
            ldw(0)
            for bi, (kind, c0, b) in enumerate(blocks):
                ws, wb = wst[bi % 2], wbb[bi % 2]
                ldw(bi + 1)
                sA = stgA[bi % 2]
                if kind == 'qa':
                    for t in range(16):
                        p = pp[t % 4]
                        lt = (t + 2) * 128

                        def mmq(e, p=p, wb=wb, lt=lt):
                            for j in range(16):
                                ins = e.matmul(p[:, 0:256], lhsT=hL[:, j, lt:lt + 128], rhs=wb[:, j, :], start=(j == 0), stop=(j == 15))
                            return ins
                        S.op('pe', mmq, reads=[hL, wb], writes=[p])
                        c_ = csq[t % 2]
                        S.dma(c_[:, :], rope_own[t * 128:(t + 1) * 128, :], c_, reads=[rope_own], writes=[c_])
                        t1, t2, qq = qf1[t % 2], qf2[t % 2], q16[t % 2]
                        S.op('dve', lambda e, p=p, t1=t1, c_=c_: e.tensor_tensor(
                            out=t1.t[:, :].rearrange("p (g d) -> p g d", g=4), in0=p.t[:, 0:256].rearrange("p (g d) -> p g d", g=4),
                            in1=bc(c_[:, 0:64], 1, [128, 4, 64]), op=ALU.mult), reads=[p, c_], writes=[t1])
                        for ab in range(2):
                            S.op('dve', lambda e, p=p, t2=t2, c_=c_, ab=ab: e.tensor_tensor(
                                out=t2.t[:, :].rearrange("p (g r a d) -> p g r a d", g=4, r=2, a=2)[:, :, :, ab, :],
                                in0=p.t[:, 0:256].rearrange("p (g r a d) -> p g r a d", g=4, r=2, a=2)[:, :, :, 1 - ab, :],
                                in1=bc(c_.t[:, 64:128].rearrange("p (r a d) -> p r a d", r=2, a=2)[:, :, ab, :], 1, [128, 4, 2, 16]),
                                op=ALU.mult), reads=[p, c_], writes=[t2])
                        S.op('pool', lambda e, t1=t1, t2=t2, qq=qq: e.tensor_tensor(out=qq[:, :], in0=t1[:, :], in1=t2[:, :], op=ALU.add),
                             reads=[t1, t2], writes=[qq])

                        def tr_stage(t_, qq_, sA=sA):
                            def trq(e):
                                for hh in range(2):
                                    ins = e.transpose(out=ptq[:, hh * 128:(hh + 1) * 128], in_=qq_[:, hh * 128:(hh + 1) * 128], identity=ident[:, :])
                                return ins
                            S.op('pe', trq, reads=[qq_, ident], writes=[ptq])
                            S.op('act', lambda e: e.copy(out=sA[:, :, t_ * 128:(t_ + 1) * 128],
                                                         in_=ptq.t[:, 0:256].rearrange("p (h t) -> p h t", h=2)),
                                 reads=[ptq], writes=[sA])
                        if t >= 1:
                            tr_stage(t - 1, q16[(t - 1) % 2])
                        if t == 15:
                            tr_stage(15, qq)
                    S.dma(QaT_d.t[2 * b:2 * b + 2, :, :].rearrange("h p t -> p h t"), sA[:, :, 0:OWN], sA, reads=[sA], writes=[QaT_d])
                elif kind in ('qb', 'kb', 'g'):
                    if kind == 'kb':
                        groups = [(g * 512, 512) for g in range(5)] + [(2560, 256)]
                    else:
                        groups = [(256 + g * 512, 512) for g in range(4)]
                    for cc in range(2):
                        for gi, (l0, n) in enumerate(groups):
                            p = pp[kctr % 4]
                            kctr += 1

                            def mmf(e, p=p, wb=wb, cc=cc, l0=l0, n=n):
                                for j in range(16):
                                    ins = e.matmul(p[:, 0:n], lhsT=wb[:, j, cc * 128:(cc + 1) * 128], rhs=hL[:, j, l0:l0 + n], start=(j == 0), stop=(j == 15))
                                return ins
                            S.op('pe', mmf, reads=[hL, wb], writes=[p])
                            o0 = l0 if kind == 'kb' else l0 - 256
                            if kind == 'g':
                                S.op('act', lambda e, p=p, sA=sA, cc=cc, o0=o0, n=n: e.activation(out=sA[:, cc, o0:o0 + n], in_=p[:, 0:n], func=AF.Sigmoid),
                                     reads=[p], writes=[sA])
                            elif kind == 'qb':
                                S.op('act', lambda e, p=p, sA=sA, cc=cc, o0=o0, n=n: e.activation(out=sA[:, cc, o0:o0 + n], in_=p[:, 0:n], func=AF.Copy, scale=128.0 ** -0.5),
                                     reads=[p], writes=[sA])
                            else:
                                S.op('act', lambda e, p=p, sA=sA, cc=cc, o0=o0, n=n: e.copy(out=sA[:, cc, o0:o0 + n], in_=p[:, 0:n]),
                                     reads=[p], writes=[sA])
                    if kind == 'qb':
                        S.dma(QbT_d.t[2 * b:2 * b + 2, :, :].rearrange("h p t -> p h t"), sA[:, :, 0:OWN], sA, reads=[sA], writes=[QbT_d])
                    elif kind == 'kb':
                        S.dma(KbT_d.t[2 * b:2 * b + 2, :, :].rearrange("h p t -> p h t"), sA[:, :, 0:NLOC], sA, reads=[sA], writes=[KbT_d])
                    else:
                        S.dma(GT_d.t[2 * b:2 * b + 2, :, :].rearrange("h p t -> p h t"), sA[:, :, 0:OWN], sA, reads=[sA], writes=[GT_d])
                else:
                    for t in range(22):
                        p = pp[t % 4]

                        def mmv(e, p=p, wb=wb, t=t):
                            for j in range(16):
                                ins = e.matmul(p[:, 0:256], lhsT=hL[:, j, t * 128:(t + 1) * 128], rhs=wb[:, j, :], start=(j == 0), stop=(j == 15))
                            return ins
                        S.op('pe', mmv, reads=[hL, wb], writes=[p])
                        vv = v16[t % 3]
                        S.op('act', lambda e, p=p, vv=vv: e.copy(out=vv[:, :], in_=p[:, 0:256]), reads=[p], writes=[vv])
                        S.dma(Vb_d[t * 128:(t + 1) * 128, b * 256:(b + 1) * 256], vv[:, :], vv, reads=[vv], writes=[Vb_d])
            S.end_phase()
            inner1b.__exit__(None, None, None)

        with phase_begin():
            kT = [S.sb("kT%d" % i, [128, NKEY], BF16) for i in range(2)]
            vA = [S.sb("vA%d" % i, [128, 130, 129], BF16) for i in range(2)]
            qA = [S.sb("qA%d" % i, [128, OWN], BF16) for i in range(2)]
            for i in range(2):
                S.op('pool', lambda e, i=i: e.memset(vA[i][:, :, 128:129], 1.0), writes=[vA[i]])
            pS = [S.ps("pS%d" % i, [128, 2, 512], F32) for i in range(2)]
            pO = [S.ps("pO%d" % i, [128, 512], F32) for i in range(3)]
            pT_ = [S.sb("pT%d" % i, [128, 2, 512], BF16) for i in range(3)]
            ptA = S.ps("ptA", [128, 1024], BF16)
            o0 = S.sb("o0", [128, 128], F32)
            accS = [S.sb("accS%d" % i, [128, 387], F32) for i in range(3)]
            dd = [S.sb("dd%d" % i, [128, 128], F32) for i in range(2)]
            rz = [S.sb("rz%d" % i, [128, 2], F32) for i in range(2)]
            jk = S.sb("jk", [128, 128], F32)
            s2 = [S.sb("s2_%d" % i, [128, 1], F32) for i in range(2)]
            sd = [S.sb("sd_%d" % i, [128, 1], F32) for i in range(2)]
            rs = [S.sb("rs_%d" % i, [128, 1], F32) for i in range(2)]
            oa16 = [S.sb("oa16_%d" % i, [128, 128], BF16) for i in range(2)]
            oaS = [S.sb("oaS%d" % i, [128, OWN], BF16) for i in range(2)]
            NKC = NKEY // 128

            def acc(mi, qt):
                a_ = mi * 4 + qt
                return pO[a_ // 3], a_ % 3
            uf = S.sb("uf", [128, D], F32)
            ub = S.sb("ub", [128, D], BF16)
            us = S.sb("us", [128, D], BF16)
            vf = S.sb("vf", [128, D], F32)
            vb2 = S.sb("vb2", [128, D], BF16)

            def puv_gen():
                for c in range(128):
                    S.dma(uf[:, :], peer_u[c * 128:(c + 1) * 128, :], uf, reads=[peer_u], writes=[uf])
                    S.dma(vf[:, :], peer_v[c * 128:(c + 1) * 128, :], vf, reads=[peer_v], writes=[vf])
                    yield
                    yield
                    S.op('dve', lambda e: e.tensor_copy(out=ub[:, :], in_=uf[:, :]), reads=[uf], writes=[ub])
                    S.op('dve', lambda e: e.tensor_copy(out=vb2[:, :], in_=vf[:, :]), reads=[vf], writes=[vb2])
                    yield
                    yield
                    for half in range(2):
                        def tru(e, half=half):
                            for jj in range(8):
                                j = half * 8 + jj
                                ins = e.transpose(out=ptA[:, jj * 128:(jj + 1) * 128], in_=ub[:, j * 128:(j + 1) * 128], identity=ident[:, :])
                            return ins
                        S.op('pe', tru, reads=[ub, ident], writes=[ptA])
                        yield
                        S.op('dve', lambda e, half=half: e.tensor_copy(out=us[:, half * 1024:(half + 1) * 1024], in_=ptA[:, :]),
                             reads=[ptA], writes=[us])
                        yield
                    S.dma(UT_d[c, :, :], us[:, :], us, reads=[us], writes=[UT_d])
                    S.dma(VB_d[c * 128:(c + 1) * 128, :], vb2[:, :], vb2, reads=[vb2], writes=[VB_d])
            puv = puv_gen()

            def head_loads(h):
                if h >= 8:
                    return
                k_, v_, q_ = kT[h % 2], vA[h % 2], qA[h % 2]
                S.dma(k_[:, :], KaT_d[h, :, :], k_, reads=[KaT_d], writes=[k_])
                for part in range(2):
                    c0, c1 = part * 65, (part + 1) * 65
                    S.dma(v_[:, c0:c1, 0:128], Va_d.t[c0 * 128:c1 * 128, h * 128:(h + 1) * 128].rearrange("(c p) e -> p c e", p=128),
                          v_, reads=[Va_d], writes=[v_])
                S.dma(q_[:, :], QaT_d[h, :, :], q_, reads=[QaT_d], writes=[q_])
            ctr = 0
            head_loads(0)
            for h in range(8):
                k_, v_, q_ = kT[h % 2], vA[h % 2], qA[h % 2]
                head_loads(h + 1)
                oS = oaS[h % 2]
                for qg in range(4):
                    def s_op(kc, k_=k_, q_=q_, qg=qg):
                        p = pS[kc % 2]

                        def f(e):
                            for mi in range(2):
                                ins = e.matmul(p[:, mi, :], lhsT=k_[64 * mi:64 * mi + 64, kc * 128:(kc + 1) * 128],
                                               rhs=q_[64 * mi:64 * mi + 64, qg * 512:(qg + 1) * 512], start=True, stop=True)
                            return ins
                        S.op('pe', f, reads=[k_, q_], writes=[p])
                    def pv_op(kc, v_=v_):
                        pt = pT_[kc % 3]

                        def pv(e):
                            for mi in range(2):
                                for qt in range(4):
                                    ab, ai = acc(mi, qt)
                                    ins = e.matmul(ab[:, ai * 129:ai * 129 + 129], lhsT=pt[:, mi, qt * 128:(qt + 1) * 128], rhs=v_[:, kc, :],
                                                   start=(kc == 0 and ai == 0), stop=(kc == NKC - 1), skip_group_check=True)
                            return ins
                        S.op('pe', pv, reads=[pt, v_], writes=pO)
                    s_op(0)
                    for kc in range(NKC):
                        p = pS[kc % 2]
                        pt = pT_[kc % 3]
                        S.op('act', lambda e, p=p, pt=pt: e.activation(out=pt[:, :, :], in_=p[:, :, :], func=AF.Exp), reads=[p], writes=[pt])
                        if kc + 1 < NKC:
                            s_op(kc + 1)
                        if kc >= 1:
                            pv_op(kc - 1)
                        if kc % 4 == 3:
                            next(puv, None)
                    pv_op(NKC - 1)
                    for b_ in range(3):
                        S.op('dve', lambda e, b_=b_: e.tensor_copy(out=accS[b_][:, :], in_=pO[b_][:, 0:387]), reads=[pO[b_]], writes=[accS[b_]])
                    for qt in range(4):
                        ctr += 1
                        r_, d_ = rz[ctr % 2], dd[ctr % 2]
                        a0, i0 = accS[(0 * 4 + qt) // 3], (0 * 4 + qt) % 3
                        a1, i1 = accS[(1 * 4 + qt) // 3], (1 * 4 + qt) % 3
                        S.op('dve', lambda e, a0=a0, i0=i0, r_=r_: e.reciprocal(out=r_[:, 0:1], in_=a0[:, i0 * 129 + 128:i0 * 129 + 129]), reads=[a0], writes=[r_])
                        S.op('dve', lambda e, a1=a1, i1=i1, r_=r_: e.reciprocal(out=r_[:, 1:2], in_=a1[:, i1 * 129 + 128:i1 * 129 + 129]), reads=[a1, r_], writes=[r_])
                        S.op('dve', lambda e, a0=a0, i0=i0, r_=r_: e.tensor_scalar(out=o0[:, :], in0=a0[:, i0 * 129:i0 * 129 + 128], scalar1=r_[:, 0:1], scalar2=None, op0=ALU.mult),
                             reads=[a0, r_], writes=[o0])
                        S.op('dve', lambda e, a1=a1, i1=i1, r_=r_, d_=d_: e.tensor_scalar(out=d_[:, :], in0=a1[:, i1 * 129:i1 * 129 + 128], scalar1=r_[:, 1:2], scalar2=lamv[:, 1:2], op0=ALU.mult, op1=ALU.mult),
                             reads=[a1, r_, lamv], writes=[d_])
                        S.op('dve', lambda e, d_=d_: e.tensor_tensor(out=d_[:, :], in0=d_[:, :], in1=o0[:, :], op=ALU.add),
                             reads=[d_, o0], writes=[d_])
                        a_, b_, c_ = s2[ctr % 2], sd[ctr % 2], rs[ctr % 2]
                        S.op('act', lambda e, d_=d_, a_=a_: e.activation(out=jk[:, :], in_=d_[:, :], func=AF.Square, accum_out=a_[:, :]),
                             reads=[d_], writes=[jk, a_])
                        S.op('act', lambda e, a_=a_, b_=b_: e.activation(out=b_[:, :], in_=a_[:, :], func=AF.Sqrt, scale=1.0 / 128, bias=EPS),
                             reads=[a_], writes=[b_])
                        S.op('dve', lambda e, b_=b_, c_=c_: e.reciprocal(out=c_[:, :], in_=b_[:, :]), reads=[b_], writes=[c_])
                        o16 = oa16[ctr % 2]
                        S.op('dve', lambda e, d_=d_, c_=c_, o16=o16: e.scalar_tensor_tensor(out=o16[:, :], in0=d_[:, :], scalar=c_[:, 0:1], in1=gsub[:, :], op0=ALU.mult, op1=ALU.mult),
                             reads=[d_, c_, gsub], writes=[o16])
                        S.op('pe', lambda e, o16=o16: e.transpose(out=ptA[:, 0:128], in_=o16[:, :], identity=ident[:, :]), reads=[o16, ident], writes=[ptA])
                        tt = qg * 4 + qt
                        S.op('act', lambda e, oS=oS, tt=tt: e.copy(out=oS[:, tt * 128:(tt + 1) * 128], in_=ptA[:, 0:128]), reads=[ptA], writes=[oS])
                S.dma(OaT_d[h, :, :], oS[:, :], oS, reads=[oS], writes=[OaT_d])
            for _ in puv:
                pass
            S.end_phase()

        with phase_begin():
            kB = [S.sb("kB%d" % i, [128, NLOC], BF16) for i in range(2)]
            vB = [S.sb("vB%d" % i, [128, 22, 129], BF16) for i in range(2)]
            qB = [S.sb("qB%d" % i, [128, OWN], BF16) for i in range(2)]
            bia = [S.sb("bia%d" % i, [128, 5, 5, 128], F32) for i in range(2)]
            for i in range(2):
                S.op('pool', lambda e, i=i: e.memset(vB[i][:, :, 128:129], 1.0), writes=[vB[i]])
            pN = [S.ps("pN%d" % i, [128, 8, 128], F32) for i in range(2)]
            pNo = [S.ps("pNo%d" % i, [128, 512], F32) for i in range(2)]
            ptB = S.ps("ptB", [128, 1024], BF16)
            sN = [S.sb("sN%d" % i, [128, 5, 128], F32) for i in range(2)]
            pTn = [S.sb("pTn%d" % i, [128, 7, 128], BF16) for i in range(2)]
            rzb = [S.sb("rzb%d" % i, [128, 1], F32) for i in range(2)]
            ob16 = [S.sb("ob16_%d" % i, [128, 128], BF16) for i in range(2)]
            obS = [S.sb("obS%d" % i, [128, OWN], BF16) for i in range(2)]
            def na_loads(h):
                if h >= 8:
                    return
                k_, v_, q_, bi_ = kB[h % 2], vB[h % 2], qB[h % 2], bia[h % 2]
                S.dma(k_[:, :], KbT_d[h, :, :], k_, reads=[KbT_d], writes=[k_])
                S.dma(v_[:, :, 0:128], Vb_d.t[:, h * 128:(h + 1) * 128].rearrange("(c p) e -> p c e", p=128), v_, reads=[Vb_d], writes=[v_])
                S.dma(q_[:, :], QbT_d[h, :, :], q_, reads=[QbT_d], writes=[q_])
                S.dma(bi_.t[:, :, :, :].rearrange("p a b q -> p (a b q)"), na_bias[h, :, :], bi_, reads=[na_bias], writes=[bi_])
            na_loads(0)
            for h in range(8):
                k_, v_, q_, bi_ = kB[h % 2], vB[h % 2], qB[h % 2], bia[h % 2]
                na_loads(h + 1)
                oS = obS[h % 2]

                def s_stage(j, k_=k_, q_=q_):
                    p = pN[j % 2]

                    def mms(e):
                        for c in range(7):
                            k0 = 128 * j + 128 * c if c < 5 else 2560 + 128 * (c - 5)
                            ins = e.matmul(p[:, c, :], lhsT=k_[:, k0:k0 + 128], rhs=q_[:, j * 128:(j + 1) * 128], start=True, stop=True)
                        return ins
                    S.op('pe', mms, reads=[k_, q_], writes=[p])

                def mid_stage(j, bi_=bi_):
                    slot = 0 if j == 0 else 1 if j == 1 else 3 if j == 14 else 4 if j == 15 else 2
                    p, s_, pt = pN[j % 2], sN[j % 2], pTn[j % 2]
                    S.op('dve', lambda e: e.tensor_tensor(out=s_[:, :, :], in0=p[:, 0:5, :], in1=bi_[:, slot, :, :], op=ALU.add),
                         reads=[p, bi_], writes=[s_])
                    S.op('act', lambda e: e.activation(out=pt[:, 0:5, :], in_=s_[:, :, :], func=AF.Exp), reads=[s_], writes=[pt])
                    S.op('act', lambda e: e.activation(out=pt[:, 5:7, :], in_=p[:, 5:7, :], func=AF.Exp), reads=[p], writes=[pt])

                def o_stage(j, v_=v_):
                    pt, po = pTn[j % 2], pNo[j % 2]

                    def mmo(e):
                        for c in range(7):
                            tile = j + c if c < 5 else 20 + (c - 5)
                            ins = e.matmul(po[:, 0:129], lhsT=pt[:, c, :], rhs=v_[:, tile, :], start=(c == 0), stop=(c == 6))
                        return ins
                    S.op('pe', mmo, reads=[pt, v_], writes=[po])
                    r_, o16 = rzb[j % 2], ob16[j % 2]
                    S.op('dve', lambda e: e.reciprocal(out=r_[:, :], in_=po[:, 128:129]), reads=[po], writes=[r_])
                    S.op('dve', lambda e: e.tensor_scalar(out=o16[:, :], in0=po[:, 0:128], scalar1=r_[:, 0:1], scalar2=None, op0=ALU.mult),
                         reads=[po, r_], writes=[o16])

                def t_stage(j, oS=oS):
                    o16 = ob16[j % 2]
                    S.op('pe', lambda e: e.transpose(out=ptB[:, 0:128], in_=o16[:, :], identity=ident[:, :]), reads=[o16, ident], writes=[ptB])
                    S.op('act', lambda e: e.copy(out=oS[:, j * 128:(j + 1) * 128], in_=ptB[:, 0:128]), reads=[ptB], writes=[oS])
                s_stage(0)
                for j in range(16):
                    if j + 1 < 16:
                        s_stage(j + 1)
                    mid_stage(j)
                    o_stage(j)
                    if j >= 1:
                        t_stage(j - 1)
                t_stage(15)
                S.dma(ObT_d[h, :, :], oS[:, :], oS, reads=[oS], writes=[ObT_d])
            S.end_phase()

        def load_w_bf16(dst, src, nrow_chunks, stgs, ncol=2048, cw=256):
            k = 0
            for c0 in range(0, ncol, cw):
                st = stgs[k % 2]
                S.dma(st[:, 0:nrow_chunks, :], src.t[:, c0:c0 + cw].rearrange("(j p) n -> p j n", p=128), st, reads=[src], writes=[st])
                (S.op('act', lambda e, st=st, c0=c0: e.copy(out=dst[:, :, c0:c0 + cw], in_=st[:, 0:nrow_chunks, :]), reads=[st], writes=[dst]) if k % 2 else
                 S.op('dve', lambda e, st=st, c0=c0: e.tensor_copy(out=dst[:, :, c0:c0 + cw], in_=st[:, 0:nrow_chunks, :]), reads=[st], writes=[dst]))
                k += 1

        with phase_begin():
            wa = S.sb("wa", [128, 8, 2048], BF16)
            wb_ = S.sb("wb_", [128, 8, 2048], BF16)
            stg4 = [S.sb("stg4_%d" % i, [128, 16, 256], F32) for i in range(2)]
            load_w_bf16(wa, w_a, 8, stg4)
            load_w_bf16(wb_, w_b, 8, stg4)
            oa = [S.sb("oa%d" % i, [128, 8, 512], BF16) for i in range(2)]
            ob = [S.sb("ob%d" % i, [128, 8, 512], BF16) for i in range(2)]
            ga = [S.sb("ga%d" % i, [128, 512], BF16) for i in range(2)]
            gb = [S.sb("gb%d" % i, [128, 512], BF16) for i in range(2)]
            pa = [S.ps("pa%d" % i, [128, 512], F32) for i in range(2)]
            pb = [S.ps("pb%d" % i, [128, 512], F32) for i in range(2)]
            t1 = [S.sb("m1_%d" % i, [128, 512], F32) for i in range(2)]
            t2 = [S.sb("m2_%d" % i, [128, 512], F32) for i in range(2)]
            mS = [S.sb("mS%d" % i, [128, 16, 512], BF16) for i in range(2)]
            k = 0
            for tg in range(4):
                oa_, ob_, ms = oa[tg % 2], ob[tg % 2], mS[tg % 2]
                S.dma(oa_[:, :, :], OaT_d.t[:, :, tg * 512:(tg + 1) * 512].rearrange("h p t -> p h t"), oa_, reads=[OaT_d], writes=[oa_])
                S.dma(ob_[:, :, :], ObT_d.t[:, :, tg * 512:(tg + 1) * 512].rearrange("h p t -> p h t"), ob_, reads=[ObT_d], writes=[ob_])
                for fc in range(16):
                    k += 1
                    ga_, gb_, pa_, pb_, t1_, t2_ = ga[k % 2], gb[k % 2], pa[k % 2], pb[k % 2], t1[k % 2], t2[k % 2]
                    S.dma(ga_[:, :], GT_d[fc, :, tg * 512:(tg + 1) * 512], ga_, reads=[GT_d], writes=[ga_])
                    S.dma(gb_[:, :], GT_d[16 + fc, :, tg * 512:(tg + 1) * 512], gb_, reads=[GT_d], writes=[gb_])

                    def mma(e, pa_=pa_, oa_=oa_, fc=fc):
                        for hh in range(8):
                            ins = e.matmul(pa_[:, :], lhsT=wa[:, hh, fc * 128:(fc + 1) * 128], rhs=oa_[:, hh, :], start=(hh == 0), stop=(hh == 7))
                        return ins

                    def mmb(e, pb_=pb_, ob_=ob_, fc=fc):
                        for hh in range(8):
                            ins = e.matmul(pb_[:, :], lhsT=wb_[:, hh, fc * 128:(fc + 1) * 128], rhs=ob_[:, hh, :], start=(hh == 0), stop=(hh == 7))
                        return ins
                    S.op('pe', mma, reads=[wa, oa_], writes=[pa_])
                    S.op('pe', mmb, reads=[wb_, ob_], writes=[pb_])
                    S.op('dve', lambda e, pa_=pa_, ga_=ga_, t1_=t1_: e.tensor_tensor(out=t1_[:, :], in0=pa_[:, :], in1=ga_[:, :], op=ALU.mult), reads=[pa_, ga_], writes=[t1_])
                    S.op('dve', lambda e, pb_=pb_, gb_=gb_, t2_=t2_: e.tensor_tensor(out=t2_[:, :], in0=pb_[:, :], in1=gb_[:, :], op=ALU.mult), reads=[pb_, gb_], writes=[t2_])
                    S.op('pool', lambda e, t1_=t1_, t2_=t2_, ms=ms, fc=fc: e.tensor_tensor(out=ms[:, fc, :], in0=t1_[:, :], in1=t2_[:, :], op=ALU.add), reads=[t1_, t2_], writes=[ms])
                S.dma(mT_d[:, :, tg * 512:(tg + 1) * 512], ms[:, :, :], ms, reads=[ms], writes=[mT_d])
            S.end_phase()

        with phase_begin():
            wo = S.sb("wo", [128, 16, 2048], BF16)
            stg5 = [S.sb("stg5_%d" % i, [128, 16, 256], F32) for i in range(2)]
            load_w_bf16(wo, w_o, 16, stg5)
            gt1 = S.sb("gt1", [128, D], F32)
            S.dma(gt1[:, :], mrow.t[0, 2 * D:3 * D].partition_broadcast(128), gt1, reads=[mrow], writes=[gt1])
            mt = [S.sb("mt%d" % i, [128, 16, 512], BF16) for i in range(2)]
            xo = [S.sb("xo%d" % i, [128, D], F32) for i in range(2)]
            x1 = [S.sb("x1_%d" % i, [128, D], F32) for i in range(2)]
            py = [S.ps("py%d" % i, [128, 512], F32) for i in range(4)]
            for tg in range(4):
                m_ = mt[tg % 2]
                S.dma(m_[:, :, :], mT_d[:, :, tg * 512:(tg + 1) * 512], m_, reads=[mT_d], writes=[m_])
                for tt in range(4):
                    t = tg * 4 + tt
                    xo_, x1_ = xo[t % 2], x1[t % 2]
                    S.dma(xo_[:, :], x_own[t * 128:(t + 1) * 128, :], xo_, reads=[x_own], writes=[xo_])
                    for cb in range(4):
                        def mmy(e, m_=m_, tt=tt, cb=cb):
                            for j in range(16):
                                ins = e.matmul(py[cb][:, :], lhsT=m_[:, j, tt * 128:(tt + 1) * 128], rhs=wo[:, j, cb * 512:(cb + 1) * 512], start=(j == 0), stop=(j == 15))
                            return ins
                        S.op('pe', mmy, reads=[m_, wo], writes=[py[cb]])
                        S.op('dve', lambda e, cb=cb, x1_=x1_: e.tensor_tensor(out=x1_[:, cb * 512:(cb + 1) * 512], in0=py[cb][:, :], in1=gt1[:, cb * 512:(cb + 1) * 512], op=ALU.mult),
                             reads=[py[cb], gt1], writes=[x1_])
                    S.op('pool', lambda e, x1_=x1_, xo_=xo_: e.tensor_tensor(out=x1_[:, :], in0=x1_[:, :], in1=xo_[:, :], op=ALU.add), reads=[x1_, xo_], writes=[x1_])
                    S.dma(X1_d[t * 128:(t + 1) * 128, :], x1_[:, :], x1_, reads=[x1_], writes=[X1_d])
            S.end_phase()

        rt1 = S.sb("rt1", [128, 16, 128], F32, True)
        rt2 = S.sb("rt2", [128, 16, 128], F32, True)
        rtg = S.sb("rtg", [128, 16, 128], F32, True)
        hn_d = dscr("hn_d", [128, 16, OWN], BF16)
        QT_d = dscr("QT_d", [16, 128, OWN], BF16)
        with phase_begin():
            R = make_norm(2)
            hst = [S.sb("hst%d" % i, [128, 16, 512], BF16) for i in range(2)]
            for t in range(16):
                hs = hst[(t // 4) % 2]
                norm_tile(R, X1_d[t * 128:(t + 1) * 128, :], X1_d, 4, 5, hs[:, :, (t % 4) * 128:(t % 4 + 1) * 128], hs)
                if t % 4 == 3:
                    tg = t // 4
                    S.dma(hn_d[:, :, tg * 512:(tg + 1) * 512], hs[:, :, :], hs, reads=[hs], writes=[hn_d])
            S.end_phase()
        with phase_begin():
            hnA = S.sb("hnA", [128, 16, OWN], BF16)
            for tg in range(4):
                S.dma(hnA[:, :, tg * 512:(tg + 1) * 512], hn_d[:, :, tg * 512:(tg + 1) * 512], hnA, reads=[hn_d], writes=[hnA])
            stg6 = [S.sb("stg6_%d" % i, [128, 16, 128], F32) for i in range(2)]
            wqb = [S.sb("wqb%d" % i, [128, 16, 128], BF16) for i in range(2)]
            qst = [S.sb("qst%d" % i, [128, OWN], BF16) for i in range(2)]
            pq = [S.ps("pq%d" % i, [128, 512], F32) for i in range(2)]
            def ldq(cq):
                if cq >= 16:
                    return
                st, wb = stg6[cq % 2], wqb[cq % 2]
                S.dma(st[:, :, :], w_q.t[:, cq * 128:(cq + 1) * 128].rearrange("(j p) n -> p j n", p=128), st, reads=[w_q], writes=[st])
                S.op('dve', lambda e: e.tensor_copy(out=wb[:, :, :], in_=st[:, :, :]), reads=[st], writes=[wb])
            ldq(0)
            for cq in range(16):
                st, wb = stg6[cq % 2], wqb[cq % 2]
                ldq(cq + 1)
                qs = qst[cq % 2]
                for tg in range(4):
                    p = pq[tg % 2]

                    def mmq2(e, p=p, wb=wb, tg=tg):
                        for j in range(16):
                            ins = e.matmul(p[:, :], lhsT=wb[:, j, :], rhs=hnA[:, j, tg * 512:(tg + 1) * 512], start=(j == 0), stop=(j == 15))
                        return ins
                    S.op('pe', mmq2, reads=[wb, hnA], writes=[p])
                    S.op('act', lambda e, p=p, qs=qs, tg=tg: e.copy(out=qs[:, tg * 512:(tg + 1) * 512], in_=p[:, :]), reads=[p], writes=[qs])
                S.dma(QT_d[cq, :, :], qs[:, :], qs, reads=[qs], writes=[QT_d])
            S.end_phase()
        with phase_begin():
            sbf = S.sb("sbf", [128, 2048], F32)
            sbk = S.sb("sbk", [128, 16, 128], BF16)
            S.dma(sbf[:, :], subT[:, :], sbf, reads=[subT], writes=[sbf])
            S.op('dve', lambda e: e.tensor_copy(out=sbk.t[:, :, :].rearrange("p a b -> p (a b)"), in_=sbf[:, :]), reads=[sbf], writes=[sbk])
            qT = [S.sb("qT%d" % i, [128, 16, 512], BF16) for i in range(2)]
            psc = [S.ps("psc%d" % i, [128, 4, 128], F32) for i in range(4)]
            ptr_ = S.ps("ptr_", [128, 3, 128], F32)
            s_sb = S.sb("s_sb", [128, 16, 128], F32)
            wk = S.sb("wk", [128, 16, 128], F32)
            top = S.sb("top", [128, 16, 16], F32)
            idx = S.sb("idx", [128, 16, 16], U32)
            idf = S.sb("idf", [128, 16, 16], F32)
            cand = S.sb("cand", [128, 8, 256], F32)
            cw = S.sb("cw", [128, 8, 256], F32)
            best = S.sb("best", [128, 8, 16], F32)
            pos = S.sb("pos", [128, 8, 16], U32)
            pa_u = S.sb("pa_u", [128, 8, 16], U32)
            pb_u = S.sb("pb_u", [128, 8, 16], U32)
            paf = S.sb("paf", [128, 8, 16], F32)
            pbf = S.sb("pbf", [128, 8, 16], F32)
            nb = S.sb("nb", [128, 8], F32)
            ex = S.sb("ex", [128, 8, 16], F32)
            zz = S.sb("zz", [128, 8], F32)
            rzz = S.sb("rzz", [128, 8], F32)
            gg = S.sb("gg", [128, 8, 16], F32)
            oh = S.sb("oh", [128, 8, 16, 16], F32)
            i1f = S.sb("i1f", [128, 8, 16], F32)
            i2f = S.sb("i2f", [128, 8, 16], F32)

            def views(b_, n):
                return [Buf("%s_v%d" % (b_.name, i), b_.t) for i in range(n)]
            s_v = views(s_sb, 4)
            top_v, idx_v, wk_v = views(top, 16), views(idx, 16), views(wk, 16)
            cand_v, best_v, pos_v, cw_v = views(cand, 8), views(best, 8), views(pos, 8), views(cw, 8)
            ex_v, zz_v = views(ex, 8), views(zz, 8)
            for tg in range(4):
                q_ = qT[tg % 2]
                S.dma(q_[:, :, :], QT_d.t[:, :, tg * 512:(tg + 1) * 512].rearrange("c p t -> p c t"), q_, reads=[QT_d], writes=[q_])
                for tt in range(4):
                    t = tg * 4 + tt
                    for g4 in range(4):
                        def mms2(e, q_=q_, tt=tt, g4=g4):
                            for c in range(4):
                                cq = g4 * 4 + c
                                ins = e.matmul(psc[g4][:, c, :], lhsT=q_[:, cq, tt * 128:(tt + 1) * 128], rhs=sbk[:, cq, :], start=True, stop=True)
                            return ins
                        S.op('pe', mms2, reads=[q_, sbk], writes=[psc[g4]])
                        S.op('act', lambda e, g4=g4: e.copy(out=s_sb[:, g4 * 4:(g4 + 1) * 4, :], in_=psc[g4][:, :, :]), reads=[psc[g4]], writes=[s_v[g4]])
                    for cq in range(16):
                        S.op('dve', lambda e, cq=cq: e.max(out=top[:, cq, 0:8], in_=s_sb[:, cq, :]), reads=[s_v[cq // 4]], writes=[top_v[cq]])
                    for cq in range(16):
                        S.op('dve', lambda e, cq=cq: e.max_index(out=idx[:, cq, 0:8], in_max=top[:, cq, 0:8], in_values=s_sb[:, cq, :]), reads=[s_v[cq // 4], top_v[cq]], writes=[idx_v[cq]])
                    for cq in range(16):
                        S.op('dve', lambda e, cq=cq: e.match_replace(out=wk[:, cq, :], in_to_replace=top[:, cq, 0:8], in_values=s_sb[:, cq, :], imm_value=-1e30), reads=[s_v[cq // 4], top_v[cq]], writes=[wk_v[cq]])
                    for cq in range(16):
                        S.op('dve', lambda e, cq=cq: e.max(out=top[:, cq, 8:16], in_=wk[:, cq, :]), reads=[wk_v[cq]], writes=[top_v[cq]])
                    for cq in range(16):
                        S.op('dve', lambda e, cq=cq: e.max_index(out=idx[:, cq, 8:16], in_max=top[:, cq, 8:16], in_values=wk[:, cq, :]), reads=[wk_v[cq], top_v[cq]], writes=[idx_v[cq]])
                    tv = lambda b_: b_.t[:, :, :].rearrange("p (h two) k -> p h two k", two=2)
                    S.op('dve', lambda e: e.tensor_tensor(out=cand.t[:, :, :].rearrange("p h (a b) -> p h a b", a=16),
                                                          in0=bc(tv(top)[:, :, 0, :], 3, [128, 8, 16, 16]),
                                                          in1=bc(tv(top)[:, :, 1, :], 2, [128, 8, 16, 16]), op=ALU.add), reads=top_v, writes=cand_v)
                    for hh in range(8):
                        S.op('dve', lambda e, hh=hh: e.max(out=best[:, hh, 0:8], in_=cand[:, hh, :]), reads=[cand_v[hh]], writes=[best_v[hh]])
                    for hh in range(8):
                        S.op('dve', lambda e, hh=hh: e.max_index(out=pos[:, hh, 0:8], in_max=best[:, hh, 0:8], in_values=cand[:, hh, :]), reads=[cand_v[hh], best_v[hh]], writes=[pos_v[hh]])
                    for hh in range(8):
                        S.op('dve', lambda e, hh=hh: e.match_replace(out=cw[:, hh, :], in_to_replace=best[:, hh, 0:8], in_values=cand[:, hh, :], imm_value=-1e30), reads=[cand_v[hh], best_v[hh]], writes=[cw_v[hh]])
                    for hh in range(8):
                        S.op('dve', lambda e, hh=hh: e.max(out=best[:, hh, 8:16], in_=cw[:, hh, :]), reads=[cw_v[hh]], writes=[best_v[hh]])
                    for hh in range(8):
                        S.op('dve', lambda e, hh=hh: e.max_index(out=pos[:, hh, 8:16], in_max=best[:, hh, 8:16], in_values=cw[:, hh, :]), reads=[cw_v[hh], best_v[hh]], writes=[pos_v[hh]])
                    S.op('dve', lambda e: e.tensor_scalar(out=nb[:, :], in0=best[:, :, 0], scalar1=-1.0, scalar2=None, op0=ALU.mult), reads=best_v, writes=[nb])
                    for hh in range(8):
                        S.op('act', lambda e, hh=hh: e.activation(out=ex[:, hh, :], in_=best[:, hh, :], func=AF.Exp, bias=nb[:, hh:hh + 1], accum_out=zz[:, hh:hh + 1]),
                             reads=[best_v[hh], nb], writes=[ex_v[hh], zz_v[hh]])
                    S.op('dve', lambda e: e.reciprocal(out=rzz[:, :], in_=zz[:, :]), reads=zz_v, writes=[rzz])
                    S.op('dve', lambda e: e.tensor_tensor(out=gg[:, :, :], in0=ex[:, :, :], in1=bc(rzz[:, :], 2, [128, 8, 16]), op=ALU.mult), reads=ex_v + [rzz], writes=[gg])
                    S.op('dve', lambda e: e.tensor_single_scalar(out=pa_u[:, :, :], in_=pos[:, :, :], scalar=4, op=ALU.logical_shift_right), reads=pos_v, writes=[pa_u])
                    S.op('dve', lambda e: e.tensor_single_scalar(out=pb_u[:, :, :], in_=pos[:, :, :], scalar=15, op=ALU.bitwise_and), reads=pos_v, writes=[pb_u])
                    S.op('dve', lambda e: e.tensor_copy(out=paf[:, :, :], in_=pa_u[:, :, :]), reads=[pa_u], writes=[paf])
                    S.op('dve', lambda e: e.tensor_copy(out=pbf[:, :, :], in_=pb_u[:, :, :]), reads=[pb_u], writes=[pbf])
                    S.op('dve', lambda e: e.tensor_copy(out=idf[:, :, :], in_=idx[:, :, :]), reads=idx_v, writes=[idf])
                    for (sel, two, dst) in ((paf, 0, i1f), (pbf, 1, i2f)):
                        S.op('dve', lambda e, sel=sel: e.tensor_tensor(out=oh[:, :, :, :], in0=bc(bc(iota_f[:, 0:16], 1, [128, 16, 16]), 1, [128, 8, 16, 16]),
                                                                       in1=bc(sel[:, :, :], 3, [128, 8, 16, 16]), op=ALU.is_equal), reads=[sel, iota_f], writes=[oh])
                        S.op('dve', lambda e, two=two: e.tensor_tensor(out=oh[:, :, :, :], in0=oh[:, :, :, :],
                                                                       in1=bc(tv(idf)[:, :, two, :], 2, [128, 8, 16, 16]), op=ALU.mult), reads=[oh, idf], writes=[oh])
                        S.op('dve', lambda e, dst=dst: e.tensor_reduce(out=dst[:, :, :], in_=oh[:, :, :, :], axis=AX.X, op=ALU.add), reads=[oh], writes=[dst])

                    def trr(e):
                        e.transpose(out=ptr_[:, 0, :], in_=i1f.t[:, :, :].rearrange("p h k -> p (h k)"), identity=identf[:, :])
                        e.transpose(out=ptr_[:, 1, :], in_=i2f.t[:, :, :].rearrange("p h k -> p (h k)"), identity=identf[:, :])
                        return e.transpose(out=ptr_[:, 2, :], in_=gg.t[:, :, :].rearrange("p h k -> p (h k)"), identity=identf[:, :])
                    S.op('pe', trr, reads=[i1f, i2f, gg, identf], writes=[ptr_])
                    S.op('act', lambda e, t=t: e.copy(out=rt1[:, t, :], in_=ptr_[:, 0, :]), reads=[ptr_], writes=[rt1])
                    S.op('act', lambda e, t=t: e.copy(out=rt2[:, t, :], in_=ptr_[:, 1, :]), reads=[ptr_], writes=[rt2])
                    S.op('act', lambda e, t=t: e.copy(out=rtg[:, t, :], in_=ptr_[:, 2, :]), reads=[ptr_], writes=[rtg])
            S.end_phase()

        WTp = S.sb("WTp", [128, 256, 128], BF16, True)
        PT_d = dscr("PT_d", [16, 128, 8 * 256], BF16)
        kkc = [0]

        def wb_dve(p, sbi, Aoh, Boh, part=None):
            t = 2 * p + sbi // 8
            n0 = (sbi % 8) * 16
            A_, B_ = Aoh[sbi % 2], Boh[sbi % 2]
            if part in (None, 0):
                S.op('dve', lambda e: e.tensor_tensor(out=A_[:, :, :], in0=bc(iota_f[:, :], 1, [128, 16, 128]),
                                                      in1=bc(rt1[:, t, n0:n0 + 16], 2, [128, 16, 128]), op=ALU.is_equal),
                     reads=[iota_f, rt1], writes=[A_])
            if part in (None, 1):
                S.op('dve', lambda e: e.tensor_tensor(out=A_[:, :, :], in0=A_[:, :, :],
                                                      in1=bc(rtg[:, t, n0:n0 + 16], 2, [128, 16, 128]), op=ALU.mult),
                     reads=[A_, rtg], writes=[A_])
            if part in (None, 2):
                S.op('dve', lambda e: e.tensor_tensor(out=B_[:, :, :], in0=bc(iota_f[:, :], 1, [128, 16, 128]),
                                                      in1=bc(rt2[:, t, n0:n0 + 16], 2, [128, 16, 128]), op=ALU.is_equal),
                     reads=[iota_f, rt2], writes=[B_])

        def wb_pe(p, sbi, q4, Aoh, Boh, pW):
            A_, B_ = Aoh[sbi % 2], Boh[sbi % 2]
            kkc[0] += 1
            pw = pW[kkc[0] % 2]

            def mmw(e):
                for n in range(4):
                    ins = e.matmul(pw[:, n, :], lhsT=B_[:, q4 * 4 + n, :], rhs=A_[:, q4 * 4 + n, :], start=True, stop=True)
                return ins
            S.op('pe', mmw, reads=[A_, B_], writes=[pw])
            nn = (sbi // 8) * 128 + (sbi % 8) * 16 + q4 * 4
            S.op('dve', lambda e: e.tensor_copy(out=WTp[:, nn:nn + 4, :], in_=pw[:, :, :]), reads=[pw], writes=[WTp])

        gt2 = S.sb("gt2", [128, D], F32, True)
        fg = S.sb("fg", [128, D], F32, True)
        x1t = S.sb("x1t", [128, D], F32, True)
        xf = [S.sb("xf%d" % i, [128, D], F32, True) for i in range(2)]
        jk2 = S.sb("jk2", [128, D], BF16, True)
        fs = [S.sb("fs%d" % i, [128, 1], F32, True) for i in range(2)]
        fd = [S.sb("fd%d" % i, [128, 1], F32, True) for i in range(2)]
        fr = [S.sb("fr%d" % i, [128, 1], F32, True) for i in range(2)]

        def epi_stage(p, a_, st):
            t = 2 * p + a_
            xf_ = xf[a_]
            fa_, fb_, fc_ = fs[a_], fd[a_], fr[a_]
            if st == 0:
                S.dma(x1t[:, :], X1_d[t * 128:(t + 1) * 128, :], x1t, reads=[X1_d], writes=[x1t])
            elif st == 1:
                S.op('pool', lambda e: e.tensor_tensor(out=xf_[:, :], in0=xf_[:, :], in1=x1t[:, :], op=ALU.add), reads=[xf_, x1t], writes=[xf_])
            elif st == 2:
                S.op('act', lambda e: e.activation(out=jk2[:, :], in_=xf_[:, :], func=AF.Square, accum_out=fa_[:, :]), reads=[xf_], writes=[jk2, fa_])
                S.op('act', lambda e: e.activation(out=fb_[:, :], in_=fa_[:, :], func=AF.Sqrt, scale=1.0 / D, bias=EPS), reads=[fa_], writes=[fb_])
            elif st == 3:
                S.op('dve', lambda e: e.reciprocal(out=fc_[:, :], in_=fb_[:, :]), reads=[fb_], writes=[fc_])
                S.op('dve', lambda e: e.scalar_tensor_tensor(out=xf_[:, :], in0=xf_[:, :], scalar=fc_[:, 0:1], in1=fg[:, :], op0=ALU.mult, op1=ALU.mult),
                     reads=[xf_, fc_, fg], writes=[xf_])

        def epi_compute(p, a_):
            for st in range(4):
                epi_stage(p, a_, st)

        def epi_store(p, a_):
            t = 2 * p + a_
            xf_ = xf[a_]
            S.dma(out[t * 128:(t + 1) * 128, :], xf_[:, :], xf_, reads=[xf_], writes=[out])

        def epilogue(p):
            for a_ in range(2):
                epi_compute(p, a_)
                epi_store(p, a_)

        with phase_begin():
            S.dma(gt2[:, :], mrow.t[0, 5 * D:6 * D].partition_broadcast(128), gt2, reads=[mrow], writes=[gt2])
            S.dma(fg[:, :], final_g.t.partition_broadcast(128), fg, reads=[final_g], writes=[fg])
            Aoh = [S.sb("Aoh%d" % i, [128, 16, 128], BF16) for i in range(2)]
            Boh = [S.sb("Boh%d" % i, [128, 16, 128], BF16) for i in range(2)]
            pW = [S.ps("pW%d" % i, [128, 4, 128], F32) for i in range(2)]
            for sbi in range(16):
                wb_dve(0, sbi, Aoh, Boh)
                for q4 in range(4):
                    wb_pe(0, sbi, q4, Aoh, Boh, pW)
            S.end_phase()

        for p in range(8):
            with phase_begin():
                hnP = S.sb("hnP", [128, 16, 256], BF16)
                S.dma(hnP[:, :, :], hn_d[:, :, p * 256:(p + 1) * 256], hnP, reads=[hn_d], writes=[hnP])
                NB = 4
                ut = [S.sb("ut%d" % i, [128, 16, 128], BF16) for i in range(NB)]
                gl = [S.sb("gl%d" % i, [128, 256], F32) for i in range(2)]
                pst = [S.sb("pst%d" % i, [128, 8, 256], BF16) for i in range(2)]
                pSe = [S.ps("pSe%d" % i, [128, 512], F32) for i in range(2)]

                def ldu(c):
                    if c >= 128:
                        return
                    u_ = ut[c % NB]
                    S.dma(u_.t[:, :, :].rearrange("p j e -> p (j e)"), UT_d[c, :, :], u_, reads=[UT_d], writes=[u_])

                def s_grp(c):
                    u_ = ut[c % NB]
                    ps_ = pSe[c % 2]

                    def mmse(e):
                        for j in range(16):
                            ins = e.matmul(ps_[:, 0:256], lhsT=u_[:, j, :], rhs=hnP[:, j, :], start=(j == 0), stop=(j == 15))
                        return ins
                    S.op('pe', mmse, reads=[u_, hnP], writes=[ps_])
                ldu(0)
                ldu(1)
                ldu(2)
                s_grp(0)
                for c in range(128):
                    if p >= 1:
                        for a_, cb_ in ((0, 8), (1, 48)):
                            if c == cb_:
                                epi_stage(p - 1, a_, 0)
                            if c == cb_ + 8:
                                epi_stage(p - 1, a_, 1)
                            if c == cb_ + 16:
                                epi_stage(p - 1, a_, 2)
                            if c == cb_ + 22:
                                epi_stage(p - 1, a_, 3)
                            if c == cb_ + 32:
                                epi_store(p - 1, a_)
                    ldu(c + 3)
                    if c + 1 < 128:
                        s_grp(c + 1)
                    ps_ = pSe[c % 2]
                    g_ = gl[c % 2]
                    st_ = pst[(c // 8) % 2]
                    S.op('act', lambda e, ps_=ps_, g_=g_: e.activation(out=g_[:, :], in_=ps_[:, 0:256], func=AF.Gelu_apprx_tanh), reads=[ps_], writes=[g_])
                    S.op('dve', lambda e, g_=g_, st_=st_, c=c: e.tensor_tensor(out=st_[:, c % 8, :], in0=g_[:, :], in1=WTp[:, :, c], op=ALU.mult),
                         reads=[g_, WTp], writes=[st_])
                    if c % 8 == 7:
                        S.dma(PT_d[c // 8, :, :], st_.t[:, :, :].rearrange("p a n -> p (a n)"), st_, reads=[st_], writes=[PT_d])
                S.end_phase()
            with phase_begin():
                ptl = [S.sb("ptl%d" % i, [128, 8, 256], BF16) for i in range(2)]
                vvl = [S.sb("vvl%d" % i, [128, 2, 1024], BF16) for i in range(4)]
                Aoh = [S.sb("Aoh%d" % i, [128, 16, 128], BF16) for i in range(2)]
                Boh = [S.sb("Boh%d" % i, [128, 16, 128], BF16) for i in range(2)]
                pOu = [S.ps("pOu%d" % i, [128, 512], F32) for i in range(4)]
                pW = [S.ps("pW%d" % i, [128, 4, 128], F32) for i in range(2)]

                def ldp(g):
                    S.dma(ptl[g % 2].t[:, :, :].rearrange("p a n -> p (a n)"), PT_d[g % 16, :, :], ptl[g % 2], reads=[PT_d], writes=[ptl[g % 2]])

                def ldv(g, half):
                    if g >= 64:
                        return
                    c0 = g * 2
                    S.dma(vvl[g % 4][:, :, :], VB_d.t[c0 * 128:(c0 + 2) * 128, half * 1024:(half + 1) * 1024].rearrange("(t e) d -> e t d", e=128),
                          vvl[g % 4], reads=[VB_d], writes=[vvl[g % 4]])
                for half in range(2):
                    ldp(half * 16)
                    ldv(0, half)
                    ldv(1, half)
                    ldv(2, half)
                    for c in range(128):
                        gp = half * 16 + c // 8
                        gv = c // 2
                        if c % 8 == 0 and c + 8 < 128:
                            ldp(gp + 1)
                        if c % 2 == 0:
                            ldv(gv + 3, half)
                        pl_, vl_ = ptl[gp % 2], vvl[gv % 4]

                        def mmv2(e, pl_=pl_, vl_=vl_, c=c):
                            for a_ in range(2):
                                for cbh in range(2):
                                    ins = e.matmul(pOu[a_ * 2 + cbh][:, :], lhsT=pl_[:, c % 8, a_ * 128:(a_ + 1) * 128],
                                                   rhs=vl_[:, c % 2, cbh * 512:(cbh + 1) * 512], start=(c == 0), stop=(c == 127))
                            return ins
                        S.op('pe', mmv2, reads=[pl_, vl_], writes=pOu)
                        if p + 1 < 8:
                            s_ = half * 128 + c
                            if s_ == 0:
                                wb_dve(p + 1, 0, Aoh, Boh)
                            if s_ % 16 in (3, 7, 11, 15):
                                wb_pe(p + 1, s_ // 16, (s_ % 16 - 3) // 4, Aoh, Boh, pW)
                            if s_ % 16 in (4, 8, 12) and s_ // 16 + 1 < 16:
                                wb_dve(p + 1, s_ // 16 + 1, Aoh, Boh, part=(s_ % 16) // 4 - 1)
                    for a_ in range(2):
                        for cbh in range(2):
                            col = half * 1024 + cbh * 512
                            S.op('dve', lambda e, a_=a_, cbh=cbh, col=col: e.tensor_tensor(out=xf[a_][:, col:col + 512], in0=pOu[a_ * 2 + cbh][:, :], in1=gt2[:, col:col + 512], op=ALU.mult),
                                 reads=[pOu[a_ * 2 + cbh], gt2], writes=[xf[a_]])
                if p == 7:
                    epilogue(7)
                S.end_phase()
    return nc


def _rope_tables():
    t = np.arange(NTOK)
    row = (t // 64).astype(np.float32)
    col = (t % 64).astype(np.float32)
    freqs = (10000.0 ** (-np.arange(16, dtype=np.float32) / 16)).astype(np.float32)
    ar = row[:, None] * freqs[None, :]
    ac = col[:, None] * freqs[None, :]
    ang = np.concatenate([ar, ar, ac, ac], axis=-1).astype(np.float32)
    cos = np.cos(ang).astype(np.float32)
    sin = np.sin(ang).astype(np.float32)
    sgn = np.concatenate([-np.ones(16), np.ones(16), -np.ones(16), np.ones(16)]).astype(np.float32)
    return np.concatenate([cos, sin * sgn[None, :]], axis=1).astype(np.float32)


def _local_rows(c):
    base = 32 * c - 4
    rows = [base + i for i in range(40)]
    if c == 0:
        rows[0:4] = [6, 7, 8, 9]
    if c == NCORE - 1:
        rows[36:40] = [248, 249, 246, 247]
    return rows


def _na_bias(rpb, c):
    rows = _local_rows(c)
    outb = np.full((8, 5, 5, 128, 128), NEG, np.float32)
    qc = np.arange(64)
    cstart = np.clip(qc - 8, 0, 48)
    for slot, j in enumerate([0, 1, 2, 14, 15]):
        seen = set()
        for kr in range(10):
            gk = rows[2 * j + kr]
            if gk in seen or gk < 0 or gk > 255:
                continue
            seen.add(gk)
            for a in range(2):
                gq = rows[2 * j + 4 + a]
                rs = min(max(gq - 4, 0), 248)
                if not (rs <= gk < rs + 8):
                    continue
                dr = gk - gq + 7
                kc = np.arange(64)
                valid = (kc[:, None] >= cstart[None, :]) & (kc[:, None] < cstart[None, :] + 16)
                dc = kc[:, None] - qc[None, :] + 15
                vals = rpb[:, dr, :][:, np.clip(dc, 0, 30)]
                blockv = np.where(valid[None], vals, NEG).astype(np.float32)
                chunk, kin = (kr * 64) // 128, (kr * 64) % 128
                outb[:, slot, chunk, kin:kin + 64, a * 64:(a + 1) * 64] = blockv
    return np.ascontiguousarray(outb.transpose(0, 3, 1, 2, 4)).reshape(8, 128, 5 * 5 * 128)


def kernel(x, c, ctx, c_ctx, w_mod, b_mod, norm1_g, norm2_g, w_in, lambda_q1, lambda_k1, lambda_q2, lambda_k2,
           subln_g, na_rpb, w_branch_a, w_branch_b, w_out, peer_w_q, peer_subkeys, peer_u, peer_v, final_g):
    f = lambda a: np.ascontiguousarray(np.asarray(a, dtype=np.float32))
    x2 = f(x)[0]
    rope = _rope_tables()
    rope_q = rope * np.float32(0.125)
    cT = np.stack([f(c)[0].reshape(16, 128).T, f(c_ctx).reshape(16, 128).T], axis=-1).reshape(128, 32)
    shared = {
        "x_all": x2, "ctx": f(ctx)[0], "cT": f(cT),
        "g1T": f(f(norm1_g)[0].reshape(16, 128).T), "g2T": f(f(norm2_g)[0].reshape(16, 128).T),
        "w_mod": f(w_mod)[0], "b_mod": f(b_mod)[0], "w_in": f(w_in)[0],
        "lams": f(np.concatenate([f(lambda_q1)[0], f(lambda_k1)[0], f(lambda_q2)[0], f(lambda_k2)[0]])),
        "subg": f(subln_g)[0], "rope_all": rope,
        "w_a": f(w_branch_a)[0], "w_b": f(w_branch_b)[0], "w_o": f(w_out)[0], "w_q": f(peer_w_q)[0],
        "subT": f(f(peer_subkeys)[0].transpose(3, 0, 1, 2).reshape(128, 16 * 128)),
        "peer_u": f(peer_u)[0], "peer_v": f(peer_v)[0], "final_g": f(final_g),
    }
    rpb = f(na_rpb)[0]
    xg = x2.reshape(256, 64, D)
    in_maps = []
    for ci in range(NCORE):
        rows = _local_rows(ci)
        halo = np.concatenate([xg[rows[0:4]].reshape(256, D), xg[rows[36:40]].reshape(256, D)], axis=0)
        m = dict(shared)
        m["x_own"] = f(x2[ci * OWN:(ci + 1) * OWN])
        m["x_halo"] = f(halo)
        m["na_bias"] = _na_bias(rpb, ci)
        m["rope_own"] = f(rope_q[ci * OWN:(ci + 1) * OWN])
        in_maps.append(m)
    nc = build_program()
    res = run_bass_kernel_spmd(nc, in_maps, core_ids=list(range(NCORE)))
    outs = [np.asarray(r["out"], dtype=np.float32) for r in res.results]
    return np.concatenate(outs, axis=0).reshape(1, NTOK, D)
```
